# Optimizing a Trainium2 kernel written in Bass

```python
import math
import jax, jax.numpy as jnp
from jax import lax
import numpy as np

D_MODEL = 2048
BATCH = 2
SEQ = 4096
DEPTH = 2

GRID_W = 64
CTX_LEN = 256
NA_HEADS = 16
NA_HEAD_DIM = 64
NA_WIN_R = 8
NA_WIN_C = 16
GLA_HEADS = 4
GLA_DK = 64
GLA_DV = 128
GLA_GATE_RANK = 16
GLA_GATE_TAU = 16.0
GLA_CHUNK = 64
GDN_HEADS = 4
GDN_DK = 128
GDN_DV = 128
GDN_CONV = 5
GDN_CHUNK = 64
NA_W = NA_HEADS * NA_HEAD_DIM
GLA_W = GLA_HEADS * GLA_DV
GDN_W = GDN_HEADS * GDN_DV
GDN_QKV_W = 2 * GDN_HEADS * GDN_DK + GDN_W
MIX_W = NA_W + GLA_W + GDN_W
IN_SPLITS = (
    NA_W, NA_W, NA_W,
    GLA_HEADS * GLA_DK, GLA_HEADS * GLA_DK, GLA_W, GLA_W,
    2 * GLA_GATE_RANK,
    GDN_QKV_W,
    GDN_W,
    2 * GDN_HEADS, 2 * GDN_HEADS,
)
IN_W = sum(IN_SPLITS)
N_EXPERTS = 16
EXPERT_FF = 2048
EC_CAPACITY = 2
ROPE_BASE = 10000.0
NORM_EPS = 1e-6

kernel_name = "hybrid_na_gla_gdn_ec_moe_prefix_dit"


def rmsnorm(x, w):
    xf = x.astype(jnp.float32)
    y = xf * lax.rsqrt(jnp.mean(xf * xf, axis=-1, keepdims=True) + NORM_EPS)
    return (y * w.astype(jnp.float32)).astype(x.dtype)


def l2norm(x):
    return x * lax.rsqrt(jnp.sum(x * x, axis=-1, keepdims=True) + 1e-6)


def split_proj(p):
    return jnp.split(p, np.cumsum(IN_SPLITS)[:-1].tolist(), axis=-1)


def to_heads(a, n_heads):
    b, t, _ = a.shape
    return jnp.swapaxes(a.reshape(b, t, n_heads, -1), 1, 2).astype(jnp.float32)


def axial_rope(x, row_pos, col_pos):
    half = x.shape[-1] // 2
    nf = half // 2
    freqs = ROPE_BASE ** (-jnp.arange(nf, dtype=jnp.float32) / nf)

    def rot(xp, pos):
        ang = jnp.asarray(pos, jnp.float32)[:, None] * freqs[None, :]
        cos = jnp.cos(ang).astype(x.dtype)[None, :, None, :]
        sin = jnp.sin(ang).astype(x.dtype)[None, :, None, :]
        x1, x2 = xp[..., :nf], xp[..., nf:]
        return jnp.concatenate([x1 * cos - x2 * sin, x2 * cos + x1 * sin], axis=-1)

    return jnp.concatenate([rot(x[..., :half], row_pos), rot(x[..., half:], col_pos)], axis=-1)


def na_latent(q, k, v, k_ctx, v_ctx, rpb):
    b, s, h, dh = q.shape
    rows = s // GRID_W
    wr = min(NA_WIN_R, rows)
    r = np.arange(rows)
    ridx = np.clip(r - wr // 2, 0, rows - wr)[:, None] + np.arange(wr)[None, :]
    cq = np.arange(GRID_W)
    cstart = np.clip(cq - NA_WIN_C // 2, 0, GRID_W - NA_WIN_C)
    colmask = (cq[None, :] >= cstart[:, None]) & (cq[None, :] < cstart[:, None] + NA_WIN_C)
    dr = ridx - r[:, None] + NA_WIN_R - 1
    dc = np.clip(cq[None, :] - cq[:, None] + NA_WIN_C - 1, 0, 2 * NA_WIN_C - 2)
    bias = rpb[:, dr[:, None, :, None], dc[None, :, None, :]].astype(jnp.float32)
    bias = jnp.where(colmask[None, None, :, None, :], bias, -jnp.inf).reshape(h, rows, GRID_W, wr * GRID_W)
    qg = (q * dh ** -0.5).reshape(b, rows, GRID_W, h, dh)
    kg = k.reshape(b, rows, GRID_W, h, dh)[:, ridx]
    vg = v.reshape(b, rows, GRID_W, h, dh)[:, ridx]
    n_loc = wr * GRID_W
    s_loc = jnp.einsum('brqhd,brjkhd->bhrqjk', qg, kg).astype(jnp.float32).reshape(b, h, rows, GRID_W, n_loc) + bias
    s_ctx = jnp.einsum('brqhd,blhd->bhrql', qg, k_ctx).astype(jnp.float32)
    p = jax.nn.softmax(jnp.concatenate([s_loc, s_ctx], axis=-1), axis=-1).astype(v.dtype)
    p_loc = p[..., :n_loc].reshape(b, h, rows, GRID_W, wr, GRID_W)
    o = (jnp.einsum('bhrqjk,brjkhd->brqhd', p_loc, vg)
         + jnp.einsum('bhrql,blhd->brqhd', p[..., n_loc:], v_ctx))
    return o.reshape(b, s, h * dh)


def na_context(q, k, v):
    b, l, h, dh = q.shape
    s = jnp.einsum('blhd,bmhd->bhlm', q * dh ** -0.5, k).astype(jnp.float32)
    p = jax.nn.softmax(s, axis=-1).astype(v.dtype)
    return jnp.einsum('bhlm,bmhd->blhd', p, v).reshape(b, l, h * dh)


def gla_chunked(q, k, v, log_a, s0):
    b, h, t, dk = q.shape
    dv = v.shape[-1]
    n = t // GLA_CHUNK
    ar = np.arange(GLA_CHUNK)
    incl = ar[:, None] >= ar[None, :]

    def chunks(a):
        return jnp.moveaxis(a.reshape(b, h, n, GLA_CHUNK, a.shape[-1]), 2, 0)

    def step(state, xs):
        qc, kc, vc, lac = xs
        cum = jnp.cumsum(lac, axis=2)
        diff = jnp.where(incl[None, None, :, :, None], cum[:, :, :, None, :] - cum[:, :, None, :, :], -jnp.inf)
        att = jnp.einsum('bhtd,bhsd,bhtsd->bhts', qc, kc, jnp.exp(diff))
        o = jnp.einsum('bhtd,bhde->bhte', qc * jnp.exp(cum), state) + jnp.einsum('bhts,bhse->bhte', att, vc)
        last = cum[:, :, -1:, :]
        state = (jnp.exp(last[:, :, 0, :])[..., None] * state
                 + jnp.einsum('bhsd,bhse->bhde', kc * jnp.exp(last - cum), vc))
        return state, o

    state, o = lax.scan(step, s0, tuple(chunks(a) for a in (q, k, v, log_a)))
    return state, jnp.moveaxis(o, 0, 2).reshape(b, h, t, dv)


def gdn_chunked(q, k, v, g, beta, s0):
    b, h, t, dk = q.shape
    dv = v.shape[-1]
    cs = GDN_CHUNK
    n = t // cs
    q = q.reshape(b, h, n, cs, dk)
    k = k.reshape(b, h, n, cs, dk)
    v = v.reshape(b, h, n, cs, dv)
    gc = jnp.cumsum(g.reshape(b, h, n, cs), axis=-1)
    beta = beta.reshape(b, h, n, cs)
    ar = np.arange(cs)
    incl = ar[:, None] >= ar[None, :]
    strict = ar[:, None] > ar[None, :]
    decay = jnp.exp(jnp.where(incl, gc[..., :, None] - gc[..., None, :], -jnp.inf))
    kb = k * beta[..., None]
    m = jnp.where(strict, jnp.einsum('bhntd,bhnsd->bhnts', kb, k) * decay, 0.0)
    lower = m + jnp.eye(cs, dtype=m.dtype)
    rhs = jnp.concatenate([v * beta[..., None], kb * jnp.exp(gc)[..., None]], axis=-1)
    sol = lax.linalg.triangular_solve(lower, rhs, left_side=True, lower=True)
    u, w = sol[..., :dv], sol[..., dv:]
    a_qk = jnp.einsum('bhntd,bhnsd->bhnts', q, k) * decay
    q_dec = q * jnp.exp(gc)[..., None]
    k_dec = k * jnp.exp(gc[..., -1:] - gc)[..., None]
    g_last = jnp.exp(gc[..., -1])

    def step(state, xs):
        u_c, w_c, qd_c, a_c, kd_c, gl_c = xs
        v_new = u_c - jnp.einsum('bhtd,bhde->bhte', w_c, state)
        o = jnp.einsum('bhtd,bhde->bhte', qd_c, state) + jnp.einsum('bhts,bhse->bhte', a_c, v_new)
        state = state * gl_c[..., None, None] + jnp.einsum('bhsd,bhse->bhde', kd_c, v_new)
        return state, o

    xs = tuple(jnp.moveaxis(a, 2, 0) for a in (u, w, q_dec, a_qk, k_dec, g_last))
    state, o = lax.scan(step, s0, xs)
    return state, jnp.moveaxis(o, 0, 2).reshape(b, h, t, dv)


def bidir_scan(scan_fn, ctx_f, ctx_b, lat_f, lat_b, s0):
    def flip(args):
        return tuple(jnp.flip(a, axis=2) for a in args)
    s_cf, o_cf = scan_fn(*ctx_f, s0)
    s_cb, o_cb = scan_fn(*flip(ctx_b), s0)
    _, o_xf = scan_fn(*lat_f, s_cf)
    _, o_xb = scan_fn(*flip(lat_b), s_cb)
    return o_xf + jnp.flip(o_xb, axis=2), o_cf + jnp.flip(o_cb, axis=2)


def gated_head_norm(o, gate, norm_w, dtype):
    b, h, t, dv = o.shape
    o = rmsnorm(jnp.swapaxes(o, 1, 2), norm_w).reshape(b, t, h * dv)
    return (o * jax.nn.silu(gate.astype(jnp.float32))).astype(dtype)


def short_conv(u, w):
    kw = w.shape[0]
    y = lax.conv_general_dilated(u, w[:, None, :], window_strides=(1,), padding=((kw // 2, kw // 2),),
                                 dimension_numbers=('NWC', 'WIO', 'NWC'), feature_group_count=u.shape[-1])
    return jax.nn.silu(y)


def gla_prep(p, gate_w, gate_b, row_pos, col_pos, rope):
    q, k, v, g, lr = p[3:8]
    b, t, _ = q.shape
    if rope:
        q = axial_rope(q.reshape(b, t, GLA_HEADS, GLA_DK), row_pos, col_pos).reshape(b, t, -1)
        k = axial_rope(k.reshape(b, t, GLA_HEADS, GLA_DK), row_pos, col_pos).reshape(b, t, -1)
    z = jnp.einsum('btnr,nrk->nbtk', lr.reshape(b, t, 2, GLA_GATE_RANK), gate_w) + gate_b[:, None, None, :]
    log_a = jax.nn.log_sigmoid(z.astype(jnp.float32)) / GLA_GATE_TAU
    qh = to_heads(q, GLA_HEADS) * GLA_DK ** -0.5
    return (qh, to_heads(k, GLA_HEADS), to_heads(v, GLA_HEADS),
            to_heads(log_a[0], GLA_HEADS), to_heads(log_a[1], GLA_HEADS), g)


def gdn_prep(p, conv_w, a_log, dt_bias):
    qkv, z, a, bb = p[8:12]
    b, t, _ = qkv.shape
    qkv = short_conv(qkv, conv_w)
    q, k, v = jnp.split(qkv, [GDN_HEADS * GDN_DK, 2 * GDN_HEADS * GDN_DK], axis=-1)
    q = l2norm(to_heads(q, GDN_HEADS)) * GDN_DK ** -0.5
    k = l2norm(to_heads(k, GDN_HEADS))
    v = to_heads(v, GDN_HEADS)
    a = jnp.transpose(a.reshape(b, t, 2, GDN_HEADS).astype(jnp.float32), (2, 0, 3, 1))
    bb = jnp.transpose(bb.reshape(b, t, 2, GDN_HEADS).astype(jnp.float32), (2, 0, 3, 1))
    a_log = a_log.astype(jnp.float32)[:, None, :, None]
    dt_bias = dt_bias.astype(jnp.float32)[:, None, :, None]
    g = -jnp.exp(a_log) * jax.nn.softplus(a + dt_bias)
    beta = jax.nn.sigmoid(bb)
    return q, k, v, g, beta, z


def token_mixers(nx, nc, w_in, w_out, na_rpb, gla_gate_w, gla_gate_b, gla_norm_w,
                 gdn_conv_w, gdn_a_log, gdn_dt_bias, gdn_norm_w, row_pos, col_pos, with_ctx_out):
    b = nx.shape[0]
    dt = nx.dtype
    px = split_proj(nx @ w_in)
    pc = split_proj(nc @ w_in)

    def heads(a, nh):
        return a.reshape(a.shape[0], a.shape[1], nh, -1)

    qx, kx, vx = (heads(a, NA_HEADS) for a in px[0:3])
    qc, kc, vc = (heads(a, NA_HEADS) for a in pc[0:3])
    ox_na = na_latent(qx, kx, vx, kc, vc, na_rpb)

    xq, xk, xv, xaf, xab, xg = gla_prep(px, gla_gate_w, gla_gate_b, row_pos, col_pos, True)
    cq, ck, cv, caf, cab, cg = gla_prep(pc, gla_gate_w, gla_gate_b, row_pos, col_pos, False)
    s0 = jnp.zeros((b, GLA_HEADS, GLA_DK, GLA_DV), jnp.float32)
    ox_gla, oc_gla = bidir_scan(gla_chunked, (cq, ck, cv, caf), (cq, ck, cv, cab),
                                (xq, xk, xv, xaf), (xq, xk, xv, xab), s0)

    yq, yk, yv, yg, yb, yz = gdn_prep(px, gdn_conv_w, gdn_a_log, gdn_dt_bias)
    dq, dk, dv, dg, db, dz = gdn_prep(pc, gdn_conv_w, gdn_a_log, gdn_dt_bias)
    s0d = jnp.zeros((b, GDN_HEADS, GDN_DK, GDN_DV), jnp.float32)
    ox_gdn, oc_gdn = bidir_scan(gdn_chunked, (dq, dk, dv, dg[0], db[0]), (dq, dk, dv, dg[1], db[1]),
                                (yq, yk, yv, yg[0], yb[0]), (yq, yk, yv, yg[1], yb[1]), s0d)

    out_x = jnp.concatenate([ox_na, gated_head_norm(ox_gla, xg, gla_norm_w, dt),
                             gated_head_norm(ox_gdn, yz, gdn_norm_w, dt)], axis=-1) @ w_out
    if not with_ctx_out:
        return out_x, None
    out_c = jnp.concatenate([na_context(qc, kc, vc), gated_head_norm(oc_gla, cg, gla_norm_w, dt),
                             gated_head_norm(oc_gdn, dz, gdn_norm_w, dt)], axis=-1) @ w_out
    return out_x, out_c


def expert_choice_ffn(h, w_router, w_gate, w_up, w_down):
    b, n, _ = h.shape
    cap = EC_CAPACITY * n // N_EXPERTS
    aff = jax.nn.softmax((h @ w_router).astype(jnp.float32), axis=-1)
    gate, idx = lax.top_k(jnp.swapaxes(aff, 1, 2), cap)
    bidx = jnp.arange(b)[:, None, None]
    xs = h[bidx, idx]
    hid = jax.nn.silu(jnp.einsum('becd,edf->becf', xs, w_gate)) * jnp.einsum('becd,edf->becf', xs, w_up)
    y = jnp.einsum('becf,efd->becd', hid, w_down) * gate[..., None].astype(h.dtype)
    return jnp.zeros_like(h).at[bidx, idx].add(y)


def setup_inputs(seed: int = 0) -> dict:
    key = jax.random.key(seed)
    ks = jax.random.split(key, 24)
    f32 = jnp.float32
    D = D_MODEL

    def nrm(k, shape, scale):
        return jax.random.normal(k, shape, f32) * scale

    dt = jnp.exp(jax.random.uniform(ks[16], (DEPTH, 2, GDN_HEADS), f32, math.log(1e-3), math.log(1e-1)))
    return {
        "x": nrm(ks[0], (BATCH, SEQ, D), 1.0),
        "c": nrm(ks[1], (BATCH, D), 1.0),
        "ctx": nrm(ks[2], (BATCH, CTX_LEN, D), 1.0),
        "c_ctx": nrm(ks[3], (D,), 1.0),
        "w_ada": nrm(ks[4], (DEPTH, D, 6 * D), 0.5 * D ** -0.5),
        "b_ada": nrm(ks[5], (DEPTH, 6 * D), 0.02),
        "norm_mix_w": 1.0 + nrm(ks[6], (DEPTH, D), 0.02),
        "norm_ffn_w": 1.0 + nrm(ks[7], (DEPTH, D), 0.02),
        "w_in": nrm(ks[8], (DEPTH, D, IN_W), D ** -0.5),
        "w_out": nrm(ks[9], (DEPTH, MIX_W, D), MIX_W ** -0.5),
        "na_rpb": nrm(ks[10], (DEPTH, NA_HEADS, 2 * NA_WIN_R - 1, 2 * NA_WIN_C - 1), 0.1),
        "gla_gate_w": nrm(ks[11], (DEPTH, 2, GLA_GATE_RANK, GLA_HEADS * GLA_DK), GLA_GATE_RANK ** -0.5),
        "gla_gate_b": nrm(ks[12], (DEPTH, 2, GLA_HEADS * GLA_DK), 0.1),
        "gla_norm_w": 1.0 + nrm(ks[13], (DEPTH, GLA_DV), 0.02),
        "gdn_conv_w": nrm(ks[14], (DEPTH, GDN_CONV, GDN_QKV_W), GDN_CONV ** -0.5),
        "gdn_a_log": jnp.log(jax.random.uniform(ks[15], (DEPTH, 2, GDN_HEADS), f32, 1.0, 16.0)),
        "gdn_dt_bias": dt + jnp.log(-jnp.expm1(-dt)),
        "gdn_norm_w": 1.0 + nrm(ks[17], (DEPTH, GDN_DV), 0.02),
        "w_router": nrm(ks[18], (DEPTH, D, N_EXPERTS), D ** -0.5),
        "w_exp_gate": nrm(ks[19], (DEPTH, N_EXPERTS, D, EXPERT_FF), D ** -0.5),
        "w_exp_up": nrm(ks[20], (DEPTH, N_EXPERTS, D, EXPERT_FF), D ** -0.5),
        "w_exp_down": nrm(ks[21], (DEPTH, N_EXPERTS, EXPERT_FF, D), EXPERT_FF ** -0.5),
        "final_norm_w": 1.0 + nrm(ks[22], (D,), 0.02),
    }


def modulated_norm(h, w, shift, scale):
    return rmsnorm(h, w) * (1 + scale) + shift


def reference(x, c, ctx, c_ctx, w_ada, b_ada, norm_mix_w, norm_ffn_w, w_in, w_out, na_rpb,
              gla_gate_w, gla_gate_b, gla_norm_w, gdn_conv_w, gdn_a_log, gdn_dt_bias, gdn_norm_w,
              w_router, w_exp_gate, w_exp_up, w_exp_down, final_norm_w):
    s = x.shape[1]
    pos = np.arange(s)
    row_pos, col_pos = pos // GRID_W, pos % GRID_W
    hx, hc = x, ctx
    for l in range(DEPTH):
        ctx_out = l < DEPTH - 1
        mod_x = (jax.nn.silu(c) @ w_ada[l] + b_ada[l])[:, None, :]
        mod_c = (jax.nn.silu(c_ctx) @ w_ada[l] + b_ada[l])[None, None, :]
        sh1x, sc1x, g1x, sh2x, sc2x, g2x = jnp.split(mod_x, 6, axis=-1)
        sh1c, sc1c, g1c, sh2c, sc2c, g2c = jnp.split(mod_c, 6, axis=-1)
        nx = modulated_norm(hx, norm_mix_w[l], sh1x, sc1x)
        nc = modulated_norm(hc, norm_mix_w[l], sh1c, sc1c)
        ox, oc = token_mixers(nx, nc, w_in[l], w_out[l], na_rpb[l], gla_gate_w[l], gla_gate_b[l], gla_norm_w[l],
                              gdn_conv_w[l], gdn_a_log[l], gdn_dt_bias[l], gdn_norm_w[l], row_pos, col_pos, ctx_out)
        hx = hx + g1x * ox
        hx = hx + g2x * expert_choice_ffn(modulated_norm(hx, norm_ffn_w[l], sh2x, sc2x), w_router[l],
                                          w_exp_gate[l], w_exp_up[l], w_exp_down[l])
        if ctx_out:
            hc = hc + g1c * oc
            hc = hc + g2c * expert_choice_ffn(modulated_norm(hc, norm_ffn_w[l], sh2c, sc2c), w_router[l],
                                              w_exp_gate[l], w_exp_up[l], w_exp_down[l])
    return rmsnorm(hx, final_norm_w)
```

```python
import numpy as np
from contextlib import ExitStack
import concourse.bass as bass
import concourse.mybir as mybir
from concourse.bass_utils import run_bass_kernel_spmd

F32 = mybir.dt.float32
BF16 = mybir.dt.bfloat16
I32 = mybir.dt.int32
U32 = mybir.dt.uint32
AF = mybir.ActivationFunctionType
ALU = mybir.AluOpType
AX = mybir.AxisListType

NCORES = 8
D = 2048
B = 2
S = 4096
LCTX = 256
DEPTH = 2
IN_W = 6704
EPS = 1e-6
NDMA = 24


class Buf:
    __slots__ = ("w", "r", "name", "excl")

    def __init__(self, name):
        self.w = None
        self.r = {}
        self.name = name
        self.excl = False


class View:
    __slots__ = ("t", "ap")

    def __init__(self, t, ap):
        self.t = t
        self.ap = ap


class Tile:
    def __init__(self, handle, name):
        self.h = handle
        self.buf = Buf(name)

    def __getitem__(self, idx):
        return View(self.buf, self.h[idx])

    def v(self, ap):
        return View(self.buf, ap)

    def sub(self, name, idx):
        t = Tile(self.h[idx], name)
        t.buf = self.buf
        return t


class Prog:
    def __init__(self):
        self.nc = bass.Bass("TRN2", target_bir_lowering=False)
        nc = self.nc
        self.es = ExitStack()
        self.engs = {"pe": nc.tensor, "act": nc.scalar, "dve": nc.vector, "pool": nc.gpsimd, "sp": nc.sync}
        self.esem = {e: self.es.enter_context(nc.semaphore("s_" + e)) for e in ("pe", "act", "dve", "pool")}
        self.ecnt = {e: 0 for e in self.esem}
        self.dsems = [self.es.enter_context(nc.semaphore("d%d" % i)) for i in range(NDMA)]
        self.dval = [0] * NDMA
        self.dnext = 0
        self.seen = {e: {} for e in self.engs}
        self.nins = 0
        self._psum = None

    def dram(self, name, shape, dt, kind):
        t = self.nc.dram_tensor(name, list(shape), dt, kind=kind)
        return Tile(t.ap(), name)

    def sb(self, name, shape, dt=F32):
        h = self.es.enter_context(self.nc.sbuf_tensor(name, list(shape), dt))
        return Tile(h, name)

    def ps(self, name, shape, dt=F32):
        h = self.es.enter_context(self.nc.psum_tensor(name, list(shape), dt))
        t = Tile(h, name)
        t.buf.excl = True
        return t

    def _wait(self, eng, evs):
        seen = self.seen[eng]
        best = {}
        for ev in evs:
            if ev is None:
                continue
            k, sem, val = ev
            if eng == "pe" and k == "e:pe":
                continue
            if seen.get(k, 0) >= val:
                continue
            if k not in best or best[k][2] < val:
                best[k] = ev
        for k, (kk, sem, val) in best.items():
            self.engs[eng].wait_ge(sem, val)
            seen[k] = val
            self.nins += 1

    @staticmethod
    def _deps(reads, writes):
        evs = []
        for b in reads:
            if b.w is not None:
                evs.append(b.w)
            if b.excl:
                evs.extend(b.r.values())
        for b in writes:
            if b.w is not None:
                evs.append(b.w)
            evs.extend(b.r.values())
        return evs

    @staticmethod
    def _mark(ev, reads, writes):
        k = ev[0]
        for b in reads:
            b.r[k] = ev
        for b in writes:
            b.w = ev
            b.r = {}

    def op(self, eng, fn, reads=(), writes=()):
        reads = [v.t if isinstance(v, View) else (v.buf if isinstance(v, Tile) else v) for v in reads]
        writes = [v.t if isinstance(v, View) else (v.buf if isinstance(v, Tile) else v) for v in writes]
        self._wait(eng, self._deps(reads, writes))
        ins = fn(self.engs[eng])
        self.ecnt[eng] += 1
        ins.then_inc(self.esem[eng], 1)
        ev = ("e:" + eng, self.esem[eng], self.ecnt[eng])
        self._mark(ev, reads, writes)
        self.nins += 1
        return ev

    def dma(self, q, out, in_, **kw):
        i = self.dnext
        self.dnext = (i + 1) % NDMA
        reads, writes = [in_.t], [out.t]
        evs = self._deps(reads, writes)
        if self.dval[i] > 0:
            evs.append(("d:%d" % i, self.dsems[i], self.dval[i]))
        self._wait(q, evs)
        ins = self.engs[q].dma_start(out=out.ap, in_=in_.ap, **kw)
        self.dval[i] += 16
        ins.then_inc(self.dsems[i], 16)
        ev = ("d:%d" % i, self.dsems[i], self.dval[i])
        self._mark(ev, reads, writes)
        self.nins += 1
        return ev

    def idma(self, out, out_off, in_, in_off, extra_reads=(), **kw):
        i = self.dnext
        self.dnext = (i + 1) % NDMA
        reads, writes = [in_.t] + [v.t for v in extra_reads], [out.t]
        evs = self._deps(reads, writes)
        if self.dval[i] > 0:
            evs.append(("d:%d" % i, self.dsems[i], self.dval[i]))
        self._wait("pool", evs)
        oo = bass.IndirectOffsetOnAxis(ap=out_off[0].ap, axis=out_off[1]) if out_off is not None else None
        io = bass.IndirectOffsetOnAxis(ap=in_off[0].ap, axis=in_off[1]) if in_off is not None else None
        ins = self.nc.gpsimd.indirect_dma_start(out=out.ap, out_offset=oo, in_=in_.ap, in_offset=io, **kw)
        self.dval[i] += 16
        ins.then_inc(self.dsems[i], 16)
        ev = ("d:%d" % i, self.dsems[i], self.dval[i])
        self._mark(ev, reads, writes)
        self.nins += 1
        return ev

    def finish(self):
        evs = [("d:%d" % i, self.dsems[i], self.dval[i]) for i in range(NDMA) if self.dval[i] > 0]
        evs += [("e:" + e, self.esem[e], self.ecnt[e]) for e in self.esem if self.ecnt[e] > 0]
        self._wait("sp", evs)
        self.es.close()
        return self.nc

    def matmul(self, out, lhsT, rhs, start=True, stop=True):
        return self.op("pe", lambda e: e.matmul(out.ap, lhsT.ap, rhs.ap, start=start, stop=stop),
                       reads=[lhsT, rhs], writes=[out])

    def transpose(self, out, in_, ident):
        return self.op("pe", lambda e: e.transpose(out.ap, in_.ap, ident.ap), reads=[in_, ident], writes=[out])

    def act(self, out, in_, func, bias=None, scale=None, accum=None, eng="act"):
        kw = {}
        reads = [in_]
        writes = [out]
        if bias is not None:
            if isinstance(bias, View):
                kw["bias"] = bias.ap
                reads.append(bias)
            else:
                kw["bias"] = bias
        if scale is not None:
            if isinstance(scale, View):
                kw["scale"] = scale.ap
                reads.append(scale)
            else:
                kw["scale"] = scale
        if accum is not None:
            kw["accum_out"] = accum.ap
            writes.append(accum)
        return self.op("act", lambda e: e.activation(out.ap, in_.ap, func, **kw), reads=reads, writes=writes)

    def copy(self, eng, out, in_):
        if eng == "act":
            return self.op("act", lambda e: e.copy(out.ap, in_.ap), reads=[in_], writes=[out])
        return self.op(eng, lambda e: e.tensor_copy(out.ap, in_.ap), reads=[in_], writes=[out])

    def tt(self, eng, out, a, b, op):
        return self.op(eng, lambda e: e.tensor_tensor(out.ap, a.ap, b.ap, op), reads=[a, b], writes=[out])

    def ts(self, eng, out, a, s1, op0, s2=None, op1=None, accum=None):
        reads = [a]
        writes = [out]
        s1a = s1.ap if isinstance(s1, View) else s1
        s2a = s2.ap if isinstance(s2, View) else s2
        if isinstance(s1, View):
            reads.append(s1)
        if isinstance(s2, View):
            reads.append(s2)
        kw = {}
        if op1 is not None:
            kw["op1"] = op1
        if accum is not None:
            kw["accum_out"] = accum.ap
            writes.append(accum)
        return self.op(eng, lambda e: e.tensor_scalar(out.ap, a.ap, s1a, s2a, op0, **kw), reads=reads, writes=writes)

    def stt(self, eng, out, a, s, b, op0, op1):
        reads = [a, b]
        sa = s.ap if isinstance(s, View) else s
        if isinstance(s, View):
            reads.append(s)
        return self.op(eng, lambda e: e.scalar_tensor_tensor(out.ap, a.ap, sa, b.ap, op0, op1), reads=reads, writes=[out])

    def recip(self, out, in_):
        return self.op("dve", lambda e: e.reciprocal(out.ap, in_.ap), reads=[in_], writes=[out])

    def memset(self, eng, out, val):
        return self.op(eng, lambda e: e.memset(out.ap, val), reads=[], writes=[out])


def run_prog(prog, in_maps):
    nc = prog.finish()
    res = run_bass_kernel_spmd(nc, in_maps, core_ids=list(range(NCORES)))
    return res.results


MODC = 6 * D // NCORES


def build_L0():
    P = Prog()
    cT = P.dram("cT", [128, 16, 3], F32, "ExternalInput")
    w = P.dram("w", [DEPTH, D, MODC], F32, "ExternalInput")
    bb = P.dram("b", [DEPTH, MODC], F32, "ExternalInput")
    out = P.dram("out", [DEPTH, 3, MODC], F32, "ExternalOutput")
    c_sb = P.sb("c_sb", [128, 16, 3])
    sc = P.sb("sc", [128, 16, 3])
    P.dma("sp", c_sb[:], cT[:])
    P.act(sc[:], c_sb[:], AF.Silu)
    wt = [P.sb("wt%d" % i, [128, 16, 512]) for i in range(2)]
    bt = [P.sb("bt%d" % i, [3, 512]) for i in range(2)]
    ot = [P.sb("ot%d" % i, [3, 512]) for i in range(2)]
    pt = [P.ps("pt%d" % i, [128, 512]) for i in range(2)]
    it = 0
    for l in range(DEPTH):
        wv = w.h[l].rearrange("(k p) n -> p k n", p=128)
        for j in range(MODC // 512):
            s = it % 2
            P.dma("sp", wt[s][:], w.v(wv[:, :, j * 512:(j + 1) * 512]))
            for r in range(3):
                P.dma("sp", bt[s][r:r + 1, :], bb.v(bb.h[l:l + 1, j * 512:(j + 1) * 512]))
            for k in range(16):
                P.matmul(pt[s][0:3, :], sc[:, k, :], wt[s][:, k, :], start=(k == 0), stop=(k == 15))
            P.tt("dve", ot[s][:], pt[s][0:3, :], bt[s][:], ALU.add)
            P.dma("sp", out.v(out.h[l, :, j * 512:(j + 1) * 512]), ot[s][:])
            it += 1
    return P


def run_L0(inp):
    c_all = np.concatenate([inp["c"], inp["c_ctx"][None, :]], axis=0).astype(np.float32)
    cT = np.ascontiguousarray(c_all.reshape(3, 16, 128).transpose(2, 1, 0))
    P = build_L0()
    maps = []
    for i in range(NCORES):
        maps.append({
            "cT": cT,
            "w": np.ascontiguousarray(inp["w_ada"][:, :, i * MODC:(i + 1) * MODC]),
            "b": np.ascontiguousarray(inp["b_ada"][:, i * MODC:(i + 1) * MODC]),
        })
    res = run_prog(P, maps)
    return np.concatenate([r["out"] for r in res], axis=2)


def _barrier(P):
    evs = [("d:%d" % i, P.dsems[i], P.dval[i]) for i in range(NDMA) if P.dval[i] > 0]
    evs += [("e:" + e, P.esem[e], P.ecnt[e]) for e in P.esem if P.ecnt[e] > 0]
    for e in P.engs:
        P._wait(e, evs)


Prog.barrier = _barrier

MMDT = BF16
NTOK1 = 1024 + 64


def rms_modulate(P, x_t, rows, A, Bt, y_t, ss, rstd):
    P.memset("dve", ss[0:rows, :], 0.0)
    P.act(y_t[0:rows, :], x_t[0:rows, :], AF.Square, accum=ss[0:rows, :])
    P.ts("dve", rstd[0:rows, :], ss[0:rows, :], 1.0 / D, ALU.mult, EPS, ALU.add)
    P.act(rstd[0:rows, :], rstd[0:rows, :], AF.Sqrt)
    P.recip(rstd[0:rows, :], rstd[0:rows, :])
    P.stt("dve", y_t[0:rows, :], x_t[0:rows, :], rstd[0:rows, 0:1], A[0:rows, :], ALU.mult, ALU.mult)
    if Bt is not None:
        P.tt("dve", y_t[0:rows, :], y_t[0:rows, :], Bt[0:rows, :], ALU.add)


def load_mod_vectors(P, vec, A, Bt):
    tmp = P.sb("tmpv_" + A.buf.name, [128, D])
    P.dma("sp", A[:], vec.v(vec.h[0:1, :].to_broadcast([128, D])))
    P.dma("sp", tmp[:], vec.v(vec.h[1:2, :].to_broadcast([128, D])))
    P.dma("sp", Bt[:], vec.v(vec.h[2:3, :].to_broadcast([128, D])))
    P.stt("dve", A[:], tmp[:], 1.0, A[:], ALU.add, ALU.mult)


def transpose_to_fm(P, y_t, rows, dstT, tok0, ident, pst, ev_i):
    for g in range(4):
        pt = pst[(ev_i + g) % len(pst)]
        for j in range(4):
            k = g * 4 + j
            P.transpose(pt[:, j * 128:j * 128 + rows], y_t[0:rows, k * 128:(k + 1) * 128], ident[0:rows, 0:rows])
        src = pt.v(pt.h[:, 0:512].rearrange("p (j t) -> p j t", j=4)[:, :, 0:rows])
        dst = dstT[:, g * 4:(g + 1) * 4, tok0:tok0 + rows]
        P.copy("act" if g % 2 == 0 else "dve", dst, src)


def build_L1():
    P = Prog()
    h = P.dram("h", [NTOK1, D], F32, "ExternalInput")
    vlat = P.dram("vlat", [3, D], F32, "ExternalInput")
    vctx = P.dram("vctx", [3, D], F32, "ExternalInput")
    w = P.dram("w", [D, IN_W], F32, "ExternalInput")
    idn = P.dram("ident", [128, 128], F32, "ExternalInput")
    p = P.dram("p", [NTOK1, IN_W], F32, "ExternalOutput")
    ident = P.sb("ident_sb", [128, 128])
    P.dma("sp", ident[:], idn[:])
    A_l, B_l, A_c, B_c = (P.sb(n, [128, D]) for n in ("A_l", "B_l", "A_c", "B_c"))
    load_mod_vectors(P, vlat, A_l, B_l)
    load_mod_vectors(P, vctx, A_c, B_c)
    nxT = P.sb("nxT", [128, 16, NTOK1], MMDT)
    xt = [P.sb("xt%d" % i, [128, D]) for i in range(2)]
    yt = P.sb("yt", [128, D])
    ss = P.sb("ss", [128, 1])
    rstd = P.sb("rstd", [128, 1])
    pst = [P.ps("ps%d" % i, [128, 512]) for i in range(8)]
    tiles = [(i * 128, 128) for i in range(8)] + [(1024, 64)]
    for ti, (t0, rows) in enumerate(tiles):
        x_t = xt[ti % 2]
        P.dma("sp", x_t[0:rows, :], h[t0:t0 + rows, :])
        lat = ti < 8
        rms_modulate(P, x_t, rows, A_l if lat else A_c, B_l if lat else B_c, yt, ss, rstd)
        transpose_to_fm(P, yt, rows, nxT, t0, ident, pst[0:4], ti * 4)
    wv = w.h.rearrange("(k p) n -> p k n", p=128)
    wb = [P.sb("wb%d" % i, [128, 16, 512], MMDT) for i in range(2)]
    ob = [P.sb("ob%d" % i, [128, 512]) for i in range(4)]
    nblk = (IN_W + 511) // 512
    oi = 0
    for nb in range(nblk):
        c0 = nb * 512
        cw = min(512, IN_W - c0)
        wt = wb[nb % 2]
        for kk in range(0, 16, 4):
            P.dma("pool", wt[:, kk:kk + 4, 0:cw], w.v(wv[:, kk:kk + 4, c0:c0 + cw]))
        for ti, (t0, rows) in enumerate(tiles):
            pt = pst[4 + oi % 4]
            for k in range(16):
                P.matmul(pt[0:rows, 0:cw], nxT[:, k, t0:t0 + rows], wt[:, k, 0:cw], start=(k == 0), stop=(k == 15))
            o = ob[oi % 4]
            P.copy("act" if oi % 2 == 0 else "dve", o[0:rows, 0:cw], pt[0:rows, 0:cw])
            P.dma("sp", p[t0:t0 + rows, c0:c0 + cw], o[0:rows, 0:cw])
            oi += 1
    return P


def tok_shard(lat, ctx, i):
    b, q = i // 4, i % 4
    return np.concatenate([lat[b, q * 1024:(q + 1) * 1024], ctx[b, q * 64:(q + 1) * 64]], axis=0)


def tok_unshard(parts, width):
    lat = np.zeros((B, S, width), np.float32)
    ctx = np.zeros((B, LCTX, width), np.float32)
    for i, pp in enumerate(parts):
        b, q = i // 4, i % 4
        lat[b, q * 1024:(q + 1) * 1024] = pp[:1024]
        ctx[b, q * 64:(q + 1) * 64] = pp[1024:]
    return lat, ctx


def run_L1(hx, hc, mod_l, norm_w, w_in):
    P = build_L1()
    ident = np.eye(128, dtype=np.float32)
    maps = []
    for i in range(NCORES):
        b = i // 4
        vlat = np.stack([norm_w, mod_l[b, D:2 * D], mod_l[b, 0:D]])
        vctx = np.stack([norm_w, mod_l[2, D:2 * D], mod_l[2, 0:D]])
        maps.append({"h": np.ascontiguousarray(tok_shard(hx, hc, i)), "vlat": vlat, "vctx": vctx,
                     "w": w_in, "ident": ident})
    res = run_prog(P, maps)
    return tok_unshard([r["p"] for r in res], IN_W)


NEG = -30000.0
NA_DT = F32


def na_row_class(r):
    return r if r < 4 else (4 if r < 60 else r - 55)


def build_NA(with_ctx):
    P = Prog()
    NH = 4
    TT = S + LCTX
    qT_d = P.dram("qT", [NH, 64, TT], F32, "ExternalInput")
    kT_d = P.dram("kT", [NH, 64, TT], F32, "ExternalInput")
    v_d = P.dram("v", [NH, 64, 68, 64], F32, "ExternalInput")
    bias_d = P.dram("bias", [NH, 64, 9, 512], F32, "ExternalInput")
    idn = P.dram("ident", [128, 128], F32, "ExternalInput")
    o_d = P.dram("o", [NH, 64, 68, 64], F32, "ExternalOutput")
    ident = P.sb("ident_sb", [128, 128])
    P.dma("sp", ident[:], idn[:])
    qT = [P.sb("qT%d" % i, [64, TT], NA_DT) for i in range(2)]
    kT = [P.sb("kT%d" % i, [64, TT], NA_DT) for i in range(2)]
    V = [P.sb("V%d" % i, [64, 68, 64], NA_DT) for i in range(2)]
    bias = [P.sb("bias%d" % i, [64, 9, 512]) for i in range(2)]
    O = [P.sb("O%d" % i, [64, 68, 64]) for i in range(2)]
    Sb = [P.sb("Sb%d" % i, [64, 768]) for i in range(2)]
    Pm = [P.sb("Pm%d" % i, [64, 768], NA_DT) for i in range(2)]
    PT = [P.sb("PT%d" % i, [64, 12, 64], NA_DT) for i in range(2)]
    st = [P.sb("st%d" % i, [64, 4]) for i in range(2)]
    s_loc = [P.ps("s_loc%d" % i, [64, 512]) for i in range(2)]
    bC = P.ps("bankC", [64, 512])
    s_ctx = [bC.sub("s_ctx%d" % i, (slice(None), slice(0, 256))) for i in range(2)]
    pt_a = [P.ps("pt_a%d" % i, [64, 512], NA_DT) for i in range(2)]
    pt_b = [P.ps("pt_b%d" % i, [64, 512], NA_DT) for i in range(2)]
    pt_b = [t.sub("pt_bs%d" % i, (slice(None), slice(0, 256))) for i, t in enumerate(pt_b)]
    bG = P.ps("bankG", [64, 512])
    o_ps = [bG.sub("o_ps%d" % i, (slice(None), slice(0, 64))) for i in range(2)]
    it = 0
    for hh in range(NH):
        s = hh % 2
        dq = "pool" if NA_DT != F32 else "sp"
        P.dma(dq, qT[s][:], qT_d.v(qT_d.h[hh]))
        P.dma(dq, kT[s][:], kT_d.v(kT_d.h[hh]))
        P.dma(dq, V[s][:], v_d.v(v_d.h[hh]))
        P.dma("sp", bias[s][:], bias_d.v(bias_d.h[hh]))
        nrows = 68 if with_ctx else 64
        if not with_ctx:
            P.memset("pool", O[s][:, 64:68, :], 0.0)
        for r in range(nrows):
            u = it % 2
            it += 1
            local = r < 64
            q_ap = qT[s][:, r * 64:(r + 1) * 64]
            nk = 12 if local else 4
            wid = nk * 64
            if local:
                r0 = min(max(r - 4, 0), 56)
                P.matmul(s_loc[u][:, :], q_ap, kT[s][:, r0 * 64:r0 * 64 + 512])
                P.matmul(s_ctx[u][:, :], q_ap, kT[s][:, S:S + 256])
                P.stt("dve", Sb[u][:, 0:512], s_loc[u][:, :], 0.125, bias[s][:, na_row_class(r), :], ALU.mult, ALU.add)
                P.act(Sb[u][:, 512:768], s_ctx[u][:, :], AF.Identity, scale=0.125)
            else:
                P.matmul(s_ctx[u][:, :], q_ap, kT[s][:, S:S + 256])
                P.act(Sb[u][:, 0:256], s_ctx[u][:, :], AF.Identity, scale=0.125)
            mx, nmx, sm, rs = (st[u][:, j:j + 1] for j in range(4))
            P.op("dve", lambda e, a=mx.ap, b_=Sb[u][:, 0:wid].ap: e.reduce_max(a, b_, AX.X), reads=[Sb[u]], writes=[st[u]])
            P.ts("dve", nmx, mx, -1.0, ALU.mult)
            P.memset("dve", sm, 0.0)
            P.act(Pm[u][:, 0:wid], Sb[u][:, 0:wid], AF.Exp, bias=nmx, accum=sm)
            P.recip(rs, sm)
            for j in range(nk):
                dst = pt_a[u][:, j * 64:(j + 1) * 64] if j < 8 else pt_b[u][:, (j - 8) * 64:(j - 7) * 64]
                P.transpose(dst, Pm[u][:, j * 64:(j + 1) * 64], ident[0:64, 0:64])
            if local:
                P.copy("dve", PT[u][:, 0:8, :], pt_a[u].v(pt_a[u].h[:, :].rearrange("p (j q) -> p j q", j=8)))
                P.copy("act", PT[u][:, 8:12, :], pt_b[u].v(pt_b[u].h[:, :].rearrange("p (j q) -> p j q", j=4)))
            else:
                P.copy("act", PT[u][:, 0:4, :], pt_a[u].v(pt_a[u].h[:, 0:256].rearrange("p (j q) -> p j q", j=4)))
            for j in range(nk):
                if local:
                    vr = (r0 + j) if j < 8 else (64 + j - 8)
                else:
                    vr = 64 + j
                P.matmul(o_ps[u][:, :], PT[u][:, j, :], V[s][:, vr, :], start=(j == 0), stop=(j == nk - 1))
            P.ts("dve", O[s][:, r, :], o_ps[u][:, :], rs, ALU.mult)
        P.dma("sp", o_d.v(o_d.h[hh]), O[s][:])
    return P


def na_bias_tables(rpb_l):
    cq = np.arange(64)
    cstart = np.clip(cq - 8, 0, 48)
    colmask = (cq[None, :] >= cstart[:, None]) & (cq[None, :] < cstart[:, None] + 16)
    dc = np.clip(cq[None, :] - cq[:, None] + 15, 0, 30)
    out = np.full((16, 64, 9, 8, 64), NEG, np.float32)
    for rc in range(9):
        r = rc if rc < 4 else (4 if rc == 4 else rc + 55)
        ridx = min(max(r - 4, 0), 56) + np.arange(8)
        dr = ridx - r + 7
        g = rpb_l[:, dr[:, None, None], dc[None, :, :]]
        g = np.where(colmask[None, None], g, NEG)
        out[:, :, rc] = g.transpose(0, 2, 1, 3)
    return out.reshape(16, 64, 9, 512)


def run_NA(px, pc, rpb_l, with_ctx):
    P = build_NA(with_ctx)
    ident = np.eye(128, dtype=np.float32)
    bias_all = na_bias_tables(rpb_l)
    maps = []
    for i in range(NCORES):
        b, h0 = i // 4, 4 * (i % 4)
        full = np.concatenate([px[b, :, 0:3072], pc[b, :, 0:3072]], axis=0)
        q = full[:, 0:1024].reshape(S + LCTX, 16, 64)[:, h0:h0 + 4]
        k = full[:, 1024:2048].reshape(S + LCTX, 16, 64)[:, h0:h0 + 4]
        v = full[:, 2048:3072].reshape(68, 64, 16, 64)[:, :, h0:h0 + 4]
        maps.append({
            "qT": np.ascontiguousarray(q.transpose(1, 2, 0)),
            "kT": np.ascontiguousarray(k.transpose(1, 2, 0)),
            "v": np.ascontiguousarray(v.transpose(2, 1, 0, 3)),
            "bias": np.ascontiguousarray(bias_all[h0:h0 + 4]),
            "ident": ident,
        })
    res = run_prog(P, maps)
    ox = np.zeros((B, S, 1024), np.float32)
    oc = np.zeros((B, LCTX, 1024), np.float32)
    for i, r in enumerate(res):
        b, h0 = i // 4, 4 * (i % 4)
        o = r["o"].transpose(2, 1, 0, 3).reshape(S + LCTX, 4, 64)
        ox[b, :, h0 * 64:(h0 + 4) * 64] = o[:S].reshape(S, 256)
        oc[b, :, h0 * 64:(h0 + 4) * 64] = o[S:].reshape(LCTX, 256)
    return ox, oc


TT = S + LCTX


def build_GLA(with_ctx):
    P = Prog()
    qT_d = P.dram("qT", [64, TT], F32, "ExternalInput")
    kT_d = P.dram("kT", [64, TT], F32, "ExternalInput")
    v_d = P.dram("v", [64, 68, 128], F32, "ExternalInput")
    g_d = P.dram("g", [64, 68, 128], F32, "ExternalInput")
    lr_d = P.dram("lr", [2, 16, TT], F32, "ExternalInput")
    gw_d = P.dram("gw", [2, 17, 64], F32, "ExternalInput")
    ct_d = P.dram("ct", [64, S], F32, "ExternalInput")
    st_d = P.dram("st", [64, S], F32, "ExternalInput")
    psw_d = P.dram("psw", [64, 64], F32, "ExternalInput")
    tri_d = P.dram("tri", [2, 64, 512], F32, "ExternalInput")
    nw_d = P.dram("nw", [64, 512], F32, "ExternalInput")
    idn = P.dram("ident", [128, 128], F32, "ExternalInput")
    o_d = P.dram("o", [64, 68, 128], F32, "ExternalOutput")

    ident = P.sb("ident_sb", [128, 128])
    P.dma("sp", ident[:], idn[:])
    psw = P.sb("psw_sb", [64, 64])
    P.dma("sp", psw[:], psw_d[:])
    tri = [P.sb("tri%d" % i, [64, 512]) for i in range(2)]
    gw = [P.sb("gw%d" % i, [17, 64]) for i in range(2)]
    for i in range(2):
        P.dma("sp", tri[i][:], tri_d.v(tri_d.h[i]))
        P.dma("sp", gw[i][:], gw_d.v(gw_d.h[i]))
    nw4 = P.sb("nw4", [64, 512])
    P.dma("sp", nw4[:], nw_d[:])
    ob_store = P.sb("ob_store", [64, 68, 128])

    NB = 2
    qg = [P.sb("qg%d" % i, [64, 512]) for i in range(NB)]
    kg = [P.sb("kg%d" % i, [64, 512]) for i in range(NB)]
    vg = [P.sb("vg%d" % i, [64, 8, 128]) for i in range(NB)]
    gg = [P.sb("gg%d" % i, [64, 8, 128]) for i in range(NB)]
    lra = [P.sb("lra%d" % i, [17, 512]) for i in range(NB)]
    ctg = [P.sb("ctg%d" % i, [64, 512]) for i in range(NB)]
    stg = [P.sb("stg%d" % i, [64, 512]) for i in range(NB)]
    tmp = P.sb("tmp", [64, 512])
    e_sb = P.sb("e_sb", [64, 512])
    l_sb = P.sb("l_sb", [64, 512])
    Eq = P.sb("Eq", [64, 512])
    Ek = P.sb("Ek", [64, 512])
    qe = [P.sb("qe%d" % i, [64, 512]) for i in range(NB)]
    keT = P.sb("keT", [64, 512])
    ke = [P.sb("ke%d" % i, [64, 512]) for i in range(NB)]
    attT = [P.sb("attT%d" % i, [64, 512]) for i in range(NB)]
    glast = [P.sb("glast%d" % i, [64, 8]) for i in range(NB)]
    mgt = [P.sb("mgt%d" % i, [64, 128]) for i in range(2)]
    Sb_ = [P.sb("S%d" % i, [64, 128]) for i in range(4)]
    osum = [P.sb("osum%d" % i, [64, 4, 128]) for i in range(2)]
    sq = P.sb("sq", [64, 4, 128])
    on = [P.sb("on%d" % i, [64, 4, 128]) for i in range(2)]
    sg = P.sb("sg", [64, 4, 128])
    ss4 = P.sb("ss4", [64, 4])
    zps = P.ps("zps", [64, 512])
    cps = P.ps("cps", [64, 512])
    aps = P.ps("aps", [64, 512])
    kps = P.ps("kps", [64, 512])
    mps = [P.ps("mps%d" % i, [64, 512]) for i in range(2)]
    ops = [P.ps("ops%d" % i, [64, 512]) for i in range(2)]

    cnt = {"g": 0, "s": 0, "m": 0, "o": 0}

    def group(dr, tok0, ntok, rope, final):
        nch = ntok // 64
        u = cnt["g"] % NB
        cnt["g"] += 1
        c0 = tok0 // 64
        q_, k_, v_, l_ = qg[u], kg[u], vg[u], lra[u]
        P.dma("sp", q_[:, 0:ntok], qT_d[:, tok0:tok0 + ntok])
        P.dma("sp", k_[:, 0:ntok], kT_d[:, tok0:tok0 + ntok])
        P.dma("sp", v_[:, 0:nch, :], v_d[:, c0:c0 + nch, :])
        P.memset("pool", l_[:, :], 1.0)
        P.dma("sp", l_[0:16, 0:ntok], lr_d.v(lr_d.h[dr, :, tok0:tok0 + ntok]))
        if final:
            P.dma("sp", gg[u][:, 0:nch, :], g_d[:, c0:c0 + nch, :])
        if rope:
            P.dma("sp", ctg[u][:, 0:ntok], ct_d[:, tok0:tok0 + ntok])
            P.dma("sp", stg[u][:, 0:ntok], st_d[:, tok0:tok0 + ntok])
            for x in (q_, k_):
                P.matmul(zps[:, 0:ntok], psw[:, :], x[:, 0:ntok])
                P.tt("dve", tmp[:, 0:ntok], zps[:, 0:ntok], stg[u][:, 0:ntok], ALU.mult)
                P.tt("pool", x[:, 0:ntok], x[:, 0:ntok], ctg[u][:, 0:ntok], ALU.mult)
                P.tt("dve", x[:, 0:ntok], x[:, 0:ntok], tmp[:, 0:ntok], ALU.add)
        for j in range(nch):
            P.matmul(zps[:, j * 64:(j + 1) * 64], l_[:, j * 64:(j + 1) * 64], gw[dr][:, :])
        P.act(e_sb[:, 0:ntok], zps[:, 0:ntok], AF.Exp, scale=-1.0)
        P.act(l_sb[:, 0:ntok], e_sb[:, 0:ntok], AF.Ln, bias=1.0)
        for j in range(nch):
            P.matmul(cps[:, j * 64:(j + 1) * 64], l_sb[:, j * 64:(j + 1) * 64], tri[dr][:, 0:64])
        P.act(Eq[:, 0:ntok], cps[:, 0:ntok], AF.Exp, scale=-1.0 / 16.0)
        P.act(Ek[:, 0:ntok], cps[:, 0:ntok], AF.Exp, scale=1.0 / 16.0)
        P.stt("dve", qe[u][:, 0:ntok], q_[:, 0:ntok], 0.125, Eq[:, 0:ntok], ALU.mult, ALU.mult)
        P.tt("dve", keT[:, 0:ntok], k_[:, 0:ntok], Ek[:, 0:ntok], ALU.mult)
        lastcol = 63 if dr == 0 else 0
        P.copy("dve", glast[u][:, 0:nch], Eq.v(Eq.h[:, 0:ntok].rearrange("p (c t) -> p c t", t=64)[:, :, lastcol]))
        for j in range(nch):
            P.matmul(aps[:, j * 64:(j + 1) * 64], keT[:, j * 64:(j + 1) * 64], qe[u][:, j * 64:(j + 1) * 64])
        P.tt("dve", attT[u][:, 0:ntok], aps[:, 0:ntok], tri[dr][:, 0:ntok], ALU.mult)
        for j in range(nch):
            P.transpose(kps[:, j * 64:(j + 1) * 64], keT[:, j * 64:(j + 1) * 64], ident[0:64, 0:64])
        P.copy("act", ke[u][:, 0:ntok], kps[:, 0:ntok])
        for j in range(nch):
            P.matmul(mps[j // 4][:, (j % 4) * 128:(j % 4 + 1) * 128], ke[u][:, j * 64:(j + 1) * 64], v_[:, j, :])
        need_out = with_ctx or tok0 < S
        js = list(range(nch)) if dr == 0 else list(range(nch - 1, -1, -1))
        for n, j in enumerate(js):
            col = (j % 4) * 128
            S_cur = Sb_[cnt["s"] % 4]
            S_nxt = Sb_[(cnt["s"] + 1) % 4]
            cnt["s"] += 1
            if need_out:
                ob = ops[j // 4]
                P.matmul(ob[:, col:col + 128], qe[u][:, j * 64:(j + 1) * 64], S_cur[:, :], start=True, stop=False)
                P.matmul(ob[:, col:col + 128], attT[u][:, j * 64:(j + 1) * 64], v_[:, j, :], start=False, stop=True)
            mg = mgt[cnt["m"] % 2]
            cnt["m"] += 1
            P.ts("dve", mg[:, :], mps[j // 4][:, col:col + 128], glast[u][:, j:j + 1], ALU.mult)
            P.stt("dve", S_nxt[:, :], S_cur[:, :], glast[u][:, j:j + 1], mg[:, :], ALU.mult, ALU.add)
            done_bank = (j % 4 == 3) if dr == 0 else (j % 4 == 0)
            if need_out and done_bank:
                q4 = j // 4
                cb = c0 + 4 * q4
                ob = ops[q4]
                ob3 = ob.v(ob.h[:, :].rearrange("p (c d) -> p c d", c=4))
                if not final:
                    P.copy("act", ob_store[:, cb:cb + 4, :], ob3)
                else:
                    w_ = cnt["o"] % 2
                    cnt["o"] += 1
                    P.tt("dve", osum[w_][:, :, :], ob3, ob_store[:, cb:cb + 4, :], ALU.add)
                    P.act(sq[:, :, :], osum[w_][:, :, :], AF.Square)
                    P.op("dve", lambda e, a=ss4[:, :].ap, b_=sq[:, :, :].ap: e.tensor_reduce(a, b_, AX.X, ALU.add),
                         reads=[sq], writes=[ss4])
                    P.ts("dve", ss4[:, :], ss4[:, :], 1.0 / 128.0, ALU.mult, EPS, ALU.add)
                    P.act(ss4[:, :], ss4[:, :], AF.Sqrt)
                    P.recip(ss4[:, :], ss4[:, :])
                    for jj in range(4):
                        P.ts("dve", on[w_][:, jj, :], osum[w_][:, jj, :], ss4[:, jj:jj + 1], ALU.mult)
                    P.tt("pool", on[w_][:, :, :], on[w_][:, :, :], nw4.v(nw4.h[:, :].rearrange("p (c d) -> p c d", c=4)), ALU.mult)
                    P.act(sg[:, :, :], gg[u][:, 4 * q4:4 * q4 + 4, :], AF.Silu)
                    P.tt("dve", on[w_][:, :, :], on[w_][:, :, :], sg[:, :, :], ALU.mult)
                    P.dma("sp", o_d[:, cb:cb + 4, :], on[w_][:, :, :])

    for dr, final in ((1, False), (0, True)):
        P.memset("dve", Sb_[cnt["s"] % 4][:, :], 0.0)
        group(dr, S, LCTX, False, final)
        gl = list(range(8)) if dr == 0 else list(range(7, -1, -1))
        for g_ in gl:
            group(dr, g_ * 512, 512, True, final)
    if not with_ctx:
        P.memset("dve", on[0][:, :, :], 0.0)
        P.dma("sp", o_d[:, 64:68, :], on[0][:, :, :])
    return P


def rope_tables():
    nf = 16
    freqs = (10000.0 ** (-np.arange(nf, dtype=np.float32) / nf)).astype(np.float32)
    pos = np.arange(S)
    rp, cp = (pos // 64).astype(np.float32), (pos % 64).astype(np.float32)
    ct = np.zeros((64, S), np.float32)
    st = np.zeros((64, S), np.float32)
    psw = np.zeros((64, 64), np.float32)
    for d in range(64):
        half, i = d // 32, d % 32
        f = i % 16
        ang = ((rp if half == 0 else cp) * freqs[f]).astype(np.float32)
        ct[d] = np.cos(ang)
        st[d] = -np.sin(ang) if i < 16 else np.sin(ang)
        partner = d + 16 if i < 16 else d - 16
        psw[partner, d] = 1.0
    return ct, st, psw


def tri_masks():
    a = np.arange(64)
    f = (a[:, None] <= a[None, :]).astype(np.float32)
    bk = (a[:, None] >= a[None, :]).astype(np.float32)
    return np.stack([np.tile(f, (1, 8)), np.tile(bk, (1, 8))])


def chunk_tm(a):
    return np.ascontiguousarray(a.reshape(68, 64, a.shape[-1]).transpose(1, 0, 2))


def unchunk_tm(a):
    return a.transpose(1, 0, 2).reshape(68 * 64, a.shape[-1])


def run_GLA(px, pc, gate_w, gate_b, norm_w, with_ctx):
    P = build_GLA(with_ctx)
    ident = np.eye(128, dtype=np.float32)
    ct, st, psw = rope_tables()
    tri = tri_masks()
    nw = np.ascontiguousarray(np.tile(norm_w[None, :], (64, 4)))
    maps = []
    o0 = 3072
    for i in range(NCORES):
        b, h = i // 4, i % 4
        full = np.concatenate([px[b], pc[b]], axis=0)
        q = full[:, o0 + h * 64:o0 + (h + 1) * 64]
        k = full[:, o0 + 256 + h * 64:o0 + 256 + (h + 1) * 64]
        v = full[:, o0 + 512 + h * 128:o0 + 512 + (h + 1) * 128]
        g = full[:, o0 + 1024 + h * 128:o0 + 1024 + (h + 1) * 128]
        lr = full[:, o0 + 1536:o0 + 1568].reshape(TT, 2, 16)
        gw = np.stack([np.concatenate([gate_w[d][:, h * 64:(h + 1) * 64], gate_b[d][None, h * 64:(h + 1) * 64]], 0) for d in range(2)])
        maps.append({"qT": np.ascontiguousarray(q.T), "kT": np.ascontiguousarray(k.T), "v": chunk_tm(v), "g": chunk_tm(g),
                     "lr": np.ascontiguousarray(lr.transpose(1, 2, 0)), "gw": gw, "ct": ct, "st": st, "psw": psw,
                     "tri": tri, "nw": nw, "ident": ident})
    res = run_prog(P, maps)
    ox = np.zeros((B, S, 512), np.float32)
    oc = np.zeros((B, LCTX, 512), np.float32)
    for i, r in enumerate(res):
        b, h = i // 4, i % 4
        o = unchunk_tm(r["o"])
        ox[b, :, h * 128:(h + 1) * 128] = o[:S]
        oc[b, :, h * 128:(h + 1) * 128] = o[S:]
    return ox, oc


def build_GDN(with_ctx):
    P = Prog()
    raw_d = P.dram("raw", [3, 128, TT], F32, "ExternalInput")
    cw_d = P.dram("cw", [3, 128, 5], F32, "ExternalInput")
    z_d = P.dram("z", [64, 68, 128], F32, "ExternalInput")
    a_d = P.dram("a", [2, 64, 68], F32, "ExternalInput")
    b_d = P.dram("bb", [2, 64, 68], F32, "ExternalInput")
    al_d = P.dram("alog", [64, 2], F32, "ExternalInput")
    dtb_d = P.dram("dtb", [64, 2], F32, "ExternalInput")
    ctri_d = P.dram("ctri", [2, 64, 64], F32, "ExternalInput")
    negm_d = P.dram("negm", [2, 64, 256], F32, "ExternalInput")
    smask_d = P.dram("smask", [2, 64, 256], F32, "ExternalInput")
    nw_d = P.dram("nw", [64, 512], F32, "ExternalInput")
    idn = P.dram("ident", [128, 128], F32, "ExternalInput")
    o_d = P.dram("o", [64, 68, 128], F32, "ExternalOutput")

    ident = P.sb("ident_sb", [128, 128])
    P.dma("sp", ident[:], idn[:])
    ident4 = P.sb("ident4", [64, 256])
    for j in range(4):
        P.dma("sp", ident4[:, j * 64:(j + 1) * 64], idn[0:64, 0:64])
    ones = P.sb("ones", [128, 128])
    P.memset("dve", ones[:], 1.0)
    negones = P.sb("negones", [64, 64])
    P.memset("dve", negones[:], -1.0)
    ctri = [P.sb("ctri%d" % i, [64, 64]) for i in range(2)]
    trione = [P.sb("trione%d" % i, [64, 192]) for i in range(2)]
    negm = [P.sb("negm%d" % i, [64, 256]) for i in range(2)]
    smask = [P.sb("smask%d" % i, [64, 256]) for i in range(2)]
    for i in range(2):
        P.dma("sp", ctri[i][:], ctri_d.v(ctri_d.h[i]))
        P.dma("sp", trione[i][:, 0:64], ctri_d.v(ctri_d.h[i]))
        P.memset("dve", trione[i][:, 64:192], 1.0)
        P.dma("sp", negm[i][:], negm_d.v(negm_d.h[i]))
        P.dma("sp", smask[i][:], smask_d.v(smask_d.h[i]))
    nw4 = P.sb("nw4", [64, 512])
    P.dma("sp", nw4[:], nw_d[:])
    cw = P.sb("cw_sb", [128, 3, 5])
    for i in range(3):
        P.dma("sp", cw[:, i, :], cw_d.v(cw_d.h[i]))

    xc = [P.sb("xc%d" % i, [128, TT]) for i in range(3)]
    upad = P.sb("upad", [128, S + 4])
    upc = P.sb("upc", [128, LCTX + 4])
    P.memset("pool", upad[:], 0.0)
    P.memset("pool", upc[:], 0.0)
    for i in range(3):
        P.dma("sp", upad[:, 2:2 + S], raw_d.v(raw_d.h[i, :, 0:S]))
        P.dma("sp", upc[:, 2:2 + LCTX], raw_d.v(raw_d.h[i, :, S:TT]))
        for (src, off, n) in ((upad, 0, S), (upc, S, LCTX)):
            y = xc[i][:, off:off + n]
            P.ts("dve", y, src[:, 0:n], cw[:, i, 0:1], ALU.mult)
            for j in range(1, 5):
                P.stt("dve", y, src[:, j:j + n], cw[:, i, j:j + 1], y, ALU.mult, ALU.add)
            P.act(y, y, AF.Silu)

    al = P.sb("al_sb", [64, 2])
    dtb = P.sb("dtb_sb", [64, 2])
    P.dma("sp", al[:], al_d[:])
    P.dma("sp", dtb[:], dtb_d[:])
    nea = P.sb("nea", [64, 2])
    P.act(nea[:], al[:], AF.Exp)
    P.ts("dve", nea[:], nea[:], -1.0, ALU.mult)
    g_all = [P.sb("g_all%d" % i, [64, 68]) for i in range(2)]
    beta = [P.sb("beta%d" % i, [64, 68]) for i in range(2)]
    nbeta = [P.sb("nbeta%d" % i, [64, 68]) for i in range(2)]
    tmpa = P.sb("tmpa", [64, 68])
    for d_ in range(2):
        P.dma("sp", tmpa[:], a_d.v(a_d.h[d_]))
        P.act(tmpa[:], tmpa[:], AF.Exp, bias=dtb[:, d_:d_ + 1])
        P.act(tmpa[:], tmpa[:], AF.Ln, bias=1.0)
        P.ts("dve", g_all[d_][:], tmpa[:], nea[:, d_:d_ + 1], ALU.mult)
        P.dma("sp", beta[d_][:], b_d.v(b_d.h[d_]))
        P.act(beta[d_][:], beta[d_][:], AF.Sigmoid)
        P.ts("dve", nbeta[d_][:], beta[d_][:], -1.0, ALU.mult)

    import os
    STOP = int(os.environ.get("GDN_STOP", "99"))
    if STOP == 0:
        return P
    ob_store = P.sb("ob_store", [64, 68, 128])
    def t64(n):
        return P.sb(n, [64, 256])
    sqt = P.sb("sqt", [128, 256])
    rn = P.sb("rn", [128, 256])
    qnT = P.sb("qnT", [128, 256])
    knT = P.sb("knT", [128, 256])
    qdT = P.sb("qdT", [128, 256])
    egcb = P.sb("egcb", [128, 256])
    Gt = P.sb("Gt", [64, 4, 192])
    glb = P.sb("glb_sb", [128, 4])
    gct = P.sb("gct_sb", [64, 4])
    egc = P.sb("egc", [64, 4])
    ekd = P.sb("ekd", [64, 4])
    bws = P.sb("bws", [64, 4])
    Dm, decay, decs, A_, aqk, AT, aqkT, Pj, PTj, RT = (t64(n) for n in
                                                     ("Dm", "decay", "decs", "A_", "aqk", "AT", "aqkT", "Pj", "PTj", "RT"))
    ktok = P.sb("ktok", [64, 4, 128])
    vtok = P.sb("vtok", [64, 4, 128])
    bu = P.sb("bu", [64, 4, 128])
    bwk = P.sb("bwk", [64, 4, 128])
    kdec = P.sb("kdec", [64, 4, 128])
    u_sb = P.sb("u_sb", [64, 4, 128])
    wT = P.sb("wT", [128, 256])
    vnew = [P.sb("vnew%d" % i, [64, 128]) for i in range(2)]
    Sst = [P.sb("Sst%d" % i, [128, 128]) for i in range(4)]
    zg = P.sb("zg", [64, 4, 128])
    osum = P.sb("osum", [64, 4, 128])
    sq4 = P.sb("sq4", [64, 4, 128])
    on = [P.sb("on%d" % i, [64, 4, 128]) for i in range(2)]
    sg = P.sb("sg", [64, 4, 128])
    ss4 = P.sb("ss4", [64, 4])
    bk = [P.ps("bank%d" % i, [128, 512]) for i in range(8)]
    half = lambda t, i, nm, rows=128: t.sub(nm, (slice(0, rows), slice(i * 256, (i + 1) * 256)))
    p_D, p_egc = half(bk[0], 0, "p_D", 64), half(bk[0], 1, "p_egc")
    p_QK, p_RP = half(bk[1], 0, "p_QK", 64), half(bk[1], 1, "p_RP", 64)
    p_A, p_B = half(bk[2], 0, "p_A", 64), half(bk[2], 1, "p_B", 64)
    p_wT = half(bk[3], 0, "p_wT")
    p_vn = bk[3].sub("p_vn", (slice(0, 64), slice(256, 384)))
    p_S = bk[3].sub("p_S", (slice(0, 128), slice(384, 512)))
    p_tok = bk[4].sub("p_tok", (slice(0, 64), slice(0, 512)))
    p_u = bk[5]
    p_o = bk[6].sub("p_o", (slice(0, 64), slice(0, 512)))
    p_glb = bk[7].sub("p_glb", (slice(0, 128), slice(0, 4)))
    p_gct = bk[7].sub("p_gct", (slice(0, 64), slice(4, 8)))

    cnt = {"s": 0, "v": 0, "o": 0}
    i64 = ident[0:64, 0:64]

    def c64(j):
        return slice(j * 64, (j + 1) * 64)

    class _Stop(Exception):
        pass

    def ck(n):
        if STOP == n:
            raise _Stop()

    def group(dr, tok0, final):
        c0 = tok0 // 64
        ts_ = slice(tok0, tok0 + 256)
        for (src, dst, sc_) in ((xc[0], qnT, 128.0 ** -0.5), (xc[1], knT, None)):
            P.act(sqt[:], src[:, ts_], AF.Square)
            P.matmul(p_u[:, 0:256], ones[:, :], sqt[:])
            P.ts("dve", rn[:], p_u[:, 0:256], 1e-6, ALU.add)
            P.act(rn[:], rn[:], AF.Sqrt)
            P.recip(rn[:], rn[:])
            if sc_ is not None:
                P.stt("dve", dst[:], src[:, ts_], sc_, rn[:], ALU.mult, ALU.mult)
            else:
                P.tt("dve", dst[:], src[:, ts_], rn[:], ALU.mult)
        ck(10)
        gsl = g_all[dr][:, c0:c0 + 4]
        P.matmul(p_glb[:, :], ones[0:64, :], gsl)
        P.matmul(p_gct[:, :], ctri[dr][:, :], gsl)
        P.act(glb[:], p_glb[:, :], AF.Exp)
        P.act(egc[:], p_gct[:, :], AF.Exp)
        P.copy("dve", gct[:], p_gct[:, :])
        P.tt("dve", ekd[:], p_glb[0:64, :], gct[:], ALU.subtract)
        P.act(ekd[:], ekd[:], AF.Exp)
        P.tt("dve", bws[:], beta[dr][:, c0:c0 + 4], egc[:], ALU.mult)
        ck(11)
        for j in range(4):
            P.ts("dve", Gt[:, j, :], trione[dr][:, :], g_all[dr][:, c0 + j:c0 + j + 1], ALU.mult)
        ck(111)
        for j in range(4):
            P.matmul(p_D[:, c64(j)], ctri[dr][:, :], Gt[:, j, 64:128], start=True, stop=False)
            P.matmul(p_D[:, c64(j)], negones[:, :], Gt[:, j, 0:64], start=False, stop=True)
        ck(112)
        for j in range(4):
            P.matmul(p_egc[:, c64(j)], Gt[:, j, 64:192], ctri[dr][:, :])
        ck(12)
        P.act(egcb[:], p_egc[:, :], AF.Exp)
        P.tt("dve", qdT[:], qnT[:], egcb[:], ALU.mult)
        P.tt("dve", Dm[:], p_D[:, :], negm[dr][:], ALU.add)
        P.act(decay[:], Dm[:], AF.Exp)
        P.tt("pool", decs[:], decay[:], smask[dr][:], ALU.mult)
        ck(13)
        for j in range(4):
            P.matmul(p_D[:, c64(j)], knT[:, c64(j)], knT[:, c64(j)])
            P.matmul(p_QK[:, c64(j)], qnT[:, c64(j)], knT[:, c64(j)])
        for j in range(4):
            P.stt("dve", A_[:, c64(j)], p_D[:, c64(j)], nbeta[dr][:, c0 + j:c0 + j + 1], decs[:, c64(j)], ALU.mult, ALU.mult)
        P.tt("dve", aqk[:], p_QK[:, :], decay[:], ALU.mult)
        for j in range(4):
            P.transpose(p_A[:, c64(j)], A_[:, c64(j)], i64)
            P.transpose(p_B[:, c64(j)], aqk[:, c64(j)], i64)
        P.copy("act", AT[:], p_A[:, :])
        P.copy("dve", aqkT[:], p_B[:, :])
        ck(14)
        P.tt("dve", RT[:], ident4[:], AT[:], ALU.add)
        Pc, PTc = A_, AT
        for lvl in range(1, 6):
            for j in range(4):
                P.matmul(p_A[:, c64(j)], PTc[:, c64(j)], Pc[:, c64(j)])
            if lvl < 5:
                for j in range(4):
                    P.matmul(p_B[:, c64(j)], Pc[:, c64(j)], PTc[:, c64(j)])
            P.copy("act", Pj[:], p_A[:, :])
            if lvl < 5:
                P.copy("dve", PTj[:], p_B[:, :])
            for j in range(4):
                P.matmul(p_RP[:, c64(j)], Pj[:, c64(j)], RT[:, c64(j)])
            P.tt("dve", RT[:], RT[:], p_RP[:, :], ALU.add)
            Pc, PTc = Pj, PTj
        ck(15)
        for (srcT, dst) in ((knT, ktok), (None, vtok)):
            for j in range(4):
                in_ = srcT[:, c64(j)] if srcT is not None else xc[2][:, tok0 + j * 64:tok0 + (j + 1) * 64]
                P.transpose(p_tok[:, j * 128:(j + 1) * 128], in_, ident[:, :])
            P.copy("act", dst[:, :, :], p_tok.v(p_tok.h[:, :].rearrange("p (c d) -> p c d", c=4)))
        ck(16)
        for j in range(4):
            P.ts("dve", bu[:, j, :], vtok[:, j, :], beta[dr][:, c0 + j:c0 + j + 1], ALU.mult)
            P.ts("pool", bwk[:, j, :], ktok[:, j, :], bws[:, j:j + 1], ALU.mult)
            P.ts("pool", kdec[:, j, :], ktok[:, j, :], ekd[:, j:j + 1], ALU.mult)
        for j in range(4):
            P.matmul(p_u[0:64, j * 128:(j + 1) * 128], RT[:, c64(j)], bu[:, j, :])
            P.matmul(p_wT[:, c64(j)], bwk[:, j, :], RT[:, c64(j)])
        P.copy("act", u_sb[:, :, :], p_u.v(p_u.h[0:64, :].rearrange("p (c d) -> p c d", c=4)))
        P.copy("dve", wT[:], p_wT[:, :])
        ck(17)
        need_out = with_ctx or tok0 < S
        if need_out and final:
            P.dma("sp", zg[:, :, :], z_d[:, c0:c0 + 4, :])
        js = list(range(4)) if dr == 0 else [3, 2, 1, 0]
        for j in js:
            S_cur = Sst[cnt["s"] % 4]
            S_nxt = Sst[(cnt["s"] + 1) % 4]
            cnt["s"] += 1
            vn = vnew[cnt["v"] % 2]
            cnt["v"] += 1
            P.matmul(p_vn[:, :], wT[:, c64(j)], S_cur[:, :])
            P.tt("dve", vn[:, :], u_sb[:, j, :], p_vn[:, :], ALU.subtract)
            if need_out:
                P.matmul(p_o[:, j * 128:(j + 1) * 128], qdT[:, c64(j)], S_cur[:, :], start=True, stop=False)
                P.matmul(p_o[:, j * 128:(j + 1) * 128], aqkT[:, c64(j)], vn[:, :], start=False, stop=True)
            P.matmul(p_S[:, :], kdec[:, j, :], vn[:, :])
            P.stt("dve", S_nxt[:, :], S_cur[:, :], glb[:, j:j + 1], p_S[:, :], ALU.mult, ALU.add)
        ck(18)
        if need_out:
            ob3 = p_o.v(p_o.h[:, :].rearrange("p (c d) -> p c d", c=4))
            if not final:
                P.copy("act", ob_store[:, c0:c0 + 4, :], ob3)
            else:
                w_ = cnt["o"] % 2
                cnt["o"] += 1
                P.tt("dve", osum[:, :, :], ob3, ob_store[:, c0:c0 + 4, :], ALU.add)
                P.act(sq4[:, :, :], osum[:, :, :], AF.Square)
                P.op("dve", lambda e, a=ss4[:, :].ap, b_=sq4[:, :, :].ap: e.tensor_reduce(a, b_, AX.X, ALU.add),
                     reads=[sq4], writes=[ss4])
                P.ts("dve", ss4[:, :], ss4[:, :], 1.0 / 128.0, ALU.mult, EPS, ALU.add)
                P.act(ss4[:, :], ss4[:, :], AF.Sqrt)
                P.recip(ss4[:, :], ss4[:, :])
                for jj in range(4):
                    P.ts("dve", on[w_][:, jj, :], osum[:, jj, :], ss4[:, jj:jj + 1], ALU.mult)
                P.tt("pool", on[w_][:, :, :], on[w_][:, :, :], nw4.v(nw4.h[:, :].rearrange("p (c d) -> p c d", c=4)), ALU.mult)
                P.act(sg[:, :, :], zg[:, :, :], AF.Silu)
                P.tt("dve", on[w_][:, :, :], on[w_][:, :, :], sg[:, :, :], ALU.mult)
                P.dma("sp", o_d[:, c0:c0 + 4, :], on[w_][:, :, :])

    for dr, final in ((1, False), (0, True)):
        P.memset("dve", Sst[cnt["s"] % 4][:, :], 0.0)
        try:
            group(dr, S, final)
        except _Stop:
            return P
        if STOP == 1:
            return P
        gl = list(range(16)) if dr == 0 else list(range(15, -1, -1))
        for g_ in gl:
            group(dr, g_ * 256, final)
    if not with_ctx:
        P.memset("dve", on[0][:, :, :], 0.0)
        P.dma("sp", o_d[:, 64:68, :], on[0][:, :, :])
    return P


def gdn_masks():
    a = np.arange(64)
    tf = (a[:, None] <= a[None, :]).astype(np.float32)
    tb = (a[:, None] >= a[None, :]).astype(np.float32)
    ctri = np.stack([tf, tb])
    incl = np.stack([tb, tf])
    strict = np.stack([(a[:, None] > a[None, :]).astype(np.float32), (a[:, None] < a[None, :]).astype(np.float32)])
    negm = np.tile((incl - 1.0) * 1e30, (1, 1, 4)).astype(np.float32)
    smask = np.tile(strict, (1, 1, 4)).astype(np.float32)
    return ctri, negm, smask


def run_GDN(px, pc, conv_w, a_log, dt_bias, norm_w, with_ctx):
    P = build_GDN(with_ctx)
    ident = np.eye(128, dtype=np.float32)
    ctri, negm, smask = gdn_masks()
    nw = np.ascontiguousarray(np.tile(norm_w[None, :], (64, 4)))
    maps = []
    o0 = 4640
    for i in range(NCORES):
        b, h = i // 4, i % 4
        full = np.concatenate([px[b], pc[b]], axis=0)
        raw = np.stack([full[:, o0 + j * 512 + h * 128:o0 + j * 512 + (h + 1) * 128].T for j in range(3)])
        cw = np.stack([conv_w[:, j * 512 + h * 128:j * 512 + (h + 1) * 128].T for j in range(3)])
        z = full[:, 6176 + h * 128:6176 + (h + 1) * 128]
        a = np.stack([full[:, 6688 + d_ * 4 + h].reshape(68, 64).T for d_ in range(2)])
        bb = np.stack([full[:, 6696 + d_ * 4 + h].reshape(68, 64).T for d_ in range(2)])
        maps.append({"raw": np.ascontiguousarray(raw), "cw": np.ascontiguousarray(cw), "z": chunk_tm(z),
                     "a": np.ascontiguousarray(a), "bb": np.ascontiguousarray(bb),
                     "alog": np.ascontiguousarray(np.tile(a_log[None, :, h], (64, 1))),
                     "dtb": np.ascontiguousarray(np.tile(dt_bias[None, :, h], (64, 1))),
                     "ctri": ctri, "negm": negm, "smask": smask, "nw": nw, "ident": ident})
    res = run_prog(P, maps)
    ox = np.zeros((B, S, 512), np.float32)
    oc = np.zeros((B, LCTX, 512), np.float32)
    for i, r in enumerate(res):
        b, h = i // 4, i % 4
        o = unchunk_tm(r["o"])
        ox[b, :, h * 128:(h + 1) * 128] = o[:S]
        oc[b, :, h * 128:(h + 1) * 128] = o[S:]
    return ox, oc


class _Scope:
    def __init__(self, P):
        self.P = P

    def __enter__(self):
        self.old = self.P.es
        self.P.es = ExitStack()
        return self

    def __exit__(self, *a):
        _barrier(self.P)
        self.P.es.close()
        self.P.es = self.old
        return False


Prog.scope = lambda self: _Scope(self)


def build_L3():
    P = Prog()
    cat = P.dram("cat", [NTOK1, D], F32, "ExternalInput")
    hin = P.dram("h", [NTOK1, D], F32, "ExternalInput")
    vlat = P.dram("vlat", [3, D], F32, "ExternalInput")
    vctx = P.dram("vctx", [3, D], F32, "ExternalInput")
    gat = P.dram("gate", [2, D], F32, "ExternalInput")
    w = P.dram("w", [D, D], F32, "ExternalInput")
    wr_d = P.dram("wr", [D, 16], F32, "ExternalInput")
    idn = P.dram("ident", [128, 128], F32, "ExternalInput")
    h1_o = P.dram("h1", [NTOK1, D], F32, "ExternalOutput")
    h2_o = P.dram("h2", [NTOK1, D], F32, "ExternalOutput")
    aff_o = P.dram("aff", [NTOK1, 16], F32, "ExternalOutput")
    ident = P.sb("ident_sb", [128, 128])
    P.dma("sp", ident[:], idn[:])
    A_l, B_l, A_c, B_c = (P.sb(n, [128, D]) for n in ("A_l", "B_l", "A_c", "B_c"))
    load_mod_vectors(P, vlat, A_l, B_l)
    load_mod_vectors(P, vctx, A_c, B_c)
    G_l, G_c = P.sb("G_l", [128, D]), P.sb("G_c", [128, D])
    P.dma("sp", G_l[:], gat.v(gat.h[0:1, :].to_broadcast([128, D])))
    P.dma("sp", G_c[:], gat.v(gat.h[1:2, :].to_broadcast([128, D])))
    wsb = P.sb("wsb", [128, 16, D], MMDT)
    wv = w.h.rearrange("(k p) n -> p k n", p=128)
    for kk in range(0, 16, 2):
        P.dma("pool", wsb[:, kk:kk + 2, :], w.v(wv[:, kk:kk + 2, :]))
    wr = P.sb("wr_sb", [128, 16, 16])
    P.dma("sp", wr[:], wr_d.v(wr_d.h.rearrange("(k p) n -> p k n", p=128)))
    ct = [P.sb("ct%d" % i, [128, D]) for i in range(2)]
    ht = [P.sb("ht%d" % i, [128, D]) for i in range(2)]
    catT = P.sb("catT", [128, 16, 128], MMDT)
    h1 = P.sb("h1_sb", [128, D])
    h2 = P.sb("h2_sb", [128, D])
    h2T = P.sb("h2T", [128, 16, 128])
    tmp = P.sb("tmp", [128, 512])
    ss = P.sb("ss", [128, 1])
    rstd = P.sb("rstd", [128, 1])
    lg = P.sb("lg", [128, 16])
    st = P.sb("st", [128, 4])
    pst = [P.ps("ps%d" % i, [128, 512]) for i in range(8)]
    tiles = [(i * 128, 128) for i in range(8)] + [(1024, 64)]
    for ti, (t0, rows) in enumerate(tiles):
        lat = ti < 8
        c_t, h_t = ct[ti % 2], ht[ti % 2]
        P.dma("sp", c_t[0:rows, :], cat[t0:t0 + rows, :])
        P.dma("sp", h_t[0:rows, :], hin[t0:t0 + rows, :])
        transpose_to_fm(P, c_t, rows, catT, 0, ident, pst[0:2], 0)
        G = G_l if lat else G_c
        for nb in range(4):
            pt = pst[2 + nb % 2]
            for k in range(16):
                P.matmul(pt[0:rows, :], catT[:, k, 0:rows], wsb[:, k, nb * 512:(nb + 1) * 512], start=(k == 0), stop=(k == 15))
            P.tt("dve", tmp[0:rows, :], pt[0:rows, :], G[0:rows, nb * 512:(nb + 1) * 512], ALU.mult)
            P.tt("pool", h1[0:rows, nb * 512:(nb + 1) * 512], tmp[0:rows, :], h_t[0:rows, nb * 512:(nb + 1) * 512], ALU.add)
        P.dma("sp", h1_o[t0:t0 + rows, :], h1[0:rows, :])
        rms_modulate(P, h1, rows, A_l if lat else A_c, B_l if lat else B_c, h2, ss, rstd)
        P.dma("sp", h2_o[t0:t0 + rows, :], h2[0:rows, :])
        transpose_to_fm(P, h2, rows, h2T, 0, ident, pst[4:6], 0)
        pl = pst[6]
        for k in range(16):
            P.matmul(pl[0:rows, 0:16], h2T[:, k, 0:rows], wr[:, k, :], start=(k == 0), stop=(k == 15))
        mx, nmx, sm, rs = (st[0:rows, j:j + 1] for j in range(4))
        P.op("dve", lambda e, a=mx.ap, b_=pl[0:rows, 0:16].ap: e.reduce_max(a, b_, AX.X), reads=[pl], writes=[st])
        P.ts("dve", nmx, mx, -1.0, ALU.mult)
        P.memset("dve", sm, 0.0)
        P.act(lg[0:rows, :], pl[0:rows, 0:16], AF.Exp, bias=nmx, accum=sm)
        P.recip(rs, sm)
        P.ts("dve", lg[0:rows, :], lg[0:rows, :], rs, ALU.mult)
        P.dma("sp", aff_o[t0:t0 + rows, :], lg[0:rows, :])
    return P


def run_L3(catx, catc, hx, hc, mod_l, norm_w, w_out, w_router):
    P = build_L3()
    ident = np.eye(128, dtype=np.float32)
    maps = []
    for i in range(NCORES):
        b = i // 4
        vlat = np.stack([norm_w, mod_l[b, 4 * D:5 * D], mod_l[b, 3 * D:4 * D]])
        vctx = np.stack([norm_w, mod_l[2, 4 * D:5 * D], mod_l[2, 3 * D:4 * D]])
        gate = np.stack([mod_l[b, 2 * D:3 * D], mod_l[2, 2 * D:3 * D]])
        maps.append({"cat": np.ascontiguousarray(tok_shard(catx, catc, i)), "h": np.ascontiguousarray(tok_shard(hx, hc, i)),
                     "vlat": vlat, "vctx": vctx, "gate": gate, "w": w_out, "wr": w_router, "ident": ident})
    res = run_prog(P, maps)
    h1x, h1c = tok_unshard([r["h1"] for r in res], D)
    h2x, h2c = tok_unshard([r["h2"] for r in res], D)
    afx, afc = tok_unshard([r["aff"] for r in res], 16)
    return h1x, h1c, h2x, h2c, afx, afc


CAPX = 512
CAPC = 32


def build_L4(with_ctx):
    P = Prog()
    NTK = 2 * CAPX + (2 * CAPC if with_ctx else 0)
    affx = P.dram("affx", [4, S], F32, "ExternalInput")
    affc = P.dram("affc", [4, LCTX], F32, "ExternalInput")
    h2x = P.dram("h2x", [B, S, D], F32, "ExternalInput")
    h2c = P.dram("h2c", [B, LCTX, D], F32, "ExternalInput")
    wg_d = P.dram("wg", [2, D, D], F32, "ExternalInput")
    wu_d = P.dram("wu", [2, D, D], F32, "ExternalInput")
    wd_d = P.dram("wd", [2, D, D], F32, "ExternalInput")
    idn = P.dram("ident", [128, 128], F32, "ExternalInput")
    accx = P.dram("accx", [B, S, D], F32, "ExternalOutput")
    accc = P.dram("accc", [B, LCTX, D], F32, "ExternalOutput")
    gsc = P.dram("gsc", [4, CAPX], F32, "Internal")
    isc = P.dram("isc", [4, CAPX], U32, "Internal")
    gscc = P.dram("gscc", [4, CAPC], F32, "Internal")
    iscc = P.dram("iscc", [4, CAPC], U32, "Internal")

    ident = P.sb("ident_sb", [128, 128])
    P.dma("sp", ident[:], idn[:])
    gcol = P.sb("gcol", [128, 4, 4])
    icol = P.sb("icol", [128, 4, 4], U32)
    gcolc = P.sb("gcolc", [32, 4])
    icolc = P.sb("icolc", [32, 4], U32)
    zero = P.sb("zero", [128, 2, D])
    P.memset("pool", zero[:], 0.0)
    for b in range(B):
        av = accx.h[b].rearrange("(n p j) d -> n p j d", p=128, j=2)
        for n in range(S // 256):
            P.dma("sp", accx.v(av[n]), zero[:])
        P.dma("sp", accc.v(accc.h[b].rearrange("(p j) d -> p j d", j=2)), zero[:])
    with P.scope():
        W = P.sb("topk_w", [4, S])
        gt = P.sb("topk_g", [4, CAPX])
        it_ = P.sb("topk_i", [4, CAPX], U32)
        P.dma("sp", W[:], affx[:])
        for i in range(CAPX // 8):
            sl = slice(i * 8, (i + 1) * 8)
            P.op("dve", lambda e, o=gt[:, sl].ap, a=W[:].ap: e.max(o, a), reads=[W], writes=[gt])
            P.op("dve", lambda e, o=it_[:, sl].ap, m=gt[:, sl].ap, a=W[:].ap: e.max_index(o, m, a), reads=[W, gt], writes=[it_])
            P.op("dve", lambda e, o=W[:].ap, m=gt[:, sl].ap, a=W[:].ap: e.match_replace(o, m, a, -1.0), reads=[gt], writes=[W])
        P.dma("sp", gsc[:], gt[:])
        P.dma("sp", isc[:], it_[:])
        P.dma("sp", gcol[:], gsc.v(gsc.h.rearrange("r (t p) -> p r t", p=128)), allow_slow_non_contiguous=True)
        P.dma("sp", icol[:], isc.v(isc.h.rearrange("r (t p) -> p r t", p=128)), allow_slow_non_contiguous=True)
        if with_ctx:
            P.dma("sp", W[:, 0:LCTX], affc[:])
            for i in range(CAPC // 8):
                sl = slice(i * 8, (i + 1) * 8)
                P.op("dve", lambda e, o=gt[:, sl].ap, a=W[:, 0:LCTX].ap: e.max(o, a), reads=[W], writes=[gt])
                P.op("dve", lambda e, o=it_[:, sl].ap, m=gt[:, sl].ap, a=W[:, 0:LCTX].ap: e.max_index(o, m, a), reads=[W, gt], writes=[it_])
                P.op("dve", lambda e, o=W[:, 0:LCTX].ap, m=gt[:, sl].ap, a=W[:, 0:LCTX].ap: e.match_replace(o, m, a, -1.0), reads=[gt], writes=[W])
            P.dma("sp", gscc[:], gt[:, 0:CAPC])
            P.dma("sp", iscc[:], it_[:, 0:CAPC])
            P.dma("sp", gcolc[:], gscc.v(gscc.h.rearrange("r p -> p r")), allow_slow_non_contiguous=True)
            P.dma("sp", icolc[:], iscc.v(iscc.h.rearrange("r p -> p r")), allow_slow_non_contiguous=True)
    xsT = P.sb("xsT", [128, 16, NTK], MMDT)
    hidT = P.sb("hidT", [128, 16, NTK], MMDT)
    wbuf = [P.sb("wbuf%d" % i, [128, 16, 512], MMDT) for i in range(4)]
    xs = [P.sb("xs%d" % i, [128, D]) for i in range(2)]
    ysb = [P.sb("ysb%d" % i, [128, D]) for i in range(2)]
    gsb = P.sb("gsb", [128, 512])
    pst = [P.ps("ps%d" % i, [128, 512]) for i in range(8)]
    acc_bufs = [Tile(accx.h.rearrange("b s d -> (b s) d"), "accx%d" % b) for b in range(B)]
    accc_bufs = [Tile(accc.h.rearrange("b s d -> (b s) d"), "accc%d" % b) for b in range(B)]
    h2xf = h2x.v(h2x.h.rearrange("b s d -> (b s) d"))
    h2cf = h2c.v(h2c.h.rearrange("b s d -> (b s) d"))
    blocks = [(0, 512), (512, 512)] + ([(1024, 64)] if with_ctx else [])
    cnt = {"x": 0, "w": 0, "y": 0}
    for el in range(2):
        for b in range(B):
            r = el * 2 + b
            for t in range(4):
                x_ = xs[cnt["x"] % 2]
                cnt["x"] += 1
                P.idma(x_[:, :], None, h2xf, (icol[:, r, t:t + 1], 0), element_offset=b * S * D)
                transpose_to_fm(P, x_, 128, xsT, b * 512 + t * 128, ident, pst[0:2], 0)
            if with_ctx:
                x_ = xs[cnt["x"] % 2]
                cnt["x"] += 1
                P.idma(x_[0:32, :], None, h2cf, (icolc[:, r:r + 1], 0), element_offset=b * LCTX * D)
                transpose_to_fm(P, x_, 32, xsT, 1024 + b * 32, ident, pst[0:2], 0)
        wgv = wg_d.h[el].rearrange("(k p) n -> p k n", p=128)
        wuv = wu_d.h[el].rearrange("(k p) n -> p k n", p=128)
        for fb in range(4):
            wg_t = wbuf[cnt["w"] % 4]
            wu_t = wbuf[(cnt["w"] + 1) % 4]
            cnt["w"] += 2
            for kk in range(0, 16, 4):
                P.dma("pool", wg_t[:, kk:kk + 4, :], wg_d.v(wgv[:, kk:kk + 4, fb * 512:(fb + 1) * 512]))
                P.dma("pool", wu_t[:, kk:kk + 4, :], wu_d.v(wuv[:, kk:kk + 4, fb * 512:(fb + 1) * 512]))
            for fi in range(4):
                ft = fb * 4 + fi
                for bi, (c0, cn) in enumerate(blocks):
                    pg = pst[2 + (bi % 2) * 2]
                    pu = pst[3 + (bi % 2) * 2]
                    for k in range(16):
                        P.matmul(pg[:, 0:cn], wg_t[:, k, fi * 128:(fi + 1) * 128], xsT[:, k, c0:c0 + cn], start=(k == 0), stop=(k == 15))
                    for k in range(16):
                        P.matmul(pu[:, 0:cn], wu_t[:, k, fi * 128:(fi + 1) * 128], xsT[:, k, c0:c0 + cn], start=(k == 0), stop=(k == 15))
                    P.act(gsb[:, 0:cn], pg[:, 0:cn], AF.Silu)
                    P.tt("dve", hidT[:, ft, c0:c0 + cn], gsb[:, 0:cn], pu[:, 0:cn], ALU.mult)
        wdv = wd_d.h[el].rearrange("(k p) n -> p k n", p=128)
        for db in range(4):
            for kk in range(0, 16, 4):
                P.dma("pool", wbuf[db][:, kk:kk + 4, :], wd_d.v(wdv[:, kk:kk + 4, db * 512:(db + 1) * 512]))
        cnt["w"] = 0
        ctiles = [(b, t, b * 512 + t * 128, 128) for b in range(B) for t in range(4)]
        if with_ctx:
            ctiles += [(b, None, 1024 + b * 32, 32) for b in range(B)]
        for (b, t, c0, rows) in ctiles:
            r = el * 2 + b
            y_ = ysb[cnt["y"] % 2]
            for db in range(4):
                pt = pst[6 + db % 2]
                for ft in range(16):
                    P.matmul(pt[0:rows, :], hidT[:, ft, c0:c0 + rows], wbuf[db][:, ft, :], start=(ft == 0), stop=(ft == 15))
                gv = gcol[:, r, t:t + 1] if t is not None else gcolc[:, r:r + 1]
                if db % 2 == 0:
                    P.ts("dve", y_[0:rows, db * 512:(db + 1) * 512], pt[0:rows, :], gv, ALU.mult)
                else:
                    P.act(y_[0:rows, db * 512:(db + 1) * 512], pt[0:rows, :], AF.Copy, scale=gv)
            cnt["y"] += 1
            if t is not None:
                P.idma(acc_bufs[b][:, :], (icol[:, r, t:t + 1], 0), y_[0:rows, :], None, compute_op=ALU.add, element_offset=b * S * D)
            else:
                P.idma(accc_bufs[b][:, :], (icolc[:, r:r + 1], 0), y_[0:rows, :], None, compute_op=ALU.add, element_offset=b * LCTX * D)
    return P


def run_L4(afx, afc, h2x, h2c, wg, wu, wd, with_ctx):
    P = build_L4(with_ctx)
    ident = np.eye(128, dtype=np.float32)
    maps = []
    for i in range(NCORES):
        ax = np.stack([afx[b, :, 2 * i + el] for el in range(2) for b in range(B)])
        ac = np.stack([afc[b, :, 2 * i + el] for el in range(2) for b in range(B)])
        maps.append({"affx": np.ascontiguousarray(ax), "affc": np.ascontiguousarray(ac), "h2x": h2x, "h2c": h2c,
                     "wg": wg[2 * i:2 * i + 2], "wu": wu[2 * i:2 * i + 2], "wd": wd[2 * i:2 * i + 2], "ident": ident})
    res = run_prog(P, maps)
    return np.stack([r["accx"] for r in res]), np.stack([r["accc"] for r in res])


def build_L5(final):
    P = Prog()
    parts = P.dram("parts", [NCORES, NTOK1, D], F32, "ExternalInput")
    h1 = P.dram("h1", [NTOK1, D], F32, "ExternalInput")
    gat = P.dram("gate", [2, D], F32, "ExternalInput")
    fw = P.dram("fw", [1, D], F32, "ExternalInput")
    out = P.dram("out", [NTOK1, D], F32, "ExternalOutput")
    G_l, G_c = P.sb("G_l", [128, D]), P.sb("G_c", [128, D])
    P.dma("sp", G_l[:], gat.v(gat.h[0:1, :].to_broadcast([128, D])))
    P.dma("sp", G_c[:], gat.v(gat.h[1:2, :].to_broadcast([128, D])))
    FW = P.sb("FW", [128, D])
    P.dma("sp", FW[:], fw.v(fw.h[0:1, :].to_broadcast([128, D])))
    pt = [P.sb("pt%d" % i, [128, D]) for i in range(3)]
    acc = [P.sb("acc%d" % i, [128, D]) for i in range(2)]
    ht = [P.sb("ht%d" % i, [128, D]) for i in range(2)]
    yt = P.sb("yt", [128, D])
    ss = P.sb("ss", [128, 1])
    rstd = P.sb("rstd", [128, 1])
    tiles = [(i * 128, 128) for i in range(8)] + [(1024, 64)]
    n = 0
    for ti, (t0, rows) in enumerate(tiles):
        a = acc[ti % 2]
        h_t = ht[ti % 2]
        P.dma("sp", h_t[0:rows, :], h1[t0:t0 + rows, :])
        P.dma("sp", a[0:rows, :], parts.v(parts.h[0, t0:t0 + rows, :]))
        for c in range(1, NCORES):
            p_ = pt[n % 3]
            n += 1
            P.dma("sp", p_[0:rows, :], parts.v(parts.h[c, t0:t0 + rows, :]))
            P.tt("dve" if c % 2 else "pool", a[0:rows, :], a[0:rows, :], p_[0:rows, :], ALU.add)
        G = G_l if ti < 8 else G_c
        P.tt("dve", a[0:rows, :], a[0:rows, :], G[0:rows, :], ALU.mult)
        P.tt("pool", a[0:rows, :], a[0:rows, :], h_t[0:rows, :], ALU.add)
        if final:
            rms_modulate(P, a, rows, FW, None, yt, ss, rstd)
            P.dma("sp", out[t0:t0 + rows, :], yt[0:rows, :])
        else:
            P.dma("sp", out[t0:t0 + rows, :], a[0:rows, :])
    return P


def run_L5(partx, partc, h1x, h1c, mod_l, final_w, final):
    P = build_L5(final)
    maps = []
    for i in range(NCORES):
        b = i // 4
        gate = np.stack([mod_l[b, 5 * D:6 * D], mod_l[2, 5 * D:6 * D]])
        parts = np.stack([tok_shard(partx[c], partc[c], i) for c in range(NCORES)])
        maps.append({"parts": np.ascontiguousarray(parts), "h1": np.ascontiguousarray(tok_shard(h1x, h1c, i)),
                     "gate": gate, "fw": np.ascontiguousarray(final_w[None, :])})
    res = run_prog(P, maps)
    return tok_unshard([r["out"] for r in res], D)


def kernel(x, c, ctx, c_ctx, w_ada, b_ada, norm_mix_w, norm_ffn_w, w_in, w_out, na_rpb,
           gla_gate_w, gla_gate_b, gla_norm_w, gdn_conv_w, gdn_a_log, gdn_dt_bias, gdn_norm_w,
           w_router, w_exp_gate, w_exp_up, w_exp_down, final_norm_w):
    f = lambda a: np.asarray(a, dtype=np.float32)
    x, c, ctx, c_ctx = f(x), f(c), f(ctx), f(c_ctx)
    mod = run_L0({"c": c, "c_ctx": c_ctx, "w_ada": f(w_ada), "b_ada": f(b_ada)})
    hx, hc = x, ctx
    for l in range(DEPTH):
        ctx_out = l < DEPTH - 1
        px, pc = run_L1(hx, hc, mod[l], f(norm_mix_w)[l], f(w_in)[l])
        ox_na, oc_na = run_NA(px, pc, f(na_rpb)[l], ctx_out)
        gx, gc = run_GLA(px, pc, f(gla_gate_w)[l], f(gla_gate_b)[l], f(gla_norm_w)[l], ctx_out)
        dx, dc = run_GDN(px, pc, f(gdn_conv_w)[l], f(gdn_a_log)[l], f(gdn_dt_bias)[l], f(gdn_norm_w)[l], ctx_out)
        catx = np.concatenate([ox_na, gx, dx], axis=-1)
        catc = np.concatenate([oc_na, gc, dc], axis=-1)
        h1x, h1c, h2x, h2c, afx, afc = run_L3(catx, catc, hx, hc, mod[l], f(norm_ffn_w)[l], f(w_out)[l], f(w_router)[l])
        partx, partc = run_L4(afx, afc, h2x, h2c, f(w_exp_gate)[l], f(w_exp_up)[l], f(w_exp_down)[l], ctx_out)
        hx, hc = run_L5(partx, partc, h1x, h1c, mod[l], f(final_norm_w), l == DEPTH - 1)
    return hx
```

```python
import numpy as np
from contextlib import ExitStack
import concourse.bass as bass
import concourse.mybir as mybir
from concourse.bass_utils import run_bass_kernel_spmd

F32 = mybir.dt.float32
BF16 = mybir.dt.bfloat16
I32 = mybir.dt.int32
U32 = mybir.dt.uint32
AF = mybir.ActivationFunctionType
ALU = mybir.AluOpType
AX = mybir.AxisListType

NCORES = 8
D = 2048
B = 2
S = 4096
LCTX = 256
DEPTH = 2
IN_W = 6704
EPS = 1e-6
NDMA = 24


class Buf:
    __slots__ = ("w", "r", "name", "excl")

    def __init__(self, name):
        self.w = None
        self.r = {}
        self.name = name
        self.excl = False


class View:
    __slots__ = ("t", "ap")

    def __init__(self, t, ap):
        self.t = t
        self.ap = ap


class Tile:
    def __init__(self, handle, name):
        self.h = handle
        self.buf = Buf(name)

    def __getitem__(self, idx):
        return View(self.buf, self.h[idx])

    def v(self, ap):
        return View(self.buf, ap)

    def sub(self, name, idx):
        t = Tile(self.h[idx], name)
        t.buf = self.buf
        return t


class Prog:
    def __init__(self):
        self.nc = bass.Bass("TRN2", target_bir_lowering=False)
        nc = self.nc
        self.es = ExitStack()
        self.engs = {"pe": nc.tensor, "act": nc.scalar, "dve": nc.vector, "pool": nc.gpsimd, "sp": nc.sync}
        self.esem = {e: self.es.enter_context(nc.semaphore("s_" + e)) for e in ("pe", "act", "dve", "pool")}
        self.ecnt = {e: 0 for e in self.esem}
        self.dsems = [self.es.enter_context(nc.semaphore("d%d" % i)) for i in range(NDMA)]
        self.dval = [0] * NDMA
        self.dnext = 0
        self.seen = {e: {} for e in self.engs}
        self.nins = 0
        self._psum = None

    def dram(self, name, shape, dt, kind):
        t = self.nc.dram_tensor(name, list(shape), dt, kind=kind)
        return Tile(t.ap(), name)

    def sb(self, name, shape, dt=F32):
        h = self.es.enter_context(self.nc.sbuf_tensor(name, list(shape), dt))
        return Tile(h, name)

    def ps(self, name, shape, dt=F32):
        h = self.es.enter_context(self.nc.psum_tensor(name, list(shape), dt))
        t = Tile(h, name)
        t.buf.excl = True
        return t

    def _wait(self, eng, evs):
        seen = self.seen[eng]
        best = {}
        for ev in evs:
            if ev is None:
                continue
            k, sem, val = ev
            if eng == "pe" and k == "e:pe":
                continue
            if seen.get(k, 0) >= val:
                continue
            if k not in best or best[k][2] < val:
                best[k] = ev
        for k, (kk, sem, val) in best.items():
            self.engs[eng].wait_ge(sem, val)
            seen[k] = val
            self.nins += 1

    @staticmethod
    def _deps(reads, writes):
        evs = []
        for b in reads:
            if b.w is not None:
                evs.append(b.w)
            if b.excl:
                evs.extend(b.r.values())
        for b in writes:
            if b.w is not None:
                evs.append(b.w)
            evs.extend(b.r.values())
        return evs

    @staticmethod
    def _mark(ev, reads, writes):
        k = ev[0]
        for b in reads:
            b.r[k] = ev
        for b in writes:
            b.w = ev
            b.r = {}

    def op(self, eng, fn, reads=(), writes=()):
        reads = [v.t if isinstance(v, View) else (v.buf if isinstance(v, Tile) else v) for v in reads]
        writes = [v.t if isinstance(v, View) else (v.buf if isinstance(v, Tile) else v) for v in writes]
        self._wait(eng, self._deps(reads, writes))
        ins = fn(self.engs[eng])
        self.ecnt[eng] += 1
        ins.then_inc(self.esem[eng], 1)
        ev = ("e:" + eng, self.esem[eng], self.ecnt[eng])
        self._mark(ev, reads, writes)
        self.nins += 1
        return ev

    def dma(self, q, out, in_, **kw):
        i = self.dnext
        self.dnext = (i + 1) % NDMA
        reads, writes = [in_.t], [out.t]
        evs = self._deps(reads, writes)
        if self.dval[i] > 0:
            evs.append(("d:%d" % i, self.dsems[i], self.dval[i]))
        self._wait(q, evs)
        ins = self.engs[q].dma_start(out=out.ap, in_=in_.ap, **kw)
        self.dval[i] += 16
        ins.then_inc(self.dsems[i], 16)
        ev = ("d:%d" % i, self.dsems[i], self.dval[i])
        self._mark(ev, reads, writes)
        self.nins += 1
        return ev

    def idma(self, out, out_off, in_, in_off, extra_reads=(), **kw):
        i = self.dnext
        self.dnext = (i + 1) % NDMA
        reads, writes = [in_.t] + [v.t for v in extra_reads], [out.t]
        evs = self._deps(reads, writes)
        if self.dval[i] > 0:
            evs.append(("d:%d" % i, self.dsems[i], self.dval[i]))
        self._wait("pool", evs)
        oo = bass.IndirectOffsetOnAxis(ap=out_off[0].ap, axis=out_off[1]) if out_off is not None else None
        io = bass.IndirectOffsetOnAxis(ap=in_off[0].ap, axis=in_off[1]) if in_off is not None else None
        ins = self.nc.gpsimd.indirect_dma_start(out=out.ap, out_offset=oo, in_=in_.ap, in_offset=io, **kw)
        self.dval[i] += 16
        ins.then_inc(self.dsems[i], 16)
        ev = ("d:%d" % i, self.dsems[i], self.dval[i])
        self._mark(ev, reads, writes)
        self.nins += 1
        return ev

    def finish(self):
        evs = [("d:%d" % i, self.dsems[i], self.dval[i]) for i in range(NDMA) if self.dval[i] > 0]
        evs += [("e:" + e, self.esem[e], self.ecnt[e]) for e in self.esem if self.ecnt[e] > 0]
        self._wait("sp", evs)
        self.es.close()
        return self.nc

    def matmul(self, out, lhsT, rhs, start=True, stop=True):
        return self.op("pe", lambda e: e.matmul(out.ap, lhsT.ap, rhs.ap, start=start, stop=stop),
                       reads=[lhsT, rhs], writes=[out])

    def transpose(self, out, in_, ident):
        return self.op("pe", lambda e: e.transpose(out.ap, in_.ap, ident.ap), reads=[in_, ident], writes=[out])

    def act(self, out, in_, func, bias=None, scale=None, accum=None, eng="act"):
        kw = {}
        reads = [in_]
        writes = [out]
        if bias is not None:
            if isinstance(bias, View):
                kw["bias"] = bias.ap
                reads.append(bias)
            else:
                kw["bias"] = bias
        if scale is not None:
            if isinstance(scale, View):
                kw["scale"] = scale.ap
                reads.append(scale)
            else:
                kw["scale"] = scale
        if accum is not None:
            kw["accum_out"] = accum.ap
            writes.append(accum)
        return self.op("act", lambda e: e.activation(out.ap, in_.ap, func, **kw), reads=reads, writes=writes)

    def copy(self, eng, out, in_):
        if eng == "act":
            return self.op("act", lambda e: e.copy(out.ap, in_.ap), reads=[in_], writes=[out])
        return self.op(eng, lambda e: e.tensor_copy(out.ap, in_.ap), reads=[in_], writes=[out])

    def tt(self, eng, out, a, b, op):
        return self.op(eng, lambda e: e.tensor_tensor(out.ap, a.ap, b.ap, op), reads=[a, b], writes=[out])

    def ts(self, eng, out, a, s1, op0, s2=None, op1=None, accum=None):
        reads = [a]
        writes = [out]
        s1a = s1.ap if isinstance(s1, View) else s1
        s2a = s2.ap if isinstance(s2, View) else s2
        if isinstance(s1, View):
            reads.append(s1)
        if isinstance(s2, View):
            reads.append(s2)
        kw = {}
        if op1 is not None:
            kw["op1"] = op1
        if accum is not None:
            kw["accum_out"] = accum.ap
            writes.append(accum)
        return self.op(eng, lambda e: e.tensor_scalar(out.ap, a.ap, s1a, s2a, op0, **kw), reads=reads, writes=writes)

    def stt(self, eng, out, a, s, b, op0, op1):
        reads = [a, b]
        sa = s.ap if isinstance(s, View) else s
        if isinstance(s, View):
            reads.append(s)
        return self.op(eng, lambda e: e.scalar_tensor_tensor(out.ap, a.ap, sa, b.ap, op0, op1), reads=reads, writes=[out])

    def recip(self, out, in_):
        return self.op("dve", lambda e: e.reciprocal(out.ap, in_.ap), reads=[in_], writes=[out])

    def memset(self, eng, out, val):
        return self.op(eng, lambda e: e.memset(out.ap, val), reads=[], writes=[out])


def run_prog(prog, in_maps):
    nc = prog.finish()
    res = run_bass_kernel_spmd(nc, in_maps, core_ids=list(range(NCORES)))
    return res.results


MODC = 6 * D // NCORES


def build_L0():
    P = Prog()
    cT = P.dram("cT", [128, 16, 3], F32, "ExternalInput")
    w = P.dram("w", [DEPTH, D, MODC], F32, "ExternalInput")
    bb = P.dram("b", [DEPTH, MODC], F32, "ExternalInput")
    out = P.dram("out", [DEPTH, 3, MODC], F32, "ExternalOutput")
    c_sb = P.sb("c_sb", [128, 16, 3])
    sc = P.sb("sc", [128, 16, 3])
    P.dma("sp", c_sb[:], cT[:])
    P.act(sc[:], c_sb[:], AF.Silu)
    wt = [P.sb("wt%d" % i, [128, 16, 512]) for i in range(2)]
    bt = [P.sb("bt%d" % i, [3, 512]) for i in range(2)]
    ot = [P.sb("ot%d" % i, [3, 512]) for i in range(2)]
    pt = [P.ps("pt%d" % i, [128, 512]) for i in range(2)]
    it = 0
    for l in range(DEPTH):
        wv = w.h[l].rearrange("(k p) n -> p k n", p=128)
        for j in range(MODC // 512):
            s = it % 2
            P.dma("sp", wt[s][:], w.v(wv[:, :, j * 512:(j + 1) * 512]))
            for r in range(3):
                P.dma("sp", bt[s][r:r + 1, :], bb.v(bb.h[l:l + 1, j * 512:(j + 1) * 512]))
            for k in range(16):
                P.matmul(pt[s][0:3, :], sc[:, k, :], wt[s][:, k, :], start=(k == 0), stop=(k == 15))
            P.tt("dve", ot[s][:], pt[s][0:3, :], bt[s][:], ALU.add)
            P.dma("sp", out.v(out.h[l, :, j * 512:(j + 1) * 512]), ot[s][:])
            it += 1
    return P


def run_L0(inp):
    c_all = np.concatenate([inp["c"], inp["c_ctx"][None, :]], axis=0).astype(np.float32)
    cT = np.ascontiguousarray(c_all.reshape(3, 16, 128).transpose(2, 1, 0))
    P = build_L0()
    maps = []
    for i in range(NCORES):
        maps.append({
            "cT": cT,
            "w": np.ascontiguousarray(inp["w_ada"][:, :, i * MODC:(i + 1) * MODC]),
            "b": np.ascontiguousarray(inp["b_ada"][:, i * MODC:(i + 1) * MODC]),
        })
    res = run_prog(P, maps)
    return np.concatenate([r["out"] for r in res], axis=2)


def _barrier(P):
    evs = [("d:%d" % i, P.dsems[i], P.dval[i]) for i in range(NDMA) if P.dval[i] > 0]
    evs += [("e:" + e, P.esem[e], P.ecnt[e]) for e in P.esem if P.ecnt[e] > 0]
    for e in P.engs:
        P._wait(e, evs)


Prog.barrier = _barrier

MMDT = BF16
NTOK1 = 1024 + 64


def rms_modulate(P, x_t, rows, A, Bt, y_t, ss, rstd):
    P.memset("dve", ss[0:rows, :], 0.0)
    P.act(y_t[0:rows, :], x_t[0:rows, :], AF.Square, accum=ss[0:rows, :])
    P.ts("dve", rstd[0:rows, :], ss[0:rows, :], 1.0 / D, ALU.mult, EPS, ALU.add)
    P.act(rstd[0:rows, :], rstd[0:rows, :], AF.Sqrt)
    P.recip(rstd[0:rows, :], rstd[0:rows, :])
    P.stt("dve", y_t[0:rows, :], x_t[0:rows, :], rstd[0:rows, 0:1], A[0:rows, :], ALU.mult, ALU.mult)
    if Bt is not None:
        P.tt("dve", y_t[0:rows, :], y_t[0:rows, :], Bt[0:rows, :], ALU.add)


def load_mod_vectors(P, vec, A, Bt):
    tmp = P.sb("tmpv_" + A.buf.name, [128, D])
    P.dma("sp", A[:], vec.v(vec.h[0:1, :].to_broadcast([128, D])))
    P.dma("sp", tmp[:], vec.v(vec.h[1:2, :].to_broadcast([128, D])))
    P.dma("sp", Bt[:], vec.v(vec.h[2:3, :].to_broadcast([128, D])))
    P.stt("dve", A[:], tmp[:], 1.0, A[:], ALU.add, ALU.mult)


def transpose_to_fm(P, y_t, rows, dstT, tok0, ident, pst, ev_i):
    for g in range(4):
        pt = pst[(ev_i + g) % len(pst)]
        for j in range(4):
            k = g * 4 + j
            P.transpose(pt[:, j * 128:j * 128 + rows], y_t[0:rows, k * 128:(k + 1) * 128], ident[0:rows, 0:rows])
        src = pt.v(pt.h[:, 0:512].rearrange("p (j t) -> p j t", j=4)[:, :, 0:rows])
        dst = dstT[:, g * 4:(g + 1) * 4, tok0:tok0 + rows]
        P.copy("act" if g % 2 == 0 else "dve", dst, src)


def build_L1():
    P = Prog()
    h = P.dram("h", [NTOK1, D], F32, "ExternalInput")
    vlat = P.dram("vlat", [3, D], F32, "ExternalInput")
    vctx = P.dram("vctx", [3, D], F32, "ExternalInput")
    w = P.dram("w", [D, IN_W], F32, "ExternalInput")
    idn = P.dram("ident", [128, 128], F32, "ExternalInput")
    p = P.dram("p", [NTOK1, IN_W], F32, "ExternalOutput")
    ident = P.sb("ident_sb", [128, 128])
    P.dma("sp", ident[:], idn[:])
    A_l, B_l, A_c, B_c = (P.sb(n, [128, D]) for n in ("A_l", "B_l", "A_c", "B_c"))
    load_mod_vectors(P, vlat, A_l, B_l)
    load_mod_vectors(P, vctx, A_c, B_c)
    nxT = P.sb("nxT", [128, 16, NTOK1], MMDT)
    xt = [P.sb("xt%d" % i, [128, D]) for i in range(2)]
    yt = P.sb("yt", [128, D])
    ss = P.sb("ss", [128, 1])
    rstd = P.sb("rstd", [128, 1])
    pst = [P.ps("ps%d" % i, [128, 512]) for i in range(8)]
    tiles = [(i * 128, 128) for i in range(8)] + [(1024, 64)]
    for ti, (t0, rows) in enumerate(tiles):
        x_t = xt[ti % 2]
        P.dma("sp", x_t[0:rows, :], h[t0:t0 + rows, :])
        lat = ti < 8
        rms_modulate(P, x_t, rows, A_l if lat else A_c, B_l if lat else B_c, yt, ss, rstd)
        transpose_to_fm(P, yt, rows, nxT, t0, ident, pst[0:4], ti * 4)
    wv = w.h.rearrange("(k p) n -> p k n", p=128)
    wb = [P.sb("wb%d" % i, [128, 16, 512], MMDT) for i in range(2)]
    ob = [P.sb("ob%d" % i, [128, 512]) for i in range(4)]
    nblk = (IN_W + 511) // 512
    oi = 0
    for nb in range(nblk):
        c0 = nb * 512
        cw = min(512, IN_W - c0)
        wt = wb[nb % 2]
        for kk in range(0, 16, 4):
            P.dma("pool", wt[:, kk:kk + 4, 0:cw], w.v(wv[:, kk:kk + 4, c0:c0 + cw]))
        for ti, (t0, rows) in enumerate(tiles):
            pt = pst[4 + oi % 4]
            for k in range(16):
                P.matmul(pt[0:rows, 0:cw], nxT[:, k, t0:t0 + rows], wt[:, k, 0:cw], start=(k == 0), stop=(k == 15))
            o = ob[oi % 4]
            P.copy("act" if oi % 2 == 0 else "dve", o[0:rows, 0:cw], pt[0:rows, 0:cw])
            P.dma("sp", p[t0:t0 + rows, c0:c0 + cw], o[0:rows, 0:cw])
            oi += 1
    return P


def tok_shard(lat, ctx, i):
    b, q = i // 4, i % 4
    return np.concatenate([lat[b, q * 1024:(q + 1) * 1024], ctx[b, q * 64:(q + 1) * 64]], axis=0)


def tok_unshard(parts, width):
    lat = np.zeros((B, S, width), np.float32)
    ctx = np.zeros((B, LCTX, width), np.float32)
    for i, pp in enumerate(parts):
        b, q = i // 4, i % 4
        lat[b, q * 1024:(q + 1) * 1024] = pp[:1024]
        ctx[b, q * 64:(q + 1) * 64] = pp[1024:]
    return lat, ctx


def run_L1(hx, hc, mod_l, norm_w, w_in):
    P = build_L1()
    ident = np.eye(128, dtype=np.float32)
    maps = []
    for i in range(NCORES):
        b = i // 4
        vlat = np.stack([norm_w, mod_l[b, D:2 * D], mod_l[b, 0:D]])
        vctx = np.stack([norm_w, mod_l[2, D:2 * D], mod_l[2, 0:D]])
        maps.append({"h": np.ascontiguousarray(tok_shard(hx, hc, i)), "vlat": vlat, "vctx": vctx,
                     "w": w_in, "ident": ident})
    res = run_prog(P, maps)
    return tok_unshard([r["p"] for r in res], IN_W)


NEG = -30000.0
NA_DT = BF16


def na_row_class(r):
    return r if r < 4 else (4 if r < 60 else r - 55)


def build_NA(with_ctx):
    P = Prog()
    NH = 4
    TT = S + LCTX
    qT_d = P.dram("qT", [NH, 64, TT], F32, "ExternalInput")
    kT_d = P.dram("kT", [NH, 64, TT], F32, "ExternalInput")
    v_d = P.dram("v", [NH, 64, 68, 64], F32, "ExternalInput")
    bias_d = P.dram("bias", [NH, 64, 9, 512], F32, "ExternalInput")
    idn = P.dram("ident", [128, 128], F32, "ExternalInput")
    o_d = P.dram("o", [NH, 64, 68, 64], F32, "ExternalOutput")
    ident = P.sb("ident_sb", [128, 128])
    P.dma("sp", ident[:], idn[:])
    qT = [P.sb("qT%d" % i, [64, TT], NA_DT) for i in range(2)]
    kT = [P.sb("kT%d" % i, [64, TT], NA_DT) for i in range(2)]
    V = [P.sb("V%d" % i, [64, 68, 64], NA_DT) for i in range(2)]
    bias = [P.sb("bias%d" % i, [64, 9, 512]) for i in range(2)]
    O = [P.sb("O%d" % i, [64, 68, 64]) for i in range(2)]
    Sb = [P.sb("Sb%d" % i, [64, 768]) for i in range(2)]
    Pm = [P.sb("Pm%d" % i, [64, 768], NA_DT) for i in range(2)]
    PT = [P.sb("PT%d" % i, [64, 12, 64], NA_DT) for i in range(2)]
    st = [P.sb("st%d" % i, [64, 4]) for i in range(2)]
    s_loc = [P.ps("s_loc%d" % i, [64, 512]) for i in range(2)]
    s_ctx = [P.ps("s_ctx%d" % i, [64, 256]) for i in range(2)]
    pt_all = [P.ps("pt%d" % i, [64, 768], NA_DT) for i in range(2)]
    o_ps = [P.ps("o_ps%d" % i, [64, 64]) for i in range(2)]
    identb = P.sb("identb", [64, 64], NA_DT)
    P.copy("dve", identb[:, :], ident[0:64, 0:64])
    it = 0
    for hh in range(NH):
        s = hh % 2
        dq = "pool" if NA_DT != F32 else "sp"
        P.dma(dq, qT[s][:], qT_d.v(qT_d.h[hh]))
        P.dma(dq, kT[s][:], kT_d.v(kT_d.h[hh]))
        P.dma(dq, V[s][:], v_d.v(v_d.h[hh]))
        P.dma("sp", bias[s][:], bias_d.v(bias_d.h[hh]))
        nrows = 68 if with_ctx else 64
        if not with_ctx:
            P.memset("pool", O[s][:, 64:68, :], 0.0)
        for r in range(nrows):
            u = it % 2
            it += 1
            local = r < 64
            q_ap = qT[s][:, r * 64:(r + 1) * 64]
            nk = 12 if local else 4
            wid = nk * 64
            if local:
                r0 = min(max(r - 4, 0), 56)
                P.matmul(s_loc[u][:, :], q_ap, kT[s][:, r0 * 64:r0 * 64 + 512])
                P.matmul(s_ctx[u][:, :], q_ap, kT[s][:, S:S + 256])
                P.stt("dve", Sb[u][:, 0:512], s_loc[u][:, :], 0.125, bias[s][:, na_row_class(r), :], ALU.mult, ALU.add)
                P.act(Sb[u][:, 512:768], s_ctx[u][:, :], AF.Identity, scale=0.125)
            else:
                P.matmul(s_ctx[u][:, :], q_ap, kT[s][:, S:S + 256])
                P.act(Sb[u][:, 0:256], s_ctx[u][:, :], AF.Identity, scale=0.125)
            mx, nmx, sm, rs = (st[u][:, j:j + 1] for j in range(4))
            P.op("dve", lambda e, a=mx.ap, b_=Sb[u][:, 0:wid].ap: e.reduce_max(a, b_, AX.X), reads=[Sb[u]], writes=[st[u]])
            P.ts("dve", nmx, mx, -1.0, ALU.mult)
            P.memset("dve", sm, 0.0)
            P.act(Pm[u][:, 0:wid], Sb[u][:, 0:wid], AF.Exp, bias=nmx, accum=sm)
            P.recip(rs, sm)
            for j in range(nk):
                P.transpose(pt_all[u][:, j * 64:(j + 1) * 64], Pm[u][:, j * 64:(j + 1) * 64], identb[:, :])
            ptv = pt_all[u].v(pt_all[u].h[:, 0:nk * 64].rearrange("p (j q) -> p j q", j=nk))
            P.copy("dve" if it % 2 else "act", PT[u][:, 0:nk, :], ptv)
            for j in range(nk):
                if local:
                    vr = (r0 + j) if j < 8 else (64 + j - 8)
                else:
                    vr = 64 + j
                P.matmul(o_ps[u][:, :], PT[u][:, j, :], V[s][:, vr, :], start=(j == 0), stop=(j == nk - 1))
            P.ts("dve", O[s][:, r, :], o_ps[u][:, :], rs, ALU.mult)
        P.dma("sp", o_d.v(o_d.h[hh]), O[s][:])
    return P


def na_bias_tables(rpb_l):
    cq = np.arange(64)
    cstart = np.clip(cq - 8, 0, 48)
    colmask = (cq[None, :] >= cstart[:, None]) & (cq[None, :] < cstart[:, None] + 16)
    dc = np.clip(cq[None, :] - cq[:, None] + 15, 0, 30)
    out = np.full((16, 64, 9, 8, 64), NEG, np.float32)
    for rc in range(9):
        r = rc if rc < 4 else (4 if rc == 4 else rc + 55)
        ridx = min(max(r - 4, 0), 56) + np.arange(8)
        dr = ridx - r + 7
        g = rpb_l[:, dr[:, None, None], dc[None, :, :]]
        g = np.where(colmask[None, None], g, NEG)
        out[:, :, rc] = g.transpose(0, 2, 1, 3)
    return out.reshape(16, 64, 9, 512)


def run_NA(px, pc, rpb_l, with_ctx):
    P = build_NA(with_ctx)
    ident = np.eye(128, dtype=np.float32)
    bias_all = na_bias_tables(rpb_l)
    maps = []
    for i in range(NCORES):
        b, h0 = i // 4, 4 * (i % 4)
        full = np.concatenate([px[b, :, 0:3072], pc[b, :, 0:3072]], axis=0)
        q = full[:, 0:1024].reshape(S + LCTX, 16, 64)[:, h0:h0 + 4]
        k = full[:, 1024:2048].reshape(S + LCTX, 16, 64)[:, h0:h0 + 4]
        v = full[:, 2048:3072].reshape(68, 64, 16, 64)[:, :, h0:h0 + 4]
        maps.append({
            "qT": np.ascontiguousarray(q.transpose(1, 2, 0)),
            "kT": np.ascontiguousarray(k.transpose(1, 2, 0)),
            "v": np.ascontiguousarray(v.transpose(2, 1, 0, 3)),
            "bias": np.ascontiguousarray(bias_all[h0:h0 + 4]),
            "ident": ident,
        })
    res = run_prog(P, maps)
    ox = np.zeros((B, S, 1024), np.float32)
    oc = np.zeros((B, LCTX, 1024), np.float32)
    for i, r in enumerate(res):
        b, h0 = i // 4, 4 * (i % 4)
        o = r["o"].transpose(2, 1, 0, 3).reshape(S + LCTX, 4, 64)
        ox[b, :, h0 * 64:(h0 + 4) * 64] = o[:S].reshape(S, 256)
        oc[b, :, h0 * 64:(h0 + 4) * 64] = o[S:].reshape(LCTX, 256)
    return ox, oc


TT = S + LCTX


def build_GLA(with_ctx):
    P = Prog()
    qT_d = P.dram("qT", [64, TT], F32, "ExternalInput")
    kT_d = P.dram("kT", [64, TT], F32, "ExternalInput")
    v_d = P.dram("v", [64, 68, 128], F32, "ExternalInput")
    g_d = P.dram("g", [64, 68, 128], F32, "ExternalInput")
    lr_d = P.dram("lr", [2, 16, TT], F32, "ExternalInput")
    gw_d = P.dram("gw", [2, 17, 64], F32, "ExternalInput")
    ct_d = P.dram("ct", [64, S], F32, "ExternalInput")
    st_d = P.dram("st", [64, S], F32, "ExternalInput")
    psw_d = P.dram("psw", [64, 64], F32, "ExternalInput")
    tri_d = P.dram("tri", [2, 64, 512], F32, "ExternalInput")
    nw_d = P.dram("nw", [64, 512], F32, "ExternalInput")
    idn = P.dram("ident", [128, 128], F32, "ExternalInput")
    o_d = P.dram("o", [64, 68, 128], F32, "ExternalOutput")

    ident = P.sb("ident_sb", [128, 128])
    P.dma("sp", ident[:], idn[:])
    psw = P.sb("psw_sb", [64, 64])
    P.dma("sp", psw[:], psw_d[:])
    tri = [P.sb("tri%d" % i, [64, 512]) for i in range(2)]
    gw = [P.sb("gw%d" % i, [17, 64]) for i in range(2)]
    for i in range(2):
        P.dma("sp", tri[i][:], tri_d.v(tri_d.h[i]))
        P.dma("sp", gw[i][:], gw_d.v(gw_d.h[i]))
    nw4 = P.sb("nw4", [64, 512])
    P.dma("sp", nw4[:], nw_d[:])
    ob_store = P.sb("ob_store", [64, 68, 128])

    NB = 2
    qg = [P.sb("qg%d" % i, [64, 512]) for i in range(NB)]
    kg = [P.sb("kg%d" % i, [64, 512]) for i in range(NB)]
    vg = [P.sb("vg%d" % i, [64, 8, 128]) for i in range(NB)]
    gg = [P.sb("gg%d" % i, [64, 8, 128]) for i in range(NB)]
    lra = [P.sb("lra%d" % i, [17, 512]) for i in range(NB)]
    ctg = [P.sb("ctg%d" % i, [64, 512]) for i in range(NB)]
    stg = [P.sb("stg%d" % i, [64, 512]) for i in range(NB)]
    tmp = P.sb("tmp", [64, 512])
    e_sb = P.sb("e_sb", [64, 512])
    l_sb = P.sb("l_sb", [64, 512])
    Eq = P.sb("Eq", [64, 512])
    Ek = P.sb("Ek", [64, 512])
    qe = [P.sb("qe%d" % i, [64, 512]) for i in range(NB)]
    keT = P.sb("keT", [64, 512])
    ke = [P.sb("ke%d" % i, [64, 512]) for i in range(NB)]
    attT = [P.sb("attT%d" % i, [64, 512]) for i in range(NB)]
    glast = [P.sb("glast%d" % i, [64, 8]) for i in range(NB)]
    mgt = [P.sb("mgt%d" % i, [64, 128]) for i in range(2)]
    Sb_ = [P.sb("S%d" % i, [64, 128]) for i in range(4)]
    osum = [P.sb("osum%d" % i, [64, 4, 128]) for i in range(2)]
    sq = P.sb("sq", [64, 4, 128])
    on = [P.sb("on%d" % i, [64, 4, 128]) for i in range(2)]
    sg = P.sb("sg", [64, 4, 128])
    ss4 = P.sb("ss4", [64, 4])
    zps = P.ps("zps", [64, 512])
    cps = P.ps("cps", [64, 512])
    aps = P.ps("aps", [64, 512])
    kps = P.ps("kps", [64, 512])
    mps = [P.ps("mps%d" % i, [64, 512]) for i in range(2)]
    ops = [P.ps("ops%d" % i, [64, 512]) for i in range(2)]

    cnt = {"g": 0, "s": 0, "m": 0, "o": 0}

    def group(dr, tok0, ntok, rope, final):
        nch = ntok // 64
        u = cnt["g"] % NB
        cnt["g"] += 1
        c0 = tok0 // 64
        q_, k_, v_, l_ = qg[u], kg[u], vg[u], lra[u]
        P.dma("sp", q_[:, 0:ntok], qT_d[:, tok0:tok0 + ntok])
        P.dma("sp", k_[:, 0:ntok], kT_d[:, tok0:tok0 + ntok])
        P.dma("sp", v_[:, 0:nch, :], v_d[:, c0:c0 + nch, :])
        P.memset("pool", l_[:, :], 1.0)
        P.dma("sp", l_[0:16, 0:ntok], lr_d.v(lr_d.h[dr, :, tok0:tok0 + ntok]))
        if final:
            P.dma("sp", gg[u][:, 0:nch, :], g_d[:, c0:c0 + nch, :])
        if rope:
            P.dma("sp", ctg[u][:, 0:ntok], ct_d[:, tok0:tok0 + ntok])
            P.dma("sp", stg[u][:, 0:ntok], st_d[:, tok0:tok0 + ntok])
            for x in (q_, k_):
                P.matmul(zps[:, 0:ntok], psw[:, :], x[:, 0:ntok])
                P.tt("dve", tmp[:, 0:ntok], zps[:, 0:ntok], stg[u][:, 0:ntok], ALU.mult)
                P.tt("pool", x[:, 0:ntok], x[:, 0:ntok], ctg[u][:, 0:ntok], ALU.mult)
                P.tt("dve", x[:, 0:ntok], x[:, 0:ntok], tmp[:, 0:ntok], ALU.add)
        for j in range(nch):
            P.matmul(zps[:, j * 64:(j + 1) * 64], l_[:, j * 64:(j + 1) * 64], gw[dr][:, :])
        P.act(e_sb[:, 0:ntok], zps[:, 0:ntok], AF.Exp, scale=-1.0)
        P.act(l_sb[:, 0:ntok], e_sb[:, 0:ntok], AF.Ln, bias=1.0)
        for j in range(nch):
            P.matmul(cps[:, j * 64:(j + 1) * 64], l_sb[:, j * 64:(j + 1) * 64], tri[dr][:, 0:64])
        P.act(Eq[:, 0:ntok], cps[:, 0:ntok], AF.Exp, scale=-1.0 / 16.0)
        P.act(Ek[:, 0:ntok], cps[:, 0:ntok], AF.Exp, scale=1.0 / 16.0)
        P.stt("dve", qe[u][:, 0:ntok], q_[:, 0:ntok], 0.125, Eq[:, 0:ntok], ALU.mult, ALU.mult)
        P.tt("dve", keT[:, 0:ntok], k_[:, 0:ntok], Ek[:, 0:ntok], ALU.mult)
        lastcol = 63 if dr == 0 else 0
        P.copy("dve", glast[u][:, 0:nch], Eq.v(Eq.h[:, 0:ntok].rearrange("p (c t) -> p c t", t=64)[:, :, lastcol]))
        for j in range(nch):
            P.matmul(aps[:, j * 64:(j + 1) * 64], keT[:, j * 64:(j + 1) * 64], qe[u][:, j * 64:(j + 1) * 64])
        P.tt("dve", attT[u][:, 0:ntok], aps[:, 0:ntok], tri[dr][:, 0:ntok], ALU.mult)
        for j in range(nch):
            P.transpose(kps[:, j * 64:(j + 1) * 64], keT[:, j * 64:(j + 1) * 64], ident[0:64, 0:64])
        P.copy("act", ke[u][:, 0:ntok], kps[:, 0:ntok])
        for j in range(nch):
            P.matmul(mps[j // 4][:, (j % 4) * 128:(j % 4 + 1) * 128], ke[u][:, j * 64:(j + 1) * 64], v_[:, j, :])
        need_out = with_ctx or tok0 < S
        js = list(range(nch)) if dr == 0 else list(range(nch - 1, -1, -1))
        for n, j in enumerate(js):
            col = (j % 4) * 128
            S_cur = Sb_[cnt["s"] % 4]
            S_nxt = Sb_[(cnt["s"] + 1) % 4]
            cnt["s"] += 1
            if need_out:
                ob = ops[j // 4]
                P.matmul(ob[:, col:col + 128], qe[u][:, j * 64:(j + 1) * 64], S_cur[:, :], start=True, stop=False)
                P.matmul(ob[:, col:col + 128], attT[u][:, j * 64:(j + 1) * 64], v_[:, j, :], start=False, stop=True)
            mg = mgt[cnt["m"] % 2]
            cnt["m"] += 1
            P.ts("dve", mg[:, :], mps[j // 4][:, col:col + 128], glast[u][:, j:j + 1], ALU.mult)
            P.stt("dve", S_nxt[:, :], S_cur[:, :], glast[u][:, j:j + 1], mg[:, :], ALU.mult, ALU.add)
            done_bank = (j % 4 == 3) if dr == 0 else (j % 4 == 0)
            if need_out and done_bank:
                q4 = j // 4
                cb = c0 + 4 * q4
                ob = ops[q4]
                ob3 = ob.v(ob.h[:, :].rearrange("p (c d) -> p c d", c=4))
                if not final:
                    P.copy("act", ob_store[:, cb:cb + 4, :], ob3)
                else:
                    w_ = cnt["o"] % 2
                    cnt["o"] += 1
                    P.tt("dve", osum[w_][:, :, :], ob3, ob_store[:, cb:cb + 4, :], ALU.add)
                    P.act(sq[:, :, :], osum[w_][:, :, :], AF.Square)
                    P.op("dve", lambda e, a=ss4[:, :].ap, b_=sq[:, :, :].ap: e.tensor_reduce(a, b_, AX.X, ALU.add),
                         reads=[sq], writes=[ss4])
                    P.ts("dve", ss4[:, :], ss4[:, :], 1.0 / 128.0, ALU.mult, EPS, ALU.add)
                    P.act(ss4[:, :], ss4[:, :], AF.Sqrt)
                    P.recip(ss4[:, :], ss4[:, :])
                    for jj in range(4):
                        P.ts("dve", on[w_][:, jj, :], osum[w_][:, jj, :], ss4[:, jj:jj + 1], ALU.mult)
                    P.tt("pool", on[w_][:, :, :], on[w_][:, :, :], nw4.v(nw4.h[:, :].rearrange("p (c d) -> p c d", c=4)), ALU.mult)
                    P.act(sg[:, :, :], gg[u][:, 4 * q4:4 * q4 + 4, :], AF.Silu)
                    P.tt("dve", on[w_][:, :, :], on[w_][:, :, :], sg[:, :, :], ALU.mult)
                    P.dma("sp", o_d[:, cb:cb + 4, :], on[w_][:, :, :])

    for dr, final in ((1, False), (0, True)):
        P.memset("dve", Sb_[cnt["s"] % 4][:, :], 0.0)
        group(dr, S, LCTX, False, final)
        gl = list(range(8)) if dr == 0 else list(range(7, -1, -1))
        for g_ in gl:
            group(dr, g_ * 512, 512, True, final)
    if not with_ctx:
        P.memset("dve", on[0][:, :, :], 0.0)
        P.dma("sp", o_d[:, 64:68, :], on[0][:, :, :])
    return P


def rope_tables():
    nf = 16
    freqs = (10000.0 ** (-np.arange(nf, dtype=np.float32) / nf)).astype(np.float32)
    pos = np.arange(S)
    rp, cp = (pos // 64).astype(np.float32), (pos % 64).astype(np.float32)
    ct = np.zeros((64, S), np.float32)
    st = np.zeros((64, S), np.float32)
    psw = np.zeros((64, 64), np.float32)
    for d in range(64):
        half, i = d // 32, d % 32
        f = i % 16
        ang = ((rp if half == 0 else cp) * freqs[f]).astype(np.float32)
        ct[d] = np.cos(ang)
        st[d] = -np.sin(ang) if i < 16 else np.sin(ang)
        partner = d + 16 if i < 16 else d - 16
        psw[partner, d] = 1.0
    return ct, st, psw


def tri_masks():
    a = np.arange(64)
    f = (a[:, None] <= a[None, :]).astype(np.float32)
    bk = (a[:, None] >= a[None, :]).astype(np.float32)
    return np.stack([np.tile(f, (1, 8)), np.tile(bk, (1, 8))])


def chunk_tm(a):
    return np.ascontiguousarray(a.reshape(68, 64, a.shape[-1]).transpose(1, 0, 2))


def unchunk_tm(a):
    return a.transpose(1, 0, 2).reshape(68 * 64, a.shape[-1])


def run_GLA(px, pc, gate_w, gate_b, norm_w, with_ctx):
    P = build_GLA(with_ctx)
    ident = np.eye(128, dtype=np.float32)
    ct, st, psw = rope_tables()
    tri = tri_masks()
    nw = np.ascontiguousarray(np.tile(norm_w[None, :], (64, 4)))
    maps = []
    o0 = 3072
    for i in range(NCORES):
        b, h = i // 4, i % 4
        full = np.concatenate([px[b], pc[b]], axis=0)
        q = full[:, o0 + h * 64:o0 + (h + 1) * 64]
        k = full[:, o0 + 256 + h * 64:o0 + 256 + (h + 1) * 64]
        v = full[:, o0 + 512 + h * 128:o0 + 512 + (h + 1) * 128]
        g = full[:, o0 + 1024 + h * 128:o0 + 1024 + (h + 1) * 128]
        lr = full[:, o0 + 1536:o0 + 1568].reshape(TT, 2, 16)
        gw = np.stack([np.concatenate([gate_w[d][:, h * 64:(h + 1) * 64], gate_b[d][None, h * 64:(h + 1) * 64]], 0) for d in range(2)])
        maps.append({"qT": np.ascontiguousarray(q.T), "kT": np.ascontiguousarray(k.T), "v": chunk_tm(v), "g": chunk_tm(g),
                     "lr": np.ascontiguousarray(lr.transpose(1, 2, 0)), "gw": gw, "ct": ct, "st": st, "psw": psw,
                     "tri": tri, "nw": nw, "ident": ident})
    res = run_prog(P, maps)
    ox = np.zeros((B, S, 512), np.float32)
    oc = np.zeros((B, LCTX, 512), np.float32)
    for i, r in enumerate(res):
        b, h = i // 4, i % 4
        o = unchunk_tm(r["o"])
        ox[b, :, h * 128:(h + 1) * 128] = o[:S]
        oc[b, :, h * 128:(h + 1) * 128] = o[S:]
    return ox, oc


def build_GDN(with_ctx):
    P = Prog()
    raw_d = P.dram("raw", [3, 128, TT], F32, "ExternalInput")
    cw_d = P.dram("cw", [3, 128, 5], F32, "ExternalInput")
    z_d = P.dram("z", [64, 68, 128], F32, "ExternalInput")
    a_d = P.dram("a", [2, 64, 68], F32, "ExternalInput")
    b_d = P.dram("bb", [2, 64, 68], F32, "ExternalInput")
    al_d = P.dram("alog", [64, 2], F32, "ExternalInput")
    dtb_d = P.dram("dtb", [64, 2], F32, "ExternalInput")
    ctri_d = P.dram("ctri", [2, 64, 64], F32, "ExternalInput")
    negm_d = P.dram("negm", [2, 64, 256], F32, "ExternalInput")
    smask_d = P.dram("smask", [2, 64, 256], F32, "ExternalInput")
    nw_d = P.dram("nw", [64, 512], F32, "ExternalInput")
    idn = P.dram("ident", [128, 128], F32, "ExternalInput")
    o_d = P.dram("o", [64, 68, 128], F32, "ExternalOutput")

    ident = P.sb("ident_sb", [128, 128])
    P.dma("sp", ident[:], idn[:])
    ident4 = P.sb("ident4", [64, 256])
    for j in range(4):
        P.dma("sp", ident4[:, j * 64:(j + 1) * 64], idn[0:64, 0:64])
    ones = P.sb("ones", [128, 128])
    P.memset("dve", ones[:], 1.0)
    negones = P.sb("negones", [64, 64])
    P.memset("dve", negones[:], -1.0)
    ctri = [P.sb("ctri%d" % i, [64, 64]) for i in range(2)]
    trione = [P.sb("trione%d" % i, [64, 192]) for i in range(2)]
    negm = [P.sb("negm%d" % i, [64, 256]) for i in range(2)]
    smask = [P.sb("smask%d" % i, [64, 256]) for i in range(2)]
    for i in range(2):
        P.dma("sp", ctri[i][:], ctri_d.v(ctri_d.h[i]))
        P.dma("sp", trione[i][:, 0:64], ctri_d.v(ctri_d.h[i]))
        P.memset("dve", trione[i][:, 64:192], 1.0)
        P.dma("sp", negm[i][:], negm_d.v(negm_d.h[i]))
        P.dma("sp", smask[i][:], smask_d.v(smask_d.h[i]))
    nw4 = P.sb("nw4", [64, 512])
    P.dma("sp", nw4[:], nw_d[:])
    cw = P.sb("cw_sb", [128, 3, 5])
    for i in range(3):
        P.dma("sp", cw[:, i, :], cw_d.v(cw_d.h[i]))

    xc = [P.sb("xc%d" % i, [128, TT]) for i in range(3)]
    upad = P.sb("upad", [128, S + 4])
    upc = P.sb("upc", [128, LCTX + 4])
    P.memset("pool", upad[:], 0.0)
    P.memset("pool", upc[:], 0.0)
    for i in range(3):
        P.dma("sp", upad[:, 2:2 + S], raw_d.v(raw_d.h[i, :, 0:S]))
        P.dma("sp", upc[:, 2:2 + LCTX], raw_d.v(raw_d.h[i, :, S:TT]))
        for (src, off, n) in ((upad, 0, S), (upc, S, LCTX)):
            y = xc[i][:, off:off + n]
            P.ts("dve", y, src[:, 0:n], cw[:, i, 0:1], ALU.mult)
            for j in range(1, 5):
                P.stt("dve", y, src[:, j:j + n], cw[:, i, j:j + 1], y, ALU.mult, ALU.add)
            P.act(y, y, AF.Silu)

    al = P.sb("al_sb", [64, 2])
    dtb = P.sb("dtb_sb", [64, 2])
    P.dma("sp", al[:], al_d[:])
    P.dma("sp", dtb[:], dtb_d[:])
    nea = P.sb("nea", [64, 2])
    P.act(nea[:], al[:], AF.Exp)
    P.ts("dve", nea[:], nea[:], -1.0, ALU.mult)
    g_all = [P.sb("g_all%d" % i, [64, 68]) for i in range(2)]
    beta = [P.sb("beta%d" % i, [64, 68]) for i in range(2)]
    nbeta = [P.sb("nbeta%d" % i, [64, 68]) for i in range(2)]
    tmpa = P.sb("tmpa", [64, 68])
    for d_ in range(2):
        P.dma("sp", tmpa[:], a_d.v(a_d.h[d_]))
        P.act(tmpa[:], tmpa[:], AF.Exp, bias=dtb[:, d_:d_ + 1])
        P.act(tmpa[:], tmpa[:], AF.Ln, bias=1.0)
        P.ts("dve", g_all[d_][:], tmpa[:], nea[:, d_:d_ + 1], ALU.mult)
        P.dma("sp", beta[d_][:], b_d.v(b_d.h[d_]))
        P.act(beta[d_][:], beta[d_][:], AF.Sigmoid)
        P.ts("dve", nbeta[d_][:], beta[d_][:], -1.0, ALU.mult)

    import os
    STOP = int(os.environ.get("GDN_STOP", "99"))
    if STOP == 0:
        return P
    ob_store = P.sb("ob_store", [64, 68, 128])
    def t64(n):
        return P.sb(n, [64, 256])
    sqt = P.sb("sqt", [128, 256])
    rn = P.sb("rn", [128, 256])
    qnT = P.sb("qnT", [128, 256])
    knT = P.sb("knT", [128, 256])
    qdT = P.sb("qdT", [128, 256])
    egcb = P.sb("egcb", [128, 256])
    Gt = P.sb("Gt", [64, 4, 192])
    glb = P.sb("glb_sb", [128, 4])
    gct = P.sb("gct_sb", [64, 4])
    egc = P.sb("egc", [64, 4])
    ekd = P.sb("ekd", [64, 4])
    bws = P.sb("bws", [64, 4])
    Dm, decay, decs, A_, aqk, AT, aqkT, Pj, PTj, RT = (t64(n) for n in
                                                     ("Dm", "decay", "decs", "A_", "aqk", "AT", "aqkT", "Pj", "PTj", "RT"))
    ktok = P.sb("ktok", [64, 4, 128])
    vtok = P.sb("vtok", [64, 4, 128])
    bu = P.sb("bu", [64, 4, 128])
    bwk = P.sb("bwk", [64, 4, 128])
    kdec = P.sb("kdec", [64, 4, 128])
    u_sb = P.sb("u_sb", [64, 4, 128])
    wT = P.sb("wT", [128, 256])
    vnew = [P.sb("vnew%d" % i, [64, 128]) for i in range(2)]
    Sst = [P.sb("Sst%d" % i, [128, 128]) for i in range(4)]
    zg = P.sb("zg", [64, 4, 128])
    osum = P.sb("osum", [64, 4, 128])
    sq4 = P.sb("sq4", [64, 4, 128])
    on = [P.sb("on%d" % i, [64, 4, 128]) for i in range(2)]
    sg = P.sb("sg", [64, 4, 128])
    ss4 = P.sb("ss4", [64, 4])
    bk = [P.ps("bank%d" % i, [128, 512]) for i in range(8)]
    half = lambda t, i, nm, rows=128: t.sub(nm, (slice(0, rows), slice(i * 256, (i + 1) * 256)))
    p_D, p_egc = half(bk[0], 0, "p_D", 64), half(bk[0], 1, "p_egc")
    p_QK, p_RP = half(bk[1], 0, "p_QK", 64), half(bk[1], 1, "p_RP", 64)
    p_A, p_B = half(bk[2], 0, "p_A", 64), half(bk[2], 1, "p_B", 64)
    p_wT = half(bk[3], 0, "p_wT")
    p_vn = bk[3].sub("p_vn", (slice(0, 64), slice(256, 384)))
    p_S = bk[3].sub("p_S", (slice(0, 128), slice(384, 512)))
    p_tok = bk[4].sub("p_tok", (slice(0, 64), slice(0, 512)))
    p_u = bk[5]
    p_o = bk[6].sub("p_o", (slice(0, 64), slice(0, 512)))
    p_glb = bk[7].sub("p_glb", (slice(0, 128), slice(0, 4)))
    p_gct = bk[7].sub("p_gct", (slice(0, 64), slice(4, 8)))

    cnt = {"s": 0, "v": 0, "o": 0}
    i64 = ident[0:64, 0:64]

    def c64(j):
        return slice(j * 64, (j + 1) * 64)

    class _Stop(Exception):
        pass

    def ck(n):
        if STOP == n:
            raise _Stop()

    def group(dr, tok0, final):
        c0 = tok0 // 64
        ts_ = slice(tok0, tok0 + 256)
        for (src, dst, sc_) in ((xc[0], qnT, 128.0 ** -0.5), (xc[1], knT, None)):
            P.act(sqt[:], src[:, ts_], AF.Square)
            P.matmul(p_u[:, 0:256], ones[:, :], sqt[:])
            P.ts("dve", rn[:], p_u[:, 0:256], 1e-6, ALU.add)
            P.act(rn[:], rn[:], AF.Sqrt)
            P.recip(rn[:], rn[:])
            if sc_ is not None:
                P.stt("dve", dst[:], src[:, ts_], sc_, rn[:], ALU.mult, ALU.mult)
            else:
                P.tt("dve", dst[:], src[:, ts_], rn[:], ALU.mult)
        ck(10)
        gsl = g_all[dr][:, c0:c0 + 4]
        P.matmul(p_glb[:, :], ones[0:64, :], gsl)
        P.matmul(p_gct[:, :], ctri[dr][:, :], gsl)
        P.act(glb[:], p_glb[:, :], AF.Exp)
        P.act(egc[:], p_gct[:, :], AF.Exp)
        P.copy("dve", gct[:], p_gct[:, :])
        P.tt("dve", ekd[:], p_glb[0:64, :], gct[:], ALU.subtract)
        P.act(ekd[:], ekd[:], AF.Exp)
        P.tt("dve", bws[:], beta[dr][:, c0:c0 + 4], egc[:], ALU.mult)
        ck(11)
        for j in range(4):
            P.ts("dve", Gt[:, j, :], trione[dr][:, :], g_all[dr][:, c0 + j:c0 + j + 1], ALU.mult)
        ck(111)
        for j in range(4):
            P.matmul(p_D[:, c64(j)], ctri[dr][:, :], Gt[:, j, 64:128], start=True, stop=False)
            P.matmul(p_D[:, c64(j)], negones[:, :], Gt[:, j, 0:64], start=False, stop=True)
        ck(112)
        for j in range(4):
            P.matmul(p_egc[:, c64(j)], Gt[:, j, 64:192], ctri[dr][:, :])
        ck(12)
        P.act(egcb[:], p_egc[:, :], AF.Exp)
        P.tt("dve", qdT[:], qnT[:], egcb[:], ALU.mult)
        P.tt("dve", Dm[:], p_D[:, :], negm[dr][:], ALU.add)
        P.act(decay[:], Dm[:], AF.Exp)
        P.tt("pool", decs[:], decay[:], smask[dr][:], ALU.mult)
        ck(13)
        for j in range(4):
            P.matmul(p_D[:, c64(j)], knT[:, c64(j)], knT[:, c64(j)])
            P.matmul(p_QK[:, c64(j)], qnT[:, c64(j)], knT[:, c64(j)])
        for j in range(4):
            P.stt("dve", A_[:, c64(j)], p_D[:, c64(j)], nbeta[dr][:, c0 + j:c0 + j + 1], decs[:, c64(j)], ALU.mult, ALU.mult)
        P.tt("dve", aqk[:], p_QK[:, :], decay[:], ALU.mult)
        for j in range(4):
            P.transpose(p_A[:, c64(j)], A_[:, c64(j)], i64)
            P.transpose(p_B[:, c64(j)], aqk[:, c64(j)], i64)
        P.copy("act", AT[:], p_A[:, :])
        P.copy("dve", aqkT[:], p_B[:, :])
        ck(14)
        P.tt("dve", RT[:], ident4[:], AT[:], ALU.add)
        Pc, PTc = A_, AT
        for lvl in range(1, 6):
            for j in range(4):
                P.matmul(p_A[:, c64(j)], PTc[:, c64(j)], Pc[:, c64(j)])
            if lvl < 5:
                for j in range(4):
                    P.matmul(p_B[:, c64(j)], Pc[:, c64(j)], PTc[:, c64(j)])
            P.copy("act", Pj[:], p_A[:, :])
            if lvl < 5:
                P.copy("dve", PTj[:], p_B[:, :])
            for j in range(4):
                P.matmul(p_RP[:, c64(j)], Pj[:, c64(j)], RT[:, c64(j)])
            P.tt("dve", RT[:], RT[:], p_RP[:, :], ALU.add)
            Pc, PTc = Pj, PTj
        ck(15)
        for (srcT, dst) in ((knT, ktok), (None, vtok)):
            for j in range(4):
                in_ = srcT[:, c64(j)] if srcT is not None else xc[2][:, tok0 + j * 64:tok0 + (j + 1) * 64]
                P.transpose(p_tok[:, j * 128:(j + 1) * 128], in_, ident[:, :])
            P.copy("act", dst[:, :, :], p_tok.v(p_tok.h[:, :].rearrange("p (c d) -> p c d", c=4)))
        ck(16)
        for j in range(4):
            P.ts("dve", bu[:, j, :], vtok[:, j, :], beta[dr][:, c0 + j:c0 + j + 1], ALU.mult)
            P.ts("pool", bwk[:, j, :], ktok[:, j, :], bws[:, j:j + 1], ALU.mult)
            P.ts("pool", kdec[:, j, :], ktok[:, j, :], ekd[:, j:j + 1], ALU.mult)
        for j in range(4):
            P.matmul(p_u[0:64, j * 128:(j + 1) * 128], RT[:, c64(j)], bu[:, j, :])
            P.matmul(p_wT[:, c64(j)], bwk[:, j, :], RT[:, c64(j)])
        P.copy("act", u_sb[:, :, :], p_u.v(p_u.h[0:64, :].rearrange("p (c d) -> p c d", c=4)))
        P.copy("dve", wT[:], p_wT[:, :])
        ck(17)
        need_out = with_ctx or tok0 < S
        if need_out and final:
            P.dma("sp", zg[:, :, :], z_d[:, c0:c0 + 4, :])
        js = list(range(4)) if dr == 0 else [3, 2, 1, 0]
        for j in js:
            S_cur = Sst[cnt["s"] % 4]
            S_nxt = Sst[(cnt["s"] + 1) % 4]
            cnt["s"] += 1
            vn = vnew[cnt["v"] % 2]
            cnt["v"] += 1
            P.matmul(p_vn[:, :], wT[:, c64(j)], S_cur[:, :])
            P.tt("dve", vn[:, :], u_sb[:, j, :], p_vn[:, :], ALU.subtract)
            if need_out:
                P.matmul(p_o[:, j * 128:(j + 1) * 128], qdT[:, c64(j)], S_cur[:, :], start=True, stop=False)
                P.matmul(p_o[:, j * 128:(j + 1) * 128], aqkT[:, c64(j)], vn[:, :], start=False, stop=True)
            P.matmul(p_S[:, :], kdec[:, j, :], vn[:, :])
            P.stt("dve", S_nxt[:, :], S_cur[:, :], glb[:, j:j + 1], p_S[:, :], ALU.mult, ALU.add)
        ck(18)
        if need_out:
            ob3 = p_o.v(p_o.h[:, :].rearrange("p (c d) -> p c d", c=4))
            if not final:
                P.copy("act", ob_store[:, c0:c0 + 4, :], ob3)
            else:
                w_ = cnt["o"] % 2
                cnt["o"] += 1
                P.tt("dve", osum[:, :, :], ob3, ob_store[:, c0:c0 + 4, :], ALU.add)
                P.act(sq4[:, :, :], osum[:, :, :], AF.Square)
                P.op("dve", lambda e, a=ss4[:, :].ap, b_=sq4[:, :, :].ap: e.tensor_reduce(a, b_, AX.X, ALU.add),
                     reads=[sq4], writes=[ss4])
                P.ts("dve", ss4[:, :], ss4[:, :], 1.0 / 128.0, ALU.mult, EPS, ALU.add)
                P.act(ss4[:, :], ss4[:, :], AF.Sqrt)
                P.recip(ss4[:, :], ss4[:, :])
                for jj in range(4):
                    P.ts("dve", on[w_][:, jj, :], osum[:, jj, :], ss4[:, jj:jj + 1], ALU.mult)
                P.tt("pool", on[w_][:, :, :], on[w_][:, :, :], nw4.v(nw4.h[:, :].rearrange("p (c d) -> p c d", c=4)), ALU.mult)
                P.act(sg[:, :, :], zg[:, :, :], AF.Silu)
                P.tt("dve", on[w_][:, :, :], on[w_][:, :, :], sg[:, :, :], ALU.mult)
                P.dma("sp", o_d[:, c0:c0 + 4, :], on[w_][:, :, :])

    for dr, final in ((1, False), (0, True)):
        P.memset("dve", Sst[cnt["s"] % 4][:, :], 0.0)
        try:
            group(dr, S, final)
        except _Stop:
            return P
        if STOP == 1:
            return P
        gl = list(range(16)) if dr == 0 else list(range(15, -1, -1))
        for g_ in gl:
            group(dr, g_ * 256, final)
    if not with_ctx:
        P.memset("dve", on[0][:, :, :], 0.0)
        P.dma("sp", o_d[:, 64:68, :], on[0][:, :, :])
    return P


def gdn_masks():
    a = np.arange(64)
    tf = (a[:, None] <= a[None, :]).astype(np.float32)
    tb = (a[:, None] >= a[None, :]).astype(np.float32)
    ctri = np.stack([tf, tb])
    incl = np.stack([tb, tf])
    strict = np.stack([(a[:, None] > a[None, :]).astype(np.float32), (a[:, None] < a[None, :]).astype(np.float32)])
    negm = np.tile((incl - 1.0) * 1e30, (1, 1, 4)).astype(np.float32)
    smask = np.tile(strict, (1, 1, 4)).astype(np.float32)
    return ctri, negm, smask


def run_GDN(px, pc, conv_w, a_log, dt_bias, norm_w, with_ctx):
    P = build_GDN(with_ctx)
    ident = np.eye(128, dtype=np.float32)
    ctri, negm, smask = gdn_masks()
    nw = np.ascontiguousarray(np.tile(norm_w[None, :], (64, 4)))
    maps = []
    o0 = 4640
    for i in range(NCORES):
        b, h = i // 4, i % 4
        full = np.concatenate([px[b], pc[b]], axis=0)
        raw = np.stack([full[:, o0 + j * 512 + h * 128:o0 + j * 512 + (h + 1) * 128].T for j in range(3)])
        cw = np.stack([conv_w[:, j * 512 + h * 128:j * 512 + (h + 1) * 128].T for j in range(3)])
        z = full[:, 6176 + h * 128:6176 + (h + 1) * 128]
        a = np.stack([full[:, 6688 + d_ * 4 + h].reshape(68, 64).T for d_ in range(2)])
        bb = np.stack([full[:, 6696 + d_ * 4 + h].reshape(68, 64).T for d_ in range(2)])
        maps.append({"raw": np.ascontiguousarray(raw), "cw": np.ascontiguousarray(cw), "z": chunk_tm(z),
                     "a": np.ascontiguousarray(a), "bb": np.ascontiguousarray(bb),
                     "alog": np.ascontiguousarray(np.tile(a_log[None, :, h], (64, 1))),
                     "dtb": np.ascontiguousarray(np.tile(dt_bias[None, :, h], (64, 1))),
                     "ctri": ctri, "negm": negm, "smask": smask, "nw": nw, "ident": ident})
    res = run_prog(P, maps)
    ox = np.zeros((B, S, 512), np.float32)
    oc = np.zeros((B, LCTX, 512), np.float32)
    for i, r in enumerate(res):
        b, h = i // 4, i % 4
        o = unchunk_tm(r["o"])
        ox[b, :, h * 128:(h + 1) * 128] = o[:S]
        oc[b, :, h * 128:(h + 1) * 128] = o[S:]
    return ox, oc


class _Scope:
    def __init__(self, P):
        self.P = P

    def __enter__(self):
        self.old = self.P.es
        self.P.es = ExitStack()
        return self

    def __exit__(self, *a):
        _barrier(self.P)
        self.P.es.close()
        self.P.es = self.old
        return False


Prog.scope = lambda self: _Scope(self)


def build_L3():
    P = Prog()
    cat = P.dram("cat", [NTOK1, D], F32, "ExternalInput")
    hin = P.dram("h", [NTOK1, D], F32, "ExternalInput")
    vlat = P.dram("vlat", [3, D], F32, "ExternalInput")
    vctx = P.dram("vctx", [3, D], F32, "ExternalInput")
    gat = P.dram("gate", [2, D], F32, "ExternalInput")
    w = P.dram("w", [D, D], F32, "ExternalInput")
    wr_d = P.dram("wr", [D, 16], F32, "ExternalInput")
    idn = P.dram("ident", [128, 128], F32, "ExternalInput")
    h1_o = P.dram("h1", [NTOK1, D], F32, "ExternalOutput")
    h2_o = P.dram("h2", [NTOK1, D], F32, "ExternalOutput")
    aff_o = P.dram("aff", [NTOK1, 16], F32, "ExternalOutput")
    ident = P.sb("ident_sb", [128, 128])
    P.dma("sp", ident[:], idn[:])
    A_l, B_l, A_c, B_c = (P.sb(n, [128, D]) for n in ("A_l", "B_l", "A_c", "B_c"))
    load_mod_vectors(P, vlat, A_l, B_l)
    load_mod_vectors(P, vctx, A_c, B_c)
    G_l, G_c = P.sb("G_l", [128, D]), P.sb("G_c", [128, D])
    P.dma("sp", G_l[:], gat.v(gat.h[0:1, :].to_broadcast([128, D])))
    P.dma("sp", G_c[:], gat.v(gat.h[1:2, :].to_broadcast([128, D])))
    wsb = P.sb("wsb", [128, 16, D], MMDT)
    wv = w.h.rearrange("(k p) n -> p k n", p=128)
    for kk in range(0, 16, 2):
        P.dma("pool", wsb[:, kk:kk + 2, :], w.v(wv[:, kk:kk + 2, :]))
    wr = P.sb("wr_sb", [128, 16, 16])
    P.dma("sp", wr[:], wr_d.v(wr_d.h.rearrange("(k p) n -> p k n", p=128)))
    ct = [P.sb("ct%d" % i, [128, D]) for i in range(2)]
    ht = [P.sb("ht%d" % i, [128, D]) for i in range(2)]
    catT = P.sb("catT", [128, 16, 128], MMDT)
    h1 = P.sb("h1_sb", [128, D])
    h2 = P.sb("h2_sb", [128, D])
    h2T = P.sb("h2T", [128, 16, 128])
    tmp = P.sb("tmp", [128, 512])
    ss = P.sb("ss", [128, 1])
    rstd = P.sb("rstd", [128, 1])
    lg = P.sb("lg", [128, 16])
    st = P.sb("st", [128, 4])
    pst = [P.ps("ps%d" % i, [128, 512]) for i in range(8)]
    tiles = [(i * 128, 128) for i in range(8)] + [(1024, 64)]
    for ti, (t0, rows) in enumerate(tiles):
        lat = ti < 8
        c_t, h_t = ct[ti % 2], ht[ti % 2]
        P.dma("sp", c_t[0:rows, :], cat[t0:t0 + rows, :])
        P.dma("sp", h_t[0:rows, :], hin[t0:t0 + rows, :])
        transpose_to_fm(P, c_t, rows, catT, 0, ident, pst[0:2], 0)
        G = G_l if lat else G_c
        for nb in range(4):
            pt = pst[2 + nb % 2]
            for k in range(16):
                P.matmul(pt[0:rows, :], catT[:, k, 0:rows], wsb[:, k, nb * 512:(nb + 1) * 512], start=(k == 0), stop=(k == 15))
            P.tt("dve", tmp[0:rows, :], pt[0:rows, :], G[0:rows, nb * 512:(nb + 1) * 512], ALU.mult)
            P.tt("pool", h1[0:rows, nb * 512:(nb + 1) * 512], tmp[0:rows, :], h_t[0:rows, nb * 512:(nb + 1) * 512], ALU.add)
        P.dma("sp", h1_o[t0:t0 + rows, :], h1[0:rows, :])
        rms_modulate(P, h1, rows, A_l if lat else A_c, B_l if lat else B_c, h2, ss, rstd)
        P.dma("sp", h2_o[t0:t0 + rows, :], h2[0:rows, :])
        transpose_to_fm(P, h2, rows, h2T, 0, ident, pst[4:6], 0)
        pl = pst[6]
        for k in range(16):
            P.matmul(pl[0:rows, 0:16], h2T[:, k, 0:rows], wr[:, k, :], start=(k == 0), stop=(k == 15))
        mx, nmx, sm, rs = (st[0:rows, j:j + 1] for j in range(4))
        P.op("dve", lambda e, a=mx.ap, b_=pl[0:rows, 0:16].ap: e.reduce_max(a, b_, AX.X), reads=[pl], writes=[st])
        P.ts("dve", nmx, mx, -1.0, ALU.mult)
        P.memset("dve", sm, 0.0)
        P.act(lg[0:rows, :], pl[0:rows, 0:16], AF.Exp, bias=nmx, accum=sm)
        P.recip(rs, sm)
        P.ts("dve", lg[0:rows, :], lg[0:rows, :], rs, ALU.mult)
        P.dma("sp", aff_o[t0:t0 + rows, :], lg[0:rows, :])
    return P


def run_L3(catx, catc, hx, hc, mod_l, norm_w, w_out, w_router):
    P = build_L3()
    ident = np.eye(128, dtype=np.float32)
    maps = []
    for i in range(NCORES):
        b = i // 4
        vlat = np.stack([norm_w, mod_l[b, 4 * D:5 * D], mod_l[b, 3 * D:4 * D]])
        vctx = np.stack([norm_w, mod_l[2, 4 * D:5 * D], mod_l[2, 3 * D:4 * D]])
        gate = np.stack([mod_l[b, 2 * D:3 * D], mod_l[2, 2 * D:3 * D]])
        maps.append({"cat": np.ascontiguousarray(tok_shard(catx, catc, i)), "h": np.ascontiguousarray(tok_shard(hx, hc, i)),
                     "vlat": vlat, "vctx": vctx, "gate": gate, "w": w_out, "wr": w_router, "ident": ident})
    res = run_prog(P, maps)
    h1x, h1c = tok_unshard([r["h1"] for r in res], D)
    h2x, h2c = tok_unshard([r["h2"] for r in res], D)
    afx, afc = tok_unshard([r["aff"] for r in res], 16)
    return h1x, h1c, h2x, h2c, afx, afc


CAPX = 512
CAPC = 32


def build_L4(with_ctx):
    P = Prog()
    NTK = 2 * CAPX + (2 * CAPC if with_ctx else 0)
    affx = P.dram("affx", [4, S], F32, "ExternalInput")
    affc = P.dram("affc", [4, LCTX], F32, "ExternalInput")
    h2x = P.dram("h2x", [B, S, D], F32, "ExternalInput")
    h2c = P.dram("h2c", [B, LCTX, D], F32, "ExternalInput")
    wg_d = P.dram("wg", [2, D, D], F32, "ExternalInput")
    wu_d = P.dram("wu", [2, D, D], F32, "ExternalInput")
    wd_d = P.dram("wd", [2, D, D], F32, "ExternalInput")
    idn = P.dram("ident", [128, 128], F32, "ExternalInput")
    accx = P.dram("accx", [B, S, D], F32, "ExternalOutput")
    accc = P.dram("accc", [B, LCTX, D], F32, "ExternalOutput")
    gsc = P.dram("gsc", [4, CAPX], F32, "Internal")
    isc = P.dram("isc", [4, CAPX], U32, "Internal")
    gscc = P.dram("gscc", [4, CAPC], F32, "Internal")
    iscc = P.dram("iscc", [4, CAPC], U32, "Internal")

    ident = P.sb("ident_sb", [128, 128])
    P.dma("sp", ident[:], idn[:])
    gcol = P.sb("gcol", [128, 4, 4])
    icol = P.sb("icol", [128, 4, 4], U32)
    gcolc = P.sb("gcolc", [32, 4])
    icolc = P.sb("icolc", [32, 4], U32)
    zero = P.sb("zero", [128, 2, D])
    P.memset("pool", zero[:], 0.0)
    for b in range(B):
        av = accx.h[b].rearrange("(n p j) d -> n p j d", p=128, j=2)
        for n in range(S // 256):
            P.dma("sp", accx.v(av[n]), zero[:])
        P.dma("sp", accc.v(accc.h[b].rearrange("(p j) d -> p j d", j=2)), zero[:])
    with P.scope():
        W = P.sb("topk_w", [4, S])
        gt = P.sb("topk_g", [4, CAPX])
        it_ = P.sb("topk_i", [4, CAPX], U32)
        P.dma("sp", W[:], affx[:])
        for i in range(CAPX // 8):
            sl = slice(i * 8, (i + 1) * 8)
            P.op("dve", lambda e, o=gt[:, sl].ap, a=W[:].ap: e.max(o, a), reads=[W], writes=[gt])
            P.op("dve", lambda e, o=it_[:, sl].ap, m=gt[:, sl].ap, a=W[:].ap: e.max_index(o, m, a), reads=[W, gt], writes=[it_])
            P.op("dve", lambda e, o=W[:].ap, m=gt[:, sl].ap, a=W[:].ap: e.match_replace(o, m, a, -1.0), reads=[gt], writes=[W])
        P.dma("sp", gsc[:], gt[:])
        P.dma("sp", isc[:], it_[:])
        P.dma("sp", gcol[:], gsc.v(gsc.h.rearrange("r (t p) -> p r t", p=128)), allow_slow_non_contiguous=True)
        P.dma("sp", icol[:], isc.v(isc.h.rearrange("r (t p) -> p r t", p=128)), allow_slow_non_contiguous=True)
        if with_ctx:
            P.dma("sp", W[:, 0:LCTX], affc[:])
            for i in range(CAPC // 8):
                sl = slice(i * 8, (i + 1) * 8)
                P.op("dve", lambda e, o=gt[:, sl].ap, a=W[:, 0:LCTX].ap: e.max(o, a), reads=[W], writes=[gt])
                P.op("dve", lambda e, o=it_[:, sl].ap, m=gt[:, sl].ap, a=W[:, 0:LCTX].ap: e.max_index(o, m, a), reads=[W, gt], writes=[it_])
                P.op("dve", lambda e, o=W[:, 0:LCTX].ap, m=gt[:, sl].ap, a=W[:, 0:LCTX].ap: e.match_replace(o, m, a, -1.0), reads=[gt], writes=[W])
            P.dma("sp", gscc[:], gt[:, 0:CAPC])
            P.dma("sp", iscc[:], it_[:, 0:CAPC])
            P.dma("sp", gcolc[:], gscc.v(gscc.h.rearrange("r p -> p r")), allow_slow_non_contiguous=True)
            P.dma("sp", icolc[:], iscc.v(iscc.h.rearrange("r p -> p r")), allow_slow_non_contiguous=True)
    xsT = P.sb("xsT", [128, 16, NTK], MMDT)
    hidT = P.sb("hidT", [128, 16, NTK], MMDT)
    wbuf = [P.sb("wbuf%d" % i, [128, 16, 512], MMDT) for i in range(4)]
    xs = [P.sb("xs%d" % i, [128, D]) for i in range(2)]
    ysb = [P.sb("ysb%d" % i, [128, D]) for i in range(2)]
    gsb = P.sb("gsb", [128, 512])
    pst = [P.ps("ps%d" % i, [128, 512]) for i in range(8)]
    acc_bufs = [Tile(accx.h.rearrange("b s d -> (b s) d"), "accx%d" % b) for b in range(B)]
    accc_bufs = [Tile(accc.h.rearrange("b s d -> (b s) d"), "accc%d" % b) for b in range(B)]
    h2xf = h2x.v(h2x.h.rearrange("b s d -> (b s) d"))
    h2cf = h2c.v(h2c.h.rearrange("b s d -> (b s) d"))
    blocks = [(0, 512), (512, 512)] + ([(1024, 64)] if with_ctx else [])
    cnt = {"x": 0, "w": 0, "y": 0}
    for el in range(2):
        for b in range(B):
            r = el * 2 + b
            for t in range(4):
                x_ = xs[cnt["x"] % 2]
                cnt["x"] += 1
                P.idma(x_[:, :], None, h2xf, (icol[:, r, t:t + 1], 0), element_offset=b * S * D)
                transpose_to_fm(P, x_, 128, xsT, b * 512 + t * 128, ident, pst[0:2], 0)
            if with_ctx:
                x_ = xs[cnt["x"] % 2]
                cnt["x"] += 1
                P.idma(x_[0:32, :], None, h2cf, (icolc[:, r:r + 1], 0), element_offset=b * LCTX * D)
                transpose_to_fm(P, x_, 32, xsT, 1024 + b * 32, ident, pst[0:2], 0)
        wgv = wg_d.h[el].rearrange("(k p) n -> p k n", p=128)
        wuv = wu_d.h[el].rearrange("(k p) n -> p k n", p=128)
        for fb in range(4):
            wg_t = wbuf[cnt["w"] % 4]
            wu_t = wbuf[(cnt["w"] + 1) % 4]
            cnt["w"] += 2
            for kk in range(0, 16, 4):
                P.dma("pool", wg_t[:, kk:kk + 4, :], wg_d.v(wgv[:, kk:kk + 4, fb * 512:(fb + 1) * 512]))
                P.dma("pool", wu_t[:, kk:kk + 4, :], wu_d.v(wuv[:, kk:kk + 4, fb * 512:(fb + 1) * 512]))
            for fi in range(4):
                ft = fb * 4 + fi
                for bi, (c0, cn) in enumerate(blocks):
                    pg = pst[2 + (bi % 2) * 2]
                    pu = pst[3 + (bi % 2) * 2]
                    for k in range(16):
                        P.matmul(pg[:, 0:cn], wg_t[:, k, fi * 128:(fi + 1) * 128], xsT[:, k, c0:c0 + cn], start=(k == 0), stop=(k == 15))
                    for k in range(16):
                        P.matmul(pu[:, 0:cn], wu_t[:, k, fi * 128:(fi + 1) * 128], xsT[:, k, c0:c0 + cn], start=(k == 0), stop=(k == 15))
                    P.act(gsb[:, 0:cn], pg[:, 0:cn], AF.Silu)
                    P.tt("dve", hidT[:, ft, c0:c0 + cn], gsb[:, 0:cn], pu[:, 0:cn], ALU.mult)
        wdv = wd_d.h[el].rearrange("(k p) n -> p k n", p=128)
        for db in range(4):
            for kk in range(0, 16, 4):
                P.dma("pool", wbuf[db][:, kk:kk + 4, :], wd_d.v(wdv[:, kk:kk + 4, db * 512:(db + 1) * 512]))
        cnt["w"] = 0
        ctiles = [(b, t, b * 512 + t * 128, 128) for b in range(B) for t in range(4)]
        if with_ctx:
            ctiles += [(b, None, 1024 + b * 32, 32) for b in range(B)]
        for (b, t, c0, rows) in ctiles:
            r = el * 2 + b
            y_ = ysb[cnt["y"] % 2]
            for db in range(4):
                pt = pst[6 + db % 2]
                for ft in range(16):
                    P.matmul(pt[0:rows, :], hidT[:, ft, c0:c0 + rows], wbuf[db][:, ft, :], start=(ft == 0), stop=(ft == 15))
                gv = gcol[:, r, t:t + 1] if t is not None else gcolc[:, r:r + 1]
                if db % 2 == 0:
                    P.ts("dve", y_[0:rows, db * 512:(db + 1) * 512], pt[0:rows, :], gv, ALU.mult)
                else:
                    P.act(y_[0:rows, db * 512:(db + 1) * 512], pt[0:rows, :], AF.Copy, scale=gv)
            cnt["y"] += 1
            if t is not None:
                P.idma(acc_bufs[b][:, :], (icol[:, r, t:t + 1], 0), y_[0:rows, :], None, compute_op=ALU.add, element_offset=b * S * D)
            else:
                P.idma(accc_bufs[b][:, :], (icolc[:, r:r + 1], 0), y_[0:rows, :], None, compute_op=ALU.add, element_offset=b * LCTX * D)
    return P


def run_L4(afx, afc, h2x, h2c, wg, wu, wd, with_ctx):
    P = build_L4(with_ctx)
    ident = np.eye(128, dtype=np.float32)
    maps = []
    for i in range(NCORES):
        ax = np.stack([afx[b, :, 2 * i + el] for el in range(2) for b in range(B)])
        ac = np.stack([afc[b, :, 2 * i + el] for el in range(2) for b in range(B)])
        maps.append({"affx": np.ascontiguousarray(ax), "affc": np.ascontiguousarray(ac), "h2x": h2x, "h2c": h2c,
                     "wg": wg[2 * i:2 * i + 2], "wu": wu[2 * i:2 * i + 2], "wd": wd[2 * i:2 * i + 2], "ident": ident})
    res = run_prog(P, maps)
    return np.stack([r["accx"] for r in res]), np.stack([r["accc"] for r in res])


def build_L5(final):
    P = Prog()
    parts = P.dram("parts", [NCORES, NTOK1, D], F32, "ExternalInput")
    h1 = P.dram("h1", [NTOK1, D], F32, "ExternalInput")
    gat = P.dram("gate", [2, D], F32, "ExternalInput")
    fw = P.dram("fw", [1, D], F32, "ExternalInput")
    out = P.dram("out", [NTOK1, D], F32, "ExternalOutput")
    G_l, G_c = P.sb("G_l", [128, D]), P.sb("G_c", [128, D])
    P.dma("sp", G_l[:], gat.v(gat.h[0:1, :].to_broadcast([128, D])))
    P.dma("sp", G_c[:], gat.v(gat.h[1:2, :].to_broadcast([128, D])))
    FW = P.sb("FW", [128, D])
    P.dma("sp", FW[:], fw.v(fw.h[0:1, :].to_broadcast([128, D])))
    pt = [P.sb("pt%d" % i, [128, D]) for i in range(3)]
    acc = [P.sb("acc%d" % i, [128, D]) for i in range(2)]
    ht = [P.sb("ht%d" % i, [128, D]) for i in range(2)]
    yt = P.sb("yt", [128, D])
    ss = P.sb("ss", [128, 1])
    rstd = P.sb("rstd", [128, 1])
    tiles = [(i * 128, 128) for i in range(8)] + [(1024, 64)]
    n = 0
    for ti, (t0, rows) in enumerate(tiles):
        a = acc[ti % 2]
        h_t = ht[ti % 2]
        P.dma("sp", h_t[0:rows, :], h1[t0:t0 + rows, :])
        P.dma("sp", a[0:rows, :], parts.v(parts.h[0, t0:t0 + rows, :]))
        for c in range(1, NCORES):
            p_ = pt[n % 3]
            n += 1
            P.dma("sp", p_[0:rows, :], parts.v(parts.h[c, t0:t0 + rows, :]))
            P.tt("dve" if c % 2 else "pool", a[0:rows, :], a[0:rows, :], p_[0:rows, :], ALU.add)
        G = G_l if ti < 8 else G_c
        P.tt("dve", a[0:rows, :], a[0:rows, :], G[0:rows, :], ALU.mult)
        P.tt("pool", a[0:rows, :], a[0:rows, :], h_t[0:rows, :], ALU.add)
        if final:
            rms_modulate(P, a, rows, FW, None, yt, ss, rstd)
            P.dma("sp", out[t0:t0 + rows, :], yt[0:rows, :])
        else:
            P.dma("sp", out[t0:t0 + rows, :], a[0:rows, :])
    return P


def run_L5(partx, partc, h1x, h1c, mod_l, final_w, final):
    P = build_L5(final)
    maps = []
    for i in range(NCORES):
        b = i // 4
        gate = np.stack([mod_l[b, 5 * D:6 * D], mod_l[2, 5 * D:6 * D]])
        parts = np.stack([tok_shard(partx[c], partc[c], i) for c in range(NCORES)])
        maps.append({"parts": np.ascontiguousarray(parts), "h1": np.ascontiguousarray(tok_shard(h1x, h1c, i)),
                     "gate": gate, "fw": np.ascontiguousarray(final_w[None, :])})
    res = run_prog(P, maps)
    return tok_unshard([r["out"] for r in res], D)


def kernel(x, c, ctx, c_ctx, w_ada, b_ada, norm_mix_w, norm_ffn_w, w_in, w_out, na_rpb,
           gla_gate_w, gla_gate_b, gla_norm_w, gdn_conv_w, gdn_a_log, gdn_dt_bias, gdn_norm_w,
           w_router, w_exp_gate, w_exp_up, w_exp_down, final_norm_w):
    f = lambda a: np.asarray(a, dtype=np.float32)
    x, c, ctx, c_ctx = f(x), f(c), f(ctx), f(c_ctx)
    mod = run_L0({"c": c, "c_ctx": c_ctx, "w_ada": f(w_ada), "b_ada": f(b_ada)})
    hx, hc = x, ctx
    for l in range(DEPTH):
        ctx_out = l < DEPTH - 1
        px, pc = run_L1(hx, hc, mod[l], f(norm_mix_w)[l], f(w_in)[l])
        ox_na, oc_na = run_NA(px, pc, f(na_rpb)[l], ctx_out)
        gx, gc = run_GLA(px, pc, f(gla_gate_w)[l], f(gla_gate_b)[l], f(gla_norm_w)[l], ctx_out)
        dx, dc = run_GDN(px, pc, f(gdn_conv_w)[l], f(gdn_a_log)[l], f(gdn_dt_bias)[l], f(gdn_norm_w)[l], ctx_out)
        catx = np.concatenate([ox_na, gx, dx], axis=-1)
        catc = np.concatenate([oc_na, gc, dc], axis=-1)
        h1x, h1c, h2x, h2c, afx, afc = run_L3(catx, catc, hx, hc, mod[l], f(norm_ffn_w)[l], f(w_out)[l], f(w_router)[l])
        partx, partc = run_L4(afx, afc, h2x, h2c, f(w_exp_gate)[l], f(w_exp_up)[l], f(w_exp_down)[l], ctx_out)
        hx, hc = run_L5(partx, partc, h1x, h1c, mod[l], f(final_norm_w), l == DEPTH - 1)
    return hx
```

```python
import numpy as np
from contextlib import ExitStack
import concourse.bass as bass
import concourse.mybir as mybir
from concourse.bass_utils import run_bass_kernel_spmd

F32 = mybir.dt.float32
BF16 = mybir.dt.bfloat16
I32 = mybir.dt.int32
U32 = mybir.dt.uint32
AF = mybir.ActivationFunctionType
ALU = mybir.AluOpType
AX = mybir.AxisListType

NCORES = 8
D = 2048
B = 2
S = 4096
LCTX = 256
DEPTH = 2
IN_W = 6704
EPS = 1e-6
NDMA = 24


class Buf:
    __slots__ = ("w", "r", "name", "excl")

    def __init__(self, name):
        self.w = None
        self.r = {}
        self.name = name
        self.excl = False


class View:
    __slots__ = ("t", "ap")

    def __init__(self, t, ap):
        self.t = t
        self.ap = ap


class Tile:
    def __init__(self, handle, name):
        self.h = handle
        self.buf = Buf(name)

    def __getitem__(self, idx):
        return View(self.buf, self.h[idx])

    def v(self, ap):
        return View(self.buf, ap)

    def sub(self, name, idx):
        t = Tile(self.h[idx], name)
        t.buf = self.buf
        return t


class Prog:
    def __init__(self):
        self.nc = bass.Bass("TRN2", target_bir_lowering=False)
        nc = self.nc
        self.es = ExitStack()
        self.engs = {"pe": nc.tensor, "act": nc.scalar, "dve": nc.vector, "pool": nc.gpsimd, "sp": nc.sync}
        self.esem = {e: self.es.enter_context(nc.semaphore("s_" + e)) for e in ("pe", "act", "dve", "pool")}
        self.ecnt = {e: 0 for e in self.esem}
        self.dsems = [self.es.enter_context(nc.semaphore("d%d" % i)) for i in range(NDMA)]
        self.dval = [0] * NDMA
        self.dnext = 0
        self.seen = {e: {} for e in self.engs}
        self.nins = 0
        self._psum = None

    def dram(self, name, shape, dt, kind):
        t = self.nc.dram_tensor(name, list(shape), dt, kind=kind)
        return Tile(t.ap(), name)

    def sb(self, name, shape, dt=F32):
        h = self.es.enter_context(self.nc.sbuf_tensor(name, list(shape), dt))
        return Tile(h, name)

    def ps(self, name, shape, dt=F32):
        h = self.es.enter_context(self.nc.psum_tensor(name, list(shape), dt))
        t = Tile(h, name)
        t.buf.excl = True
        return t

    def _wait(self, eng, evs):
        seen = self.seen[eng]
        best = {}
        for ev in evs:
            if ev is None:
                continue
            k, sem, val = ev
            if eng == "pe" and k == "e:pe":
                continue
            if seen.get(k, 0) >= val:
                continue
            if k not in best or best[k][2] < val:
                best[k] = ev
        for k, (kk, sem, val) in best.items():
            self.engs[eng].wait_ge(sem, val)
            seen[k] = val
            self.nins += 1

    @staticmethod
    def _deps(reads, writes):
        evs = []
        for b in reads:
            if b.w is not None:
                evs.append(b.w)
            if b.excl:
                evs.extend(b.r.values())
        for b in writes:
            if b.w is not None:
                evs.append(b.w)
            evs.extend(b.r.values())
        return evs

    @staticmethod
    def _mark(ev, reads, writes):
        k = ev[0]
        for b in reads:
            b.r[k] = ev
        for b in writes:
            b.w = ev
            b.r = {}

    def op(self, eng, fn, reads=(), writes=()):
        reads = [v.t if isinstance(v, View) else (v.buf if isinstance(v, Tile) else v) for v in reads]
        writes = [v.t if isinstance(v, View) else (v.buf if isinstance(v, Tile) else v) for v in writes]
        self._wait(eng, self._deps(reads, writes))
        ins = fn(self.engs[eng])
        self.ecnt[eng] += 1
        ins.then_inc(self.esem[eng], 1)
        ev = ("e:" + eng, self.esem[eng], self.ecnt[eng])
        self._mark(ev, reads, writes)
        self.nins += 1
        return ev

    def dma(self, q, out, in_, **kw):
        i = self.dnext
        self.dnext = (i + 1) % NDMA
        reads, writes = [in_.t], [out.t]
        evs = self._deps(reads, writes)
        if self.dval[i] > 0:
            evs.append(("d:%d" % i, self.dsems[i], self.dval[i]))
        self._wait(q, evs)
        ins = self.engs[q].dma_start(out=out.ap, in_=in_.ap, **kw)
        self.dval[i] += 16
        ins.then_inc(self.dsems[i], 16)
        ev = ("d:%d" % i, self.dsems[i], self.dval[i])
        self._mark(ev, reads, writes)
        self.nins += 1
        return ev

    def idma(self, out, out_off, in_, in_off, extra_reads=(), **kw):
        i = self.dnext
        self.dnext = (i + 1) % NDMA
        reads, writes = [in_.t] + [v.t for v in extra_reads], [out.t]
        evs = self._deps(reads, writes)
        if self.dval[i] > 0:
            evs.append(("d:%d" % i, self.dsems[i], self.dval[i]))
        self._wait("pool", evs)
        oo = bass.IndirectOffsetOnAxis(ap=out_off[0].ap, axis=out_off[1]) if out_off is not None else None
        io = bass.IndirectOffsetOnAxis(ap=in_off[0].ap, axis=in_off[1]) if in_off is not None else None
        ins = self.nc.gpsimd.indirect_dma_start(out=out.ap, out_offset=oo, in_=in_.ap, in_offset=io, **kw)
        self.dval[i] += 16
        ins.then_inc(self.dsems[i], 16)
        ev = ("d:%d" % i, self.dsems[i], self.dval[i])
        self._mark(ev, reads, writes)
        self.nins += 1
        return ev

    def finish(self):
        evs = [("d:%d" % i, self.dsems[i], self.dval[i]) for i in range(NDMA) if self.dval[i] > 0]
        evs += [("e:" + e, self.esem[e], self.ecnt[e]) for e in self.esem if self.ecnt[e] > 0]
        self._wait("sp", evs)
        self.es.close()
        return self.nc

    def matmul(self, out, lhsT, rhs, start=True, stop=True):
        return self.op("pe", lambda e: e.matmul(out.ap, lhsT.ap, rhs.ap, start=start, stop=stop),
                       reads=[lhsT, rhs], writes=[out])

    def transpose(self, out, in_, ident):
        return self.op("pe", lambda e: e.transpose(out.ap, in_.ap, ident.ap), reads=[in_, ident], writes=[out])

    def act(self, out, in_, func, bias=None, scale=None, accum=None, eng="act"):
        kw = {}
        reads = [in_]
        writes = [out]
        if bias is not None:
            if isinstance(bias, View):
                kw["bias"] = bias.ap
                reads.append(bias)
            else:
                kw["bias"] = bias
        if scale is not None:
            if isinstance(scale, View):
                kw["scale"] = scale.ap
                reads.append(scale)
            else:
                kw["scale"] = scale
        if accum is not None:
            kw["accum_out"] = accum.ap
            writes.append(accum)
        return self.op("act", lambda e: e.activation(out.ap, in_.ap, func, **kw), reads=reads, writes=writes)

    def copy(self, eng, out, in_):
        if eng == "act":
            return self.op("act", lambda e: e.copy(out.ap, in_.ap), reads=[in_], writes=[out])
        return self.op(eng, lambda e: e.tensor_copy(out.ap, in_.ap), reads=[in_], writes=[out])

    def tt(self, eng, out, a, b, op):
        return self.op(eng, lambda e: e.tensor_tensor(out.ap, a.ap, b.ap, op), reads=[a, b], writes=[out])

    def ts(self, eng, out, a, s1, op0, s2=None, op1=None, accum=None):
        reads = [a]
        writes = [out]
        s1a = s1.ap if isinstance(s1, View) else s1
        s2a = s2.ap if isinstance(s2, View) else s2
        if isinstance(s1, View):
            reads.append(s1)
        if isinstance(s2, View):
            reads.append(s2)
        kw = {}
        if op1 is not None:
            kw["op1"] = op1
        if accum is not None:
            kw["accum_out"] = accum.ap
            writes.append(accum)
        return self.op(eng, lambda e: e.tensor_scalar(out.ap, a.ap, s1a, s2a, op0, **kw), reads=reads, writes=writes)

    def stt(self, eng, out, a, s, b, op0, op1):
        reads = [a, b]
        sa = s.ap if isinstance(s, View) else s
        if isinstance(s, View):
            reads.append(s)
        return self.op(eng, lambda e: e.scalar_tensor_tensor(out.ap, a.ap, sa, b.ap, op0, op1), reads=reads, writes=[out])

    def recip(self, out, in_):
        return self.op("dve", lambda e: e.reciprocal(out.ap, in_.ap), reads=[in_], writes=[out])

    def memset(self, eng, out, val):
        return self.op(eng, lambda e: e.memset(out.ap, val), reads=[], writes=[out])


def run_prog(prog, in_maps):
    nc = prog.finish()
    res = run_bass_kernel_spmd(nc, in_maps, core_ids=list(range(NCORES)))
    return res.results


MODC = 6 * D // NCORES


def build_L0():
    P = Prog()
    cT = P.dram("cT", [128, 16, 3], F32, "ExternalInput")
    w = P.dram("w", [DEPTH, D, MODC], F32, "ExternalInput")
    bb = P.dram("b", [DEPTH, MODC], F32, "ExternalInput")
    out = P.dram("out", [DEPTH, 3, MODC], F32, "ExternalOutput")
    c_sb = P.sb("c_sb", [128, 16, 3])
    sc = P.sb("sc", [128, 16, 3])
    P.dma("sp", c_sb[:], cT[:])
    P.act(sc[:], c_sb[:], AF.Silu)
    wt = [P.sb("wt%d" % i, [128, 16, 512]) for i in range(2)]
    bt = [P.sb("bt%d" % i, [3, 512]) for i in range(2)]
    ot = [P.sb("ot%d" % i, [3, 512]) for i in range(2)]
    pt = [P.ps("pt%d" % i, [128, 512]) for i in range(2)]
    it = 0
    for l in range(DEPTH):
        wv = w.h[l].rearrange("(k p) n -> p k n", p=128)
        for j in range(MODC // 512):
            s = it % 2
            P.dma("sp", wt[s][:], w.v(wv[:, :, j * 512:(j + 1) * 512]))
            for r in range(3):
                P.dma("sp", bt[s][r:r + 1, :], bb.v(bb.h[l:l + 1, j * 512:(j + 1) * 512]))
            for k in range(16):
                P.matmul(pt[s][0:3, :], sc[:, k, :], wt[s][:, k, :], start=(k == 0), stop=(k == 15))
            P.tt("dve", ot[s][:], pt[s][0:3, :], bt[s][:], ALU.add)
            P.dma("sp", out.v(out.h[l, :, j * 512:(j + 1) * 512]), ot[s][:])
            it += 1
    return P


def run_L0(inp):
    c_all = np.concatenate([inp["c"], inp["c_ctx"][None, :]], axis=0).astype(np.float32)
    cT = np.ascontiguousarray(c_all.reshape(3, 16, 128).transpose(2, 1, 0))
    P = build_L0()
    maps = []
    for i in range(NCORES):
        maps.append({
            "cT": cT,
            "w": np.ascontiguousarray(inp["w_ada"][:, :, i * MODC:(i + 1) * MODC]),
            "b": np.ascontiguousarray(inp["b_ada"][:, i * MODC:(i + 1) * MODC]),
        })
    res = run_prog(P, maps)
    return np.concatenate([r["out"] for r in res], axis=2)


def _barrier(P):
    evs = [("d:%d" % i, P.dsems[i], P.dval[i]) for i in range(NDMA) if P.dval[i] > 0]
    evs += [("e:" + e, P.esem[e], P.ecnt[e]) for e in P.esem if P.ecnt[e] > 0]
    for e in P.engs:
        P._wait(e, evs)


Prog.barrier = _barrier

MMDT = BF16
NTOK1 = 1024 + 64


def rms_modulate(P, x_t, rows, A, Bt, y_t, ss, rstd):
    P.memset("dve", ss[0:rows, :], 0.0)
    P.act(y_t[0:rows, :], x_t[0:rows, :], AF.Square, accum=ss[0:rows, :])
    P.ts("dve", rstd[0:rows, :], ss[0:rows, :], 1.0 / D, ALU.mult, EPS, ALU.add)
    P.act(rstd[0:rows, :], rstd[0:rows, :], AF.Sqrt)
    P.recip(rstd[0:rows, :], rstd[0:rows, :])
    P.stt("dve", y_t[0:rows, :], x_t[0:rows, :], rstd[0:rows, 0:1], A[0:rows, :], ALU.mult, ALU.mult)
    if Bt is not None:
        P.tt("dve", y_t[0:rows, :], y_t[0:rows, :], Bt[0:rows, :], ALU.add)


def load_mod_vectors(P, vec, A, Bt):
    tmp = P.sb("tmpv_" + A.buf.name, [128, D])
    P.dma("sp", A[:], vec.v(vec.h[0:1, :].to_broadcast([128, D])))
    P.dma("sp", tmp[:], vec.v(vec.h[1:2, :].to_broadcast([128, D])))
    P.dma("sp", Bt[:], vec.v(vec.h[2:3, :].to_broadcast([128, D])))
    P.stt("dve", A[:], tmp[:], 1.0, A[:], ALU.add, ALU.mult)


def transpose_to_fm(P, y_t, rows, dstT, tok0, ident, pst, ev_i):
    for g in range(4):
        pt = pst[(ev_i + g) % len(pst)]
        for j in range(4):
            k = g * 4 + j
            P.transpose(pt[:, j * 128:j * 128 + rows], y_t[0:rows, k * 128:(k + 1) * 128], ident[0:rows, 0:rows])
        src = pt.v(pt.h[:, 0:512].rearrange("p (j t) -> p j t", j=4)[:, :, 0:rows])
        dst = dstT[:, g * 4:(g + 1) * 4, tok0:tok0 + rows]
        P.copy("act" if g % 2 == 0 else "dve", dst, src)


def build_L1():
    P = Prog()
    h = P.dram("h", [NTOK1, D], F32, "ExternalInput")
    vlat = P.dram("vlat", [3, D], F32, "ExternalInput")
    vctx = P.dram("vctx", [3, D], F32, "ExternalInput")
    w = P.dram("w", [D, IN_W], F32, "ExternalInput")
    idn = P.dram("ident", [128, 128], F32, "ExternalInput")
    p = P.dram("p", [NTOK1, IN_W], F32, "ExternalOutput")
    ident = P.sb("ident_sb", [128, 128])
    P.dma("sp", ident[:], idn[:])
    A_l, B_l, A_c, B_c = (P.sb(n, [128, D]) for n in ("A_l", "B_l", "A_c", "B_c"))
    load_mod_vectors(P, vlat, A_l, B_l)
    load_mod_vectors(P, vctx, A_c, B_c)
    nxT = P.sb("nxT", [128, 16, NTOK1], MMDT)
    xt = [P.sb("xt%d" % i, [128, D]) for i in range(2)]
    yt = P.sb("yt", [128, D])
    ss = P.sb("ss", [128, 1])
    rstd = P.sb("rstd", [128, 1])
    pst = [P.ps("ps%d" % i, [128, 512]) for i in range(8)]
    tiles = [(i * 128, 128) for i in range(8)] + [(1024, 64)]
    for ti, (t0, rows) in enumerate(tiles):
        x_t = xt[ti % 2]
        P.dma("sp", x_t[0:rows, :], h[t0:t0 + rows, :])
        lat = ti < 8
        rms_modulate(P, x_t, rows, A_l if lat else A_c, B_l if lat else B_c, yt, ss, rstd)
        transpose_to_fm(P, yt, rows, nxT, t0, ident, pst[0:4], ti * 4)
    wv = w.h.rearrange("(k p) n -> p k n", p=128)
    wb = [P.sb("wb%d" % i, [128, 16, 512], MMDT) for i in range(2)]
    ob = [P.sb("ob%d" % i, [128, 512]) for i in range(4)]
    nblk = (IN_W + 511) // 512
    oi = 0
    for nb in range(nblk):
        c0 = nb * 512
        cw = min(512, IN_W - c0)
        wt = wb[nb % 2]
        for kk in range(0, 16, 4):
            P.dma("pool", wt[:, kk:kk + 4, 0:cw], w.v(wv[:, kk:kk + 4, c0:c0 + cw]))
        for ti, (t0, rows) in enumerate(tiles):
            pt = pst[4 + oi % 4]
            for k in range(16):
                P.matmul(pt[0:rows, 0:cw], nxT[:, k, t0:t0 + rows], wt[:, k, 0:cw], start=(k == 0), stop=(k == 15))
            o = ob[oi % 4]
            P.copy("act" if oi % 2 == 0 else "dve", o[0:rows, 0:cw], pt[0:rows, 0:cw])
            P.dma("sp", p[t0:t0 + rows, c0:c0 + cw], o[0:rows, 0:cw])
            oi += 1
    return P


def tok_shard(lat, ctx, i):
    b, q = i // 4, i % 4
    return np.concatenate([lat[b, q * 1024:(q + 1) * 1024], ctx[b, q * 64:(q + 1) * 64]], axis=0)


def tok_unshard(parts, width):
    lat = np.zeros((B, S, width), np.float32)
    ctx = np.zeros((B, LCTX, width), np.float32)
    for i, pp in enumerate(parts):
        b, q = i // 4, i % 4
        lat[b, q * 1024:(q + 1) * 1024] = pp[:1024]
        ctx[b, q * 64:(q + 1) * 64] = pp[1024:]
    return lat, ctx


def run_L1(hx, hc, mod_l, norm_w, w_in):
    P = build_L1()
    ident = np.eye(128, dtype=np.float32)
    maps = []
    for i in range(NCORES):
        b = i // 4
        vlat = np.stack([norm_w, mod_l[b, D:2 * D], mod_l[b, 0:D]])
        vctx = np.stack([norm_w, mod_l[2, D:2 * D], mod_l[2, 0:D]])
        maps.append({"h": np.ascontiguousarray(tok_shard(hx, hc, i)), "vlat": vlat, "vctx": vctx,
                     "w": w_in, "ident": ident})
    res = run_prog(P, maps)
    return tok_unshard([r["p"] for r in res], IN_W)


NEG = -30000.0
NA_DT = BF16


def na_row_class(r):
    return r if r < 4 else (4 if r < 60 else r - 55)


def build_NA(with_ctx):
    P = Prog()
    NH = 4
    TT = S + LCTX
    qT_d = P.dram("qT", [NH, 64, TT], F32, "ExternalInput")
    kT_d = P.dram("kT", [NH, 64, TT], F32, "ExternalInput")
    v_d = P.dram("v", [NH, 64, 68, 64], F32, "ExternalInput")
    bias_d = P.dram("bias", [NH, 64, 9, 512], F32, "ExternalInput")
    idn = P.dram("ident", [128, 128], F32, "ExternalInput")
    o_d = P.dram("o", [NH, 64, 68, 64], F32, "ExternalOutput")
    ident = P.sb("ident_sb", [128, 128])
    P.dma("sp", ident[:], idn[:])
    qT = [P.sb("qT%d" % i, [64, TT], NA_DT) for i in range(2)]
    kT = [P.sb("kT%d" % i, [64, TT], NA_DT) for i in range(2)]
    V = [P.sb("V%d" % i, [64, 68, 64], NA_DT) for i in range(2)]
    bias = [P.sb("bias%d" % i, [64, 9, 512]) for i in range(2)]
    O = [P.sb("O%d" % i, [64, 68, 64]) for i in range(2)]
    Sb = [P.sb("Sb%d" % i, [64, 768]) for i in range(2)]
    Pm = [P.sb("Pm%d" % i, [64, 768], NA_DT) for i in range(2)]
    PT = [P.sb("PT%d" % i, [64, 12, 64], NA_DT) for i in range(2)]
    st = [P.sb("st%d" % i, [64, 4]) for i in range(2)]
    s_loc = [P.ps("s_loc%d" % i, [64, 512]) for i in range(2)]
    s_ctx = [P.ps("s_ctx%d" % i, [64, 256]) for i in range(2)]
    pt_all = [P.ps("pt%d" % i, [64, 768], NA_DT) for i in range(2)]
    o_ps = [P.ps("o_ps%d" % i, [64, 64]) for i in range(2)]
    identb = P.sb("identb", [64, 64], NA_DT)
    P.copy("dve", identb[:, :], ident[0:64, 0:64])
    it = 0
    for hh in range(NH):
        s = hh % 2
        dq = "pool" if NA_DT != F32 else "sp"
        P.dma(dq, qT[s][:], qT_d.v(qT_d.h[hh]))
        P.dma(dq, kT[s][:], kT_d.v(kT_d.h[hh]))
        P.dma(dq, V[s][:], v_d.v(v_d.h[hh]))
        P.dma("sp", bias[s][:], bias_d.v(bias_d.h[hh]))
        nrows = 68 if with_ctx else 64
        if not with_ctx:
            P.memset("pool", O[s][:, 64:68, :], 0.0)
        for r in range(nrows):
            u = it % 2
            it += 1
            local = r < 64
            q_ap = qT[s][:, r * 64:(r + 1) * 64]
            nk = 12 if local else 4
            wid = nk * 64
            if local:
                r0 = min(max(r - 4, 0), 56)
                P.matmul(s_loc[u][:, :], q_ap, kT[s][:, r0 * 64:r0 * 64 + 512])
                P.matmul(s_ctx[u][:, :], q_ap, kT[s][:, S:S + 256])
                P.stt("dve", Sb[u][:, 0:512], s_loc[u][:, :], 0.125, bias[s][:, na_row_class(r), :], ALU.mult, ALU.add)
                P.act(Sb[u][:, 512:768], s_ctx[u][:, :], AF.Identity, scale=0.125)
            else:
                P.matmul(s_ctx[u][:, :], q_ap, kT[s][:, S:S + 256])
                P.act(Sb[u][:, 0:256], s_ctx[u][:, :], AF.Identity, scale=0.125)
            mx, nmx, sm, rs = (st[u][:, j:j + 1] for j in range(4))
            P.op("dve", lambda e, a=mx.ap, b_=Sb[u][:, 0:wid].ap: e.reduce_max(a, b_, AX.X), reads=[Sb[u]], writes=[st[u]])
            P.ts("dve", nmx, mx, -1.0, ALU.mult)
            P.memset("dve", sm, 0.0)
            P.act(Pm[u][:, 0:wid], Sb[u][:, 0:wid], AF.Exp, bias=nmx, accum=sm)
            P.recip(rs, sm)
            for j in range(nk):
                P.transpose(pt_all[u][:, j * 64:(j + 1) * 64], Pm[u][:, j * 64:(j + 1) * 64], identb[:, :])
            ptv = pt_all[u].v(pt_all[u].h[:, 0:nk * 64].rearrange("p (j q) -> p j q", j=nk))
            P.copy("dve" if it % 2 else "act", PT[u][:, 0:nk, :], ptv)
            for j in range(nk):
                if local:
                    vr = (r0 + j) if j < 8 else (64 + j - 8)
                else:
                    vr = 64 + j
                P.matmul(o_ps[u][:, :], PT[u][:, j, :], V[s][:, vr, :], start=(j == 0), stop=(j == nk - 1))
            P.ts("dve", O[s][:, r, :], o_ps[u][:, :], rs, ALU.mult)
        P.dma("sp", o_d.v(o_d.h[hh]), O[s][:])
    return P


def na_bias_tables(rpb_l):
    cq = np.arange(64)
    cstart = np.clip(cq - 8, 0, 48)
    colmask = (cq[None, :] >= cstart[:, None]) & (cq[None, :] < cstart[:, None] + 16)
    dc = np.clip(cq[None, :] - cq[:, None] + 15, 0, 30)
    out = np.full((16, 64, 9, 8, 64), NEG, np.float32)
    for rc in range(9):
        r = rc if rc < 4 else (4 if rc == 4 else rc + 55)
        ridx = min(max(r - 4, 0), 56) + np.arange(8)
        dr = ridx - r + 7
        g = rpb_l[:, dr[:, None, None], dc[None, :, :]]
        g = np.where(colmask[None, None], g, NEG)
        out[:, :, rc] = g.transpose(0, 2, 1, 3)
    return out.reshape(16, 64, 9, 512)


def run_NA(px, pc, rpb_l, with_ctx):
    P = build_NA(with_ctx)
    ident = np.eye(128, dtype=np.float32)
    bias_all = na_bias_tables(rpb_l)
    maps = []
    for i in range(NCORES):
        b, h0 = i // 4, 4 * (i % 4)
        full = np.concatenate([px[b, :, 0:3072], pc[b, :, 0:3072]], axis=0)
        q = full[:, 0:1024].reshape(S + LCTX, 16, 64)[:, h0:h0 + 4]
        k = full[:, 1024:2048].reshape(S + LCTX, 16, 64)[:, h0:h0 + 4]
        v = full[:, 2048:3072].reshape(68, 64, 16, 64)[:, :, h0:h0 + 4]
        maps.append({
            "qT": np.ascontiguousarray(q.transpose(1, 2, 0)),
            "kT": np.ascontiguousarray(k.transpose(1, 2, 0)),
            "v": np.ascontiguousarray(v.transpose(2, 1, 0, 3)),
            "bias": np.ascontiguousarray(bias_all[h0:h0 + 4]),
            "ident": ident,
        })
    res = run_prog(P, maps)
    ox = np.zeros((B, S, 1024), np.float32)
    oc = np.zeros((B, LCTX, 1024), np.float32)
    for i, r in enumerate(res):
        b, h0 = i // 4, 4 * (i % 4)
        o = r["o"].transpose(2, 1, 0, 3).reshape(S + LCTX, 4, 64)
        ox[b, :, h0 * 64:(h0 + 4) * 64] = o[:S].reshape(S, 256)
        oc[b, :, h0 * 64:(h0 + 4) * 64] = o[S:].reshape(LCTX, 256)
    return ox, oc


TT = S + LCTX


def build_GLA(with_ctx):
    P = Prog()
    qT_d = P.dram("qT", [64, TT], F32, "ExternalInput")
    kT_d = P.dram("kT", [64, TT], F32, "ExternalInput")
    v_d = P.dram("v", [64, 68, 128], F32, "ExternalInput")
    g_d = P.dram("g", [64, 68, 128], F32, "ExternalInput")
    lr_d = P.dram("lr", [2, 16, TT], F32, "ExternalInput")
    gw_d = P.dram("gw", [2, 17, 64], F32, "ExternalInput")
    ct_d = P.dram("ct", [64, S], F32, "ExternalInput")
    st_d = P.dram("st", [64, S], F32, "ExternalInput")
    psw_d = P.dram("psw", [64, 64], F32, "ExternalInput")
    tri_d = P.dram("tri", [2, 64, 512], F32, "ExternalInput")
    nw_d = P.dram("nw", [64, 512], F32, "ExternalInput")
    idn = P.dram("ident", [128, 128], F32, "ExternalInput")
    o_d = P.dram("o", [64, 68, 128], F32, "ExternalOutput")

    ident = P.sb("ident_sb", [128, 128])
    P.dma("sp", ident[:], idn[:])
    psw = P.sb("psw_sb", [64, 64])
    P.dma("sp", psw[:], psw_d[:])
    tri = [P.sb("tri%d" % i, [64, 512]) for i in range(2)]
    gw = [P.sb("gw%d" % i, [17, 64]) for i in range(2)]
    for i in range(2):
        P.dma("sp", tri[i][:], tri_d.v(tri_d.h[i]))
        P.dma("sp", gw[i][:], gw_d.v(gw_d.h[i]))
    nw4 = P.sb("nw4", [64, 512])
    P.dma("sp", nw4[:], nw_d[:])
    ob_store = P.sb("ob_store", [64, 68, 128])

    NB = 2
    qg = [P.sb("qg%d" % i, [64, 512]) for i in range(NB)]
    kg = [P.sb("kg%d" % i, [64, 512]) for i in range(NB)]
    vg = [P.sb("vg%d" % i, [64, 8, 128]) for i in range(NB)]
    gg = [P.sb("gg%d" % i, [64, 8, 128]) for i in range(NB)]
    lra = [P.sb("lra%d" % i, [17, 512]) for i in range(NB)]
    ctg = [P.sb("ctg%d" % i, [64, 512]) for i in range(NB)]
    stg = [P.sb("stg%d" % i, [64, 512]) for i in range(NB)]
    tmp = P.sb("tmp", [64, 512])
    e_sb = P.sb("e_sb", [64, 512])
    l_sb = P.sb("l_sb", [64, 512])
    Eq = P.sb("Eq", [64, 512])
    Ek = P.sb("Ek", [64, 512])
    qe = [P.sb("qe%d" % i, [64, 512]) for i in range(NB)]
    keT = P.sb("keT", [64, 512])
    ke = [P.sb("ke%d" % i, [64, 512]) for i in range(NB)]
    attT = [P.sb("attT%d" % i, [64, 512]) for i in range(NB)]
    glast = [P.sb("glast%d" % i, [64, 8]) for i in range(NB)]
    mgt = [P.sb("mgt%d" % i, [64, 128]) for i in range(2)]
    Sb_ = [P.sb("S%d" % i, [64, 128]) for i in range(4)]
    osum = [P.sb("osum%d" % i, [64, 4, 128]) for i in range(2)]
    sq = P.sb("sq", [64, 4, 128])
    on = [P.sb("on%d" % i, [64, 4, 128]) for i in range(2)]
    sg = P.sb("sg", [64, 4, 128])
    ss4 = P.sb("ss4", [64, 4])
    zps = P.ps("zps", [64, 512])
    cps = P.ps("cps", [64, 512])
    aps = P.ps("aps", [64, 512])
    kps = P.ps("kps", [64, 512])
    mps = [P.ps("mps%d" % i, [64, 512]) for i in range(2)]
    ops = [P.ps("ops%d" % i, [64, 512]) for i in range(2)]

    cnt = {"g": 0, "s": 0, "m": 0, "o": 0}

    def group(dr, tok0, ntok, rope, final):
        nch = ntok // 64
        u = cnt["g"] % NB
        cnt["g"] += 1
        c0 = tok0 // 64
        q_, k_, v_, l_ = qg[u], kg[u], vg[u], lra[u]
        P.dma("sp", q_[:, 0:ntok], qT_d[:, tok0:tok0 + ntok])
        P.dma("sp", k_[:, 0:ntok], kT_d[:, tok0:tok0 + ntok])
        P.dma("sp", v_[:, 0:nch, :], v_d[:, c0:c0 + nch, :])
        P.memset("pool", l_[:, :], 1.0)
        P.dma("sp", l_[0:16, 0:ntok], lr_d.v(lr_d.h[dr, :, tok0:tok0 + ntok]))
        if final:
            P.dma("sp", gg[u][:, 0:nch, :], g_d[:, c0:c0 + nch, :])
        if rope:
            P.dma("sp", ctg[u][:, 0:ntok], ct_d[:, tok0:tok0 + ntok])
            P.dma("sp", stg[u][:, 0:ntok], st_d[:, tok0:tok0 + ntok])
            for x in (q_, k_):
                P.matmul(zps[:, 0:ntok], psw[:, :], x[:, 0:ntok])
                P.tt("dve", tmp[:, 0:ntok], zps[:, 0:ntok], stg[u][:, 0:ntok], ALU.mult)
                P.tt("pool", x[:, 0:ntok], x[:, 0:ntok], ctg[u][:, 0:ntok], ALU.mult)
                P.tt("dve", x[:, 0:ntok], x[:, 0:ntok], tmp[:, 0:ntok], ALU.add)
        for j in range(nch):
            P.matmul(zps[:, j * 64:(j + 1) * 64], l_[:, j * 64:(j + 1) * 64], gw[dr][:, :])
        P.act(e_sb[:, 0:ntok], zps[:, 0:ntok], AF.Exp, scale=-1.0)
        P.act(l_sb[:, 0:ntok], e_sb[:, 0:ntok], AF.Ln, bias=1.0)
        for j in range(nch):
            P.matmul(cps[:, j * 64:(j + 1) * 64], l_sb[:, j * 64:(j + 1) * 64], tri[dr][:, 0:64])
        P.act(Eq[:, 0:ntok], cps[:, 0:ntok], AF.Exp, scale=-1.0 / 16.0)
        P.act(Ek[:, 0:ntok], cps[:, 0:ntok], AF.Exp, scale=1.0 / 16.0)
        P.stt("dve", qe[u][:, 0:ntok], q_[:, 0:ntok], 0.125, Eq[:, 0:ntok], ALU.mult, ALU.mult)
        P.tt("dve", keT[:, 0:ntok], k_[:, 0:ntok], Ek[:, 0:ntok], ALU.mult)
        lastcol = 63 if dr == 0 else 0
        P.copy("dve", glast[u][:, 0:nch], Eq.v(Eq.h[:, 0:ntok].rearrange("p (c t) -> p c t", t=64)[:, :, lastcol]))
        for j in range(nch):
            P.matmul(aps[:, j * 64:(j + 1) * 64], keT[:, j * 64:(j + 1) * 64], qe[u][:, j * 64:(j + 1) * 64])
        P.tt("dve", attT[u][:, 0:ntok], aps[:, 0:ntok], tri[dr][:, 0:ntok], ALU.mult)
        for j in range(nch):
            P.transpose(kps[:, j * 64:(j + 1) * 64], keT[:, j * 64:(j + 1) * 64], ident[0:64, 0:64])
        P.copy("act", ke[u][:, 0:ntok], kps[:, 0:ntok])
        for j in range(nch):
            P.matmul(mps[j // 4][:, (j % 4) * 128:(j % 4 + 1) * 128], ke[u][:, j * 64:(j + 1) * 64], v_[:, j, :])
        need_out = with_ctx or tok0 < S
        js = list(range(nch)) if dr == 0 else list(range(nch - 1, -1, -1))
        for n, j in enumerate(js):
            col = (j % 4) * 128
            S_cur = Sb_[cnt["s"] % 4]
            S_nxt = Sb_[(cnt["s"] + 1) % 4]
            cnt["s"] += 1
            if need_out:
                ob = ops[j // 4]
                P.matmul(ob[:, col:col + 128], qe[u][:, j * 64:(j + 1) * 64], S_cur[:, :], start=True, stop=False)
                P.matmul(ob[:, col:col + 128], attT[u][:, j * 64:(j + 1) * 64], v_[:, j, :], start=False, stop=True)
            mg = mgt[cnt["m"] % 2]
            cnt["m"] += 1
            P.ts("dve", mg[:, :], mps[j // 4][:, col:col + 128], glast[u][:, j:j + 1], ALU.mult)
            P.stt("dve", S_nxt[:, :], S_cur[:, :], glast[u][:, j:j + 1], mg[:, :], ALU.mult, ALU.add)
            done_bank = (j % 4 == 3) if dr == 0 else (j % 4 == 0)
            if need_out and done_bank:
                q4 = j // 4
                cb = c0 + 4 * q4
                ob = ops[q4]
                ob3 = ob.v(ob.h[:, :].rearrange("p (c d) -> p c d", c=4))
                if not final:
                    P.copy("act", ob_store[:, cb:cb + 4, :], ob3)
                else:
                    w_ = cnt["o"] % 2
                    cnt["o"] += 1
                    P.tt("dve", osum[w_][:, :, :], ob3, ob_store[:, cb:cb + 4, :], ALU.add)
                    P.act(sq[:, :, :], osum[w_][:, :, :], AF.Square)
                    P.op("dve", lambda e, a=ss4[:, :].ap, b_=sq[:, :, :].ap: e.tensor_reduce(a, b_, AX.X, ALU.add),
                         reads=[sq], writes=[ss4])
                    P.ts("dve", ss4[:, :], ss4[:, :], 1.0 / 128.0, ALU.mult, EPS, ALU.add)
                    P.act(ss4[:, :], ss4[:, :], AF.Sqrt)
                    P.recip(ss4[:, :], ss4[:, :])
                    for jj in range(4):
                        P.ts("dve", on[w_][:, jj, :], osum[w_][:, jj, :], ss4[:, jj:jj + 1], ALU.mult)
                    P.tt("pool", on[w_][:, :, :], on[w_][:, :, :], nw4.v(nw4.h[:, :].rearrange("p (c d) -> p c d", c=4)), ALU.mult)
                    P.act(sg[:, :, :], gg[u][:, 4 * q4:4 * q4 + 4, :], AF.Silu)
                    P.tt("dve", on[w_][:, :, :], on[w_][:, :, :], sg[:, :, :], ALU.mult)
                    P.dma("sp", o_d[:, cb:cb + 4, :], on[w_][:, :, :])

    for dr, final in ((1, False), (0, True)):
        P.memset("dve", Sb_[cnt["s"] % 4][:, :], 0.0)
        group(dr, S, LCTX, False, final)
        gl = list(range(8)) if dr == 0 else list(range(7, -1, -1))
        for g_ in gl:
            group(dr, g_ * 512, 512, True, final)
    if not with_ctx:
        P.memset("dve", on[0][:, :, :], 0.0)
        P.dma("sp", o_d[:, 64:68, :], on[0][:, :, :])
    return P


def rope_tables():
    nf = 16
    freqs = (10000.0 ** (-np.arange(nf, dtype=np.float32) / nf)).astype(np.float32)
    pos = np.arange(S)
    rp, cp = (pos // 64).astype(np.float32), (pos % 64).astype(np.float32)
    ct = np.zeros((64, S), np.float32)
    st = np.zeros((64, S), np.float32)
    psw = np.zeros((64, 64), np.float32)
    for d in range(64):
        half, i = d // 32, d % 32
        f = i % 16
        ang = ((rp if half == 0 else cp) * freqs[f]).astype(np.float32)
        ct[d] = np.cos(ang)
        st[d] = -np.sin(ang) if i < 16 else np.sin(ang)
        partner = d + 16 if i < 16 else d - 16
        psw[partner, d] = 1.0
    return ct, st, psw


def tri_masks():
    a = np.arange(64)
    f = (a[:, None] <= a[None, :]).astype(np.float32)
    bk = (a[:, None] >= a[None, :]).astype(np.float32)
    return np.stack([np.tile(f, (1, 8)), np.tile(bk, (1, 8))])


def chunk_tm(a):
    return np.ascontiguousarray(a.reshape(68, 64, a.shape[-1]).transpose(1, 0, 2))


def unchunk_tm(a):
    return a.transpose(1, 0, 2).reshape(68 * 64, a.shape[-1])


def run_GLA(px, pc, gate_w, gate_b, norm_w, with_ctx):
    P = build_GLA(with_ctx)
    ident = np.eye(128, dtype=np.float32)
    ct, st, psw = rope_tables()
    tri = tri_masks()
    nw = np.ascontiguousarray(np.tile(norm_w[None, :], (64, 4)))
    maps = []
    o0 = 3072
    for i in range(NCORES):
        b, h = i // 4, i % 4
        full = np.concatenate([px[b], pc[b]], axis=0)
        q = full[:, o0 + h * 64:o0 + (h + 1) * 64]
        k = full[:, o0 + 256 + h * 64:o0 + 256 + (h + 1) * 64]
        v = full[:, o0 + 512 + h * 128:o0 + 512 + (h + 1) * 128]
        g = full[:, o0 + 1024 + h * 128:o0 + 1024 + (h + 1) * 128]
        lr = full[:, o0 + 1536:o0 + 1568].reshape(TT, 2, 16)
        gw = np.stack([np.concatenate([gate_w[d][:, h * 64:(h + 1) * 64], gate_b[d][None, h * 64:(h + 1) * 64]], 0) for d in range(2)])
        maps.append({"qT": np.ascontiguousarray(q.T), "kT": np.ascontiguousarray(k.T), "v": chunk_tm(v), "g": chunk_tm(g),
                     "lr": np.ascontiguousarray(lr.transpose(1, 2, 0)), "gw": gw, "ct": ct, "st": st, "psw": psw,
                     "tri": tri, "nw": nw, "ident": ident})
    res = run_prog(P, maps)
    ox = np.zeros((B, S, 512), np.float32)
    oc = np.zeros((B, LCTX, 512), np.float32)
    for i, r in enumerate(res):
        b, h = i // 4, i % 4
        o = unchunk_tm(r["o"])
        ox[b, :, h * 128:(h + 1) * 128] = o[:S]
        oc[b, :, h * 128:(h + 1) * 128] = o[S:]
    return ox, oc


def build_GDN(with_ctx):
    P = Prog()
    raw_d = P.dram("raw", [3, 128, TT], F32, "ExternalInput")
    cw_d = P.dram("cw", [3, 128, 5], F32, "ExternalInput")
    z_d = P.dram("z", [64, 68, 128], F32, "ExternalInput")
    a_d = P.dram("a", [2, 64, 68], F32, "ExternalInput")
    b_d = P.dram("bb", [2, 64, 68], F32, "ExternalInput")
    al_d = P.dram("alog", [64, 2], F32, "ExternalInput")
    dtb_d = P.dram("dtb", [64, 2], F32, "ExternalInput")
    ctri_d = P.dram("ctri", [2, 64, 64], F32, "ExternalInput")
    negm_d = P.dram("negm", [2, 64, 256], F32, "ExternalInput")
    smask_d = P.dram("smask", [2, 64, 256], F32, "ExternalInput")
    nw_d = P.dram("nw", [64, 512], F32, "ExternalInput")
    idn = P.dram("ident", [128, 128], F32, "ExternalInput")
    o_d = P.dram("o", [64, 68, 128], F32, "ExternalOutput")

    ident = P.sb("ident_sb", [128, 128])
    P.dma("sp", ident[:], idn[:])
    ident4 = P.sb("ident4", [64, 256])
    for j in range(4):
        P.dma("sp", ident4[:, j * 64:(j + 1) * 64], idn[0:64, 0:64])
    ones = P.sb("ones", [128, 128])
    P.memset("dve", ones[:], 1.0)
    negones = P.sb("negones", [64, 64])
    P.memset("dve", negones[:], -1.0)
    ctri = [P.sb("ctri%d" % i, [64, 64]) for i in range(2)]
    trione = [P.sb("trione%d" % i, [64, 192]) for i in range(2)]
    negm = [P.sb("negm%d" % i, [64, 256]) for i in range(2)]
    smask = [P.sb("smask%d" % i, [64, 256]) for i in range(2)]
    for i in range(2):
        P.dma("sp", ctri[i][:], ctri_d.v(ctri_d.h[i]))
        P.dma("sp", trione[i][:, 0:64], ctri_d.v(ctri_d.h[i]))
        P.memset("dve", trione[i][:, 64:192], 1.0)
        P.dma("sp", negm[i][:], negm_d.v(negm_d.h[i]))
        P.dma("sp", smask[i][:], smask_d.v(smask_d.h[i]))
    nw4 = P.sb("nw4", [64, 512])
    P.dma("sp", nw4[:], nw_d[:])
    cw = P.sb("cw_sb", [128, 3, 5])
    for i in range(3):
        P.dma("sp", cw[:, i, :], cw_d.v(cw_d.h[i]))

    xc = [P.sb("xc%d" % i, [128, TT]) for i in range(3)]
    upad = P.sb("upad", [128, S + 4])
    upc = P.sb("upc", [128, LCTX + 4])
    P.memset("pool", upad[:], 0.0)
    P.memset("pool", upc[:], 0.0)
    for i in range(3):
        P.dma("sp", upad[:, 2:2 + S], raw_d.v(raw_d.h[i, :, 0:S]))
        P.dma("sp", upc[:, 2:2 + LCTX], raw_d.v(raw_d.h[i, :, S:TT]))
        for (src, off, n) in ((upad, 0, S), (upc, S, LCTX)):
            y = xc[i][:, off:off + n]
            P.ts("dve", y, src[:, 0:n], cw[:, i, 0:1], ALU.mult)
            for j in range(1, 5):
                P.stt("dve", y, src[:, j:j + n], cw[:, i, j:j + 1], y, ALU.mult, ALU.add)
            P.act(y, y, AF.Silu)

    al = P.sb("al_sb", [64, 2])
    dtb = P.sb("dtb_sb", [64, 2])
    P.dma("sp", al[:], al_d[:])
    P.dma("sp", dtb[:], dtb_d[:])
    nea = P.sb("nea", [64, 2])
    P.act(nea[:], al[:], AF.Exp)
    P.ts("dve", nea[:], nea[:], -1.0, ALU.mult)
    g_all = [P.sb("g_all%d" % i, [64, 68]) for i in range(2)]
    beta = [P.sb("beta%d" % i, [64, 68]) for i in range(2)]
    nbeta = [P.sb("nbeta%d" % i, [64, 68]) for i in range(2)]
    tmpa = P.sb("tmpa", [64, 68])
    for d_ in range(2):
        P.dma("sp", tmpa[:], a_d.v(a_d.h[d_]))
        P.act(tmpa[:], tmpa[:], AF.Exp, bias=dtb[:, d_:d_ + 1])
        P.act(tmpa[:], tmpa[:], AF.Ln, bias=1.0)
        P.ts("dve", g_all[d_][:], tmpa[:], nea[:, d_:d_ + 1], ALU.mult)
        P.dma("sp", beta[d_][:], b_d.v(b_d.h[d_]))
        P.act(beta[d_][:], beta[d_][:], AF.Sigmoid)
        P.ts("dve", nbeta[d_][:], beta[d_][:], -1.0, ALU.mult)

    import os
    STOP = int(os.environ.get("GDN_STOP", "99"))
    if STOP == 0:
        return P
    ob_store = P.sb("ob_store", [64, 68, 128])
    def t64(n):
        return P.sb(n, [64, 256])
    qdT = [P.sb("qdT%d" % i, [128, 256]) for i in range(2)]
    egcb = P.sb("egcb", [128, 256])
    Gt = P.sb("Gt", [64, 4, 192])
    glb = [P.sb("glb_sb%d" % i, [128, 4]) for i in range(2)]
    gct = P.sb("gct_sb", [64, 4])
    egc = P.sb("egc", [64, 4])
    ekd = P.sb("ekd", [64, 4])
    bws = P.sb("bws", [64, 4])
    Dm, decay, decs, A_, aqk, AT, Pj, PTj, RT = (t64(n) for n in
                                                 ("Dm", "decay", "decs", "A_", "aqk", "AT", "Pj", "PTj", "RT"))
    aqkT = [t64("aqkT%d" % i) for i in range(2)]
    ktok = P.sb("ktok", [64, 4, 128])
    vtok = P.sb("vtok", [64, 4, 128])
    bu = P.sb("bu", [64, 4, 128])
    bwk = P.sb("bwk", [64, 4, 128])
    kdec = [P.sb("kdec%d" % i, [64, 4, 128]) for i in range(2)]
    u_sb = [P.sb("u_sb%d" % i, [64, 4, 128]) for i in range(2)]
    wT = [P.sb("wT%d" % i, [128, 256]) for i in range(2)]
    vnew = [P.sb("vnew%d" % i, [64, 128]) for i in range(2)]
    Sst = [P.sb("Sst%d" % i, [128, 128]) for i in range(4)]
    zg = [P.sb("zg%d" % i, [64, 4, 128]) for i in range(2)]
    osum = P.sb("osum", [64, 4, 128])
    sq4 = P.sb("sq4", [64, 4, 128])
    on = [P.sb("on%d" % i, [64, 4, 128]) for i in range(2)]
    sg = P.sb("sg", [64, 4, 128])
    ss4 = P.sb("ss4", [64, 4])
    bk = [P.ps("bank%d" % i, [128, 512]) for i in range(8)]
    half = lambda t, i, nm, rows=128: t.sub(nm, (slice(0, rows), slice(i * 256, (i + 1) * 256)))
    p_D, p_egc = half(bk[0], 0, "p_D", 64), half(bk[0], 1, "p_egc")
    p_QK, p_RP = half(bk[1], 0, "p_QK", 64), half(bk[1], 1, "p_RP", 64)
    p_A, p_B = half(bk[2], 0, "p_A", 64), half(bk[2], 1, "p_B", 64)
    p_wT = half(bk[7], 1, "p_wT")
    p_vn = bk[3].sub("p_vn", (slice(0, 64), slice(256, 384)))
    p_S = bk[3].sub("p_S", (slice(0, 128), slice(384, 512)))
    p_tok = bk[4].sub("p_tok", (slice(0, 64), slice(0, 512)))
    p_u = bk[5]
    p_o = bk[6].sub("p_o", (slice(0, 64), slice(0, 512)))
    p_glb = bk[7].sub("p_glb", (slice(0, 128), slice(0, 4)))
    p_gct = bk[7].sub("p_gct", (slice(0, 64), slice(4, 8)))

    cnt = {"s": 0, "v": 0, "o": 0}
    i64 = ident[0:64, 0:64]

    def c64(j):
        return slice(j * 64, (j + 1) * 64)

    class _Stop(Exception):
        pass

    def ck(n):
        if STOP == n:
            raise _Stop()

    def prep(dr, tok0, final, par):
        c0 = tok0 // 64
        ts_ = slice(tok0, tok0 + 256)
        qnT = xc[0].sub("qnTv", (slice(None), ts_))
        knT = xc[1].sub("knTv", (slice(None), ts_))
        yield
        gsl = g_all[dr][:, c0:c0 + 4]
        P.matmul(p_glb[:, :], ones[0:64, :], gsl)
        P.matmul(p_gct[:, :], ctri[dr][:, :], gsl)
        P.act(glb[par][:], p_glb[:, :], AF.Exp)
        P.act(egc[:], p_gct[:, :], AF.Exp)
        P.copy("dve", gct[:], p_gct[:, :])
        P.tt("dve", ekd[:], p_glb[0:64, :], gct[:], ALU.subtract)
        P.act(ekd[:], ekd[:], AF.Exp)
        P.tt("dve", bws[:], beta[dr][:, c0:c0 + 4], egc[:], ALU.mult)
        yield
        for (srcT, dst) in ((knT, ktok), (None, vtok)):
            for j in range(4):
                in_ = srcT[:, c64(j)] if srcT is not None else xc[2][:, tok0 + j * 64:tok0 + (j + 1) * 64]
                P.transpose(p_tok[:, j * 128:(j + 1) * 128], in_, ident[:, :])
            P.copy("act", dst[:, :, :], p_tok.v(p_tok.h[:, :].rearrange("p (c d) -> p c d", c=4)))
        for j in range(4):
            P.ts("dve", bu[:, j, :], vtok[:, j, :], beta[dr][:, c0 + j:c0 + j + 1], ALU.mult)
            P.ts("pool", bwk[:, j, :], ktok[:, j, :], bws[:, j:j + 1], ALU.mult)
            P.ts("pool", kdec[par][:, j, :], ktok[:, j, :], ekd[:, j:j + 1], ALU.mult)
        yield
        for j in range(4):
            P.ts("dve", Gt[:, j, :], trione[dr][:, :], g_all[dr][:, c0 + j:c0 + j + 1], ALU.mult)
        yield
        for j in range(4):
            P.matmul(p_D[:, c64(j)], ctri[dr][:, :], Gt[:, j, 64:128], start=True, stop=False)
            P.matmul(p_D[:, c64(j)], negones[:, :], Gt[:, j, 0:64], start=False, stop=True)
        yield
        for j in range(4):
            P.matmul(p_egc[:, c64(j)], Gt[:, j, 64:192], ctri[dr][:, :])
        yield
        P.act(egcb[:], p_egc[:, :], AF.Exp)
        P.tt("dve", qdT[par][:], qnT[:], egcb[:], ALU.mult)
        P.tt("dve", Dm[:], p_D[:, :], negm[dr][:], ALU.add)
        P.act(decay[:], Dm[:], AF.Exp)
        P.tt("pool", decs[:], decay[:], smask[dr][:], ALU.mult)
        yield
        for j in range(4):
            P.matmul(p_D[:, c64(j)], knT[:, c64(j)], knT[:, c64(j)])
            P.matmul(p_QK[:, c64(j)], qnT[:, c64(j)], knT[:, c64(j)])
        for j in range(4):
            P.stt("dve", A_[:, c64(j)], p_D[:, c64(j)], nbeta[dr][:, c0 + j:c0 + j + 1], decs[:, c64(j)], ALU.mult, ALU.mult)
        P.tt("dve", aqk[:], p_QK[:, :], decay[:], ALU.mult)
        for j in range(4):
            P.transpose(p_A[:, c64(j)], A_[:, c64(j)], i64)
            P.transpose(p_B[:, c64(j)], aqk[:, c64(j)], i64)
        P.copy("act", AT[:], p_A[:, :])
        P.copy("dve", aqkT[par][:], p_B[:, :])
        yield
        P.tt("dve", RT[:], ident4[:], AT[:], ALU.add)
        Pc, PTc = A_, AT
        for lvl in range(1, 6):
            for j in range(4):
                P.matmul(p_A[:, c64(j)], PTc[:, c64(j)], Pc[:, c64(j)])
            if lvl < 5:
                for j in range(4):
                    P.matmul(p_B[:, c64(j)], Pc[:, c64(j)], PTc[:, c64(j)])
            P.copy("act", Pj[:], p_A[:, :])
            if lvl < 5:
                P.copy("dve", PTj[:], p_B[:, :])
            for j in range(4):
                P.matmul(p_RP[:, c64(j)], Pj[:, c64(j)], RT[:, c64(j)])
            P.tt("dve", RT[:], RT[:], p_RP[:, :], ALU.add)
            Pc, PTc = Pj, PTj
            yield
        yield
        for j in range(4):
            P.matmul(p_u[0:64, j * 128:(j + 1) * 128], RT[:, c64(j)], bu[:, j, :])
            P.matmul(p_wT[:, c64(j)], bwk[:, j, :], RT[:, c64(j)])
        P.copy("act", u_sb[par][:, :, :], p_u.v(p_u.h[0:64, :].rearrange("p (c d) -> p c d", c=4)))
        P.copy("dve", wT[par][:], p_wT[:, :])
        yield

    def recur(dr, tok0, final, par, first):
        c0 = tok0 // 64
        if first:
            P.memset("dve", Sst[cnt["s"] % 4][:, :], 0.0)
        need_out = with_ctx or tok0 < S
        if need_out and final:
            P.dma("sp", zg[par][:, :, :], z_d[:, c0:c0 + 4, :])
        js = list(range(4)) if dr == 0 else [3, 2, 1, 0]
        for j in js:
            S_cur = Sst[cnt["s"] % 4]
            S_nxt = Sst[(cnt["s"] + 1) % 4]
            cnt["s"] += 1
            vn = vnew[cnt["v"] % 2]
            cnt["v"] += 1
            P.matmul(p_vn[:, :], wT[par][:, c64(j)], S_cur[:, :])
            P.tt("dve", vn[:, :], u_sb[par][:, j, :], p_vn[:, :], ALU.subtract)
            if need_out:
                P.matmul(p_o[:, j * 128:(j + 1) * 128], qdT[par][:, c64(j)], S_cur[:, :], start=True, stop=False)
                P.matmul(p_o[:, j * 128:(j + 1) * 128], aqkT[par][:, c64(j)], vn[:, :], start=False, stop=True)
            P.matmul(p_S[:, :], kdec[par][:, j, :], vn[:, :])
            P.stt("dve", S_nxt[:, :], S_cur[:, :], glb[par][:, j:j + 1], p_S[:, :], ALU.mult, ALU.add)
            yield
        if need_out:
            ob3 = p_o.v(p_o.h[:, :].rearrange("p (c d) -> p c d", c=4))
            if not final:
                P.copy("act", ob_store[:, c0:c0 + 4, :], ob3)
            else:
                w_ = cnt["o"] % 2
                cnt["o"] += 1
                P.tt("dve", osum[:, :, :], ob3, ob_store[:, c0:c0 + 4, :], ALU.add)
                P.act(sq4[:, :, :], osum[:, :, :], AF.Square)
                P.op("dve", lambda e, a=ss4[:, :].ap, b_=sq4[:, :, :].ap: e.tensor_reduce(a, b_, AX.X, ALU.add),
                     reads=[sq4], writes=[ss4])
                P.ts("dve", ss4[:, :], ss4[:, :], 1.0 / 128.0, ALU.mult, EPS, ALU.add)
                P.act(ss4[:, :], ss4[:, :], AF.Sqrt)
                P.recip(ss4[:, :], ss4[:, :])
                for jj in range(4):
                    P.ts("dve", on[w_][:, jj, :], osum[:, jj, :], ss4[:, jj:jj + 1], ALU.mult)
                P.tt("pool", on[w_][:, :, :], on[w_][:, :, :], nw4.v(nw4.h[:, :].rearrange("p (c d) -> p c d", c=4)), ALU.mult)
                P.act(sg[:, :, :], zg[par][:, :, :], AF.Silu)
                P.tt("dve", on[w_][:, :, :], on[w_][:, :, :], sg[:, :, :], ALU.mult)
                P.dma("sp", o_d[:, c0:c0 + 4, :], on[w_][:, :, :])

    sq2 = [P.sb("sq2_%d" % i, [128, 512]) for i in range(2)]
    rn2 = [P.sb("rn2_%d" % i, [128, 512]) for i in range(2)]
    nb_ = 0
    for blk in range(0, TT, 512):
        n_ = min(512, TT - blk)
        for (src, sc_) in ((xc[0], 128.0 ** -0.5), (xc[1], None)):
            q_, r_, pb = sq2[nb_ % 2], rn2[nb_ % 2], bk[5 + nb_ % 2]
            nb_ += 1
            P.act(q_[:, 0:n_], src[:, blk:blk + n_], AF.Square)
            P.matmul(pb[:, 0:n_], ones[:, :], q_[:, 0:n_])
            P.ts("dve", r_[:, 0:n_], pb[:, 0:n_], 1e-6, ALU.add)
            P.act(r_[:, 0:n_], r_[:, 0:n_], AF.Sqrt)
            P.recip(r_[:, 0:n_], r_[:, 0:n_])
            if sc_ is not None:
                P.stt("dve", src[:, blk:blk + n_], src[:, blk:blk + n_], sc_, r_[:, 0:n_], ALU.mult, ALU.mult)
            else:
                P.tt("dve", src[:, blk:blk + n_], src[:, blk:blk + n_], r_[:, 0:n_], ALU.mult)

    groups = []
    for dr, final in ((1, False), (0, True)):
        gl = list(range(16)) if dr == 0 else list(range(15, -1, -1))
        toks = [S] + [g_ * 256 for g_ in gl]
        for n_, t_ in enumerate(toks):
            groups.append((dr, t_, final, len(groups) % 2, n_ == 0))

    def interleave(ga, gb):
        la, lb = ga is not None, gb is not None
        while la or lb:
            for _ in range(3):
                if la:
                    try:
                        next(ga)
                    except StopIteration:
                        la = False
            if lb:
                try:
                    next(gb)
                except StopIteration:
                    lb = False

    prev = None
    for g in groups:
        interleave(prep(g[0], g[1], g[2], g[3]), recur(*prev) if prev is not None else None)
        prev = g
    interleave(None, recur(*prev))
    if not with_ctx:
        P.memset("dve", on[0][:, :, :], 0.0)
        P.dma("sp", o_d[:, 64:68, :], on[0][:, :, :])
    return P


def gdn_masks():
    a = np.arange(64)
    tf = (a[:, None] <= a[None, :]).astype(np.float32)
    tb = (a[:, None] >= a[None, :]).astype(np.float32)
    ctri = np.stack([tf, tb])
    incl = np.stack([tb, tf])
    strict = np.stack([(a[:, None] > a[None, :]).astype(np.float32), (a[:, None] < a[None, :]).astype(np.float32)])
    negm = np.tile((incl - 1.0) * 1e30, (1, 1, 4)).astype(np.float32)
    smask = np.tile(strict, (1, 1, 4)).astype(np.float32)
    return ctri, negm, smask


def run_GDN(px, pc, conv_w, a_log, dt_bias, norm_w, with_ctx):
    P = build_GDN(with_ctx)
    ident = np.eye(128, dtype=np.float32)
    ctri, negm, smask = gdn_masks()
    nw = np.ascontiguousarray(np.tile(norm_w[None, :], (64, 4)))
    maps = []
    o0 = 4640
    for i in range(NCORES):
        b, h = i // 4, i % 4
        full = np.concatenate([px[b], pc[b]], axis=0)
        raw = np.stack([full[:, o0 + j * 512 + h * 128:o0 + j * 512 + (h + 1) * 128].T for j in range(3)])
        cw = np.stack([conv_w[:, j * 512 + h * 128:j * 512 + (h + 1) * 128].T for j in range(3)])
        z = full[:, 6176 + h * 128:6176 + (h + 1) * 128]
        a = np.stack([full[:, 6688 + d_ * 4 + h].reshape(68, 64).T for d_ in range(2)])
        bb = np.stack([full[:, 6696 + d_ * 4 + h].reshape(68, 64).T for d_ in range(2)])
        maps.append({"raw": np.ascontiguousarray(raw), "cw": np.ascontiguousarray(cw), "z": chunk_tm(z),
                     "a": np.ascontiguousarray(a), "bb": np.ascontiguousarray(bb),
                     "alog": np.ascontiguousarray(np.tile(a_log[None, :, h], (64, 1))),
                     "dtb": np.ascontiguousarray(np.tile(dt_bias[None, :, h], (64, 1))),
                     "ctri": ctri, "negm": negm, "smask": smask, "nw": nw, "ident": ident})
    res = run_prog(P, maps)
    ox = np.zeros((B, S, 512), np.float32)
    oc = np.zeros((B, LCTX, 512), np.float32)
    for i, r in enumerate(res):
        b, h = i // 4, i % 4
        o = unchunk_tm(r["o"])
        ox[b, :, h * 128:(h + 1) * 128] = o[:S]
        oc[b, :, h * 128:(h + 1) * 128] = o[S:]
    return ox, oc


class _Scope:
    def __init__(self, P):
        self.P = P

    def __enter__(self):
        self.old = self.P.es
        self.P.es = ExitStack()
        return self

    def __exit__(self, *a):
        _barrier(self.P)
        self.P.es.close()
        self.P.es = self.old
        return False


Prog.scope = lambda self: _Scope(self)


def build_L3():
    P = Prog()
    cat = P.dram("cat", [NTOK1, D], F32, "ExternalInput")
    hin = P.dram("h", [NTOK1, D], F32, "ExternalInput")
    vlat = P.dram("vlat", [3, D], F32, "ExternalInput")
    vctx = P.dram("vctx", [3, D], F32, "ExternalInput")
    gat = P.dram("gate", [2, D], F32, "ExternalInput")
    w = P.dram("w", [D, D], F32, "ExternalInput")
    wr_d = P.dram("wr", [D, 16], F32, "ExternalInput")
    idn = P.dram("ident", [128, 128], F32, "ExternalInput")
    h1_o = P.dram("h1", [NTOK1, D], F32, "ExternalOutput")
    h2_o = P.dram("h2", [NTOK1, D], F32, "ExternalOutput")
    aff_o = P.dram("aff", [NTOK1, 16], F32, "ExternalOutput")
    ident = P.sb("ident_sb", [128, 128])
    P.dma("sp", ident[:], idn[:])
    A_l, B_l, A_c, B_c = (P.sb(n, [128, D]) for n in ("A_l", "B_l", "A_c", "B_c"))
    load_mod_vectors(P, vlat, A_l, B_l)
    load_mod_vectors(P, vctx, A_c, B_c)
    G_l, G_c = P.sb("G_l", [128, D]), P.sb("G_c", [128, D])
    P.dma("sp", G_l[:], gat.v(gat.h[0:1, :].to_broadcast([128, D])))
    P.dma("sp", G_c[:], gat.v(gat.h[1:2, :].to_broadcast([128, D])))
    wsb = P.sb("wsb", [128, 16, D], MMDT)
    wv = w.h.rearrange("(k p) n -> p k n", p=128)
    for kk in range(0, 16, 2):
        P.dma("pool", wsb[:, kk:kk + 2, :], w.v(wv[:, kk:kk + 2, :]))
    wr = P.sb("wr_sb", [128, 16, 16])
    P.dma("sp", wr[:], wr_d.v(wr_d.h.rearrange("(k p) n -> p k n", p=128)))
    ct = [P.sb("ct%d" % i, [128, D]) for i in range(2)]
    ht = [P.sb("ht%d" % i, [128, D]) for i in range(2)]
    catT = P.sb("catT", [128, 16, 128], MMDT)
    h1 = P.sb("h1_sb", [128, D])
    h2 = P.sb("h2_sb", [128, D])
    h2T = P.sb("h2T", [128, 16, 128])
    tmp = P.sb("tmp", [128, 512])
    ss = P.sb("ss", [128, 1])
    rstd = P.sb("rstd", [128, 1])
    lg = P.sb("lg", [128, 16])
    st = P.sb("st", [128, 4])
    pst = [P.ps("ps%d" % i, [128, 512]) for i in range(8)]
    tiles = [(i * 128, 128) for i in range(8)] + [(1024, 64)]
    for ti, (t0, rows) in enumerate(tiles):
        lat = ti < 8
        c_t, h_t = ct[ti % 2], ht[ti % 2]
        P.dma("sp", c_t[0:rows, :], cat[t0:t0 + rows, :])
        P.dma("sp", h_t[0:rows, :], hin[t0:t0 + rows, :])
        transpose_to_fm(P, c_t, rows, catT, 0, ident, pst[0:2], 0)
        G = G_l if lat else G_c
        for nb in range(4):
            pt = pst[2 + nb % 2]
            for k in range(16):
                P.matmul(pt[0:rows, :], catT[:, k, 0:rows], wsb[:, k, nb * 512:(nb + 1) * 512], start=(k == 0), stop=(k == 15))
            P.tt("dve", tmp[0:rows, :], pt[0:rows, :], G[0:rows, nb * 512:(nb + 1) * 512], ALU.mult)
            P.tt("pool", h1[0:rows, nb * 512:(nb + 1) * 512], tmp[0:rows, :], h_t[0:rows, nb * 512:(nb + 1) * 512], ALU.add)
        P.dma("sp", h1_o[t0:t0 + rows, :], h1[0:rows, :])
        rms_modulate(P, h1, rows, A_l if lat else A_c, B_l if lat else B_c, h2, ss, rstd)
        P.dma("sp", h2_o[t0:t0 + rows, :], h2[0:rows, :])
        transpose_to_fm(P, h2, rows, h2T, 0, ident, pst[4:6], 0)
        pl = pst[6]
        for k in range(16):
            P.matmul(pl[0:rows, 0:16], h2T[:, k, 0:rows], wr[:, k, :], start=(k == 0), stop=(k == 15))
        mx, nmx, sm, rs = (st[0:rows, j:j + 1] for j in range(4))
        P.op("dve", lambda e, a=mx.ap, b_=pl[0:rows, 0:16].ap: e.reduce_max(a, b_, AX.X), reads=[pl], writes=[st])
        P.ts("dve", nmx, mx, -1.0, ALU.mult)
        P.memset("dve", sm, 0.0)
        P.act(lg[0:rows, :], pl[0:rows, 0:16], AF.Exp, bias=nmx, accum=sm)
        P.recip(rs, sm)
        P.ts("dve", lg[0:rows, :], lg[0:rows, :], rs, ALU.mult)
        P.dma("sp", aff_o[t0:t0 + rows, :], lg[0:rows, :])
    return P


def run_L3(catx, catc, hx, hc, mod_l, norm_w, w_out, w_router):
    P = build_L3()
    ident = np.eye(128, dtype=np.float32)
    maps = []
    for i in range(NCORES):
        b = i // 4
        vlat = np.stack([norm_w, mod_l[b, 4 * D:5 * D], mod_l[b, 3 * D:4 * D]])
        vctx = np.stack([norm_w, mod_l[2, 4 * D:5 * D], mod_l[2, 3 * D:4 * D]])
        gate = np.stack([mod_l[b, 2 * D:3 * D], mod_l[2, 2 * D:3 * D]])
        maps.append({"cat": np.ascontiguousarray(tok_shard(catx, catc, i)), "h": np.ascontiguousarray(tok_shard(hx, hc, i)),
                     "vlat": vlat, "vctx": vctx, "gate": gate, "w": w_out, "wr": w_router, "ident": ident})
    res = run_prog(P, maps)
    h1x, h1c = tok_unshard([r["h1"] for r in res], D)
    h2x, h2c = tok_unshard([r["h2"] for r in res], D)
    afx, afc = tok_unshard([r["aff"] for r in res], 16)
    return h1x, h1c, h2x, h2c, afx, afc


CAPX = 512
CAPC = 32


def build_L4(with_ctx):
    P = Prog()
    NTK = 2 * CAPX + (2 * CAPC if with_ctx else 0)
    affx = P.dram("affx", [4, S], F32, "ExternalInput")
    affc = P.dram("affc", [4, LCTX], F32, "ExternalInput")
    h2x = P.dram("h2x", [B, S, D], F32, "ExternalInput")
    h2c = P.dram("h2c", [B, LCTX, D], F32, "ExternalInput")
    wg_d = P.dram("wg", [2, D, D], F32, "ExternalInput")
    wu_d = P.dram("wu", [2, D, D], F32, "ExternalInput")
    wd_d = P.dram("wd", [2, D, D], F32, "ExternalInput")
    idn = P.dram("ident", [128, 128], F32, "ExternalInput")
    accx = P.dram("accx", [B, S, D], F32, "ExternalOutput")
    accc = P.dram("accc", [B, LCTX, D], F32, "ExternalOutput")
    gsc = P.dram("gsc", [4, CAPX], F32, "Internal")
    isc = P.dram("isc", [4, CAPX], U32, "Internal")
    gscc = P.dram("gscc", [4, CAPC], F32, "Internal")
    iscc = P.dram("iscc", [4, CAPC], U32, "Internal")

    ident = P.sb("ident_sb", [128, 128])
    P.dma("sp", ident[:], idn[:])
    gcol = P.sb("gcol", [128, 4, 4])
    icol = P.sb("icol", [128, 4, 4], U32)
    gcolc = P.sb("gcolc", [32, 4])
    icolc = P.sb("icolc", [32, 4], U32)
    zero = P.sb("zero", [128, 2, D])
    P.memset("pool", zero[:], 0.0)
    for b in range(B):
        av = accx.h[b].rearrange("(n p j) d -> n p j d", p=128, j=2)
        for n in range(S // 256):
            P.dma("sp", accx.v(av[n]), zero[:])
        P.dma("sp", accc.v(accc.h[b].rearrange("(p j) d -> p j d", j=2)), zero[:])
    with P.scope():
        W = P.sb("topk_w", [4, S])
        gt = P.sb("topk_g", [4, CAPX])
        it_ = P.sb("topk_i", [4, CAPX], U32)
        P.dma("sp", W[:], affx[:])
        for i in range(CAPX // 8):
            sl = slice(i * 8, (i + 1) * 8)
            P.op("dve", lambda e, o=gt[:, sl].ap, a=W[:].ap: e.max(o, a), reads=[W], writes=[gt])
            P.op("dve", lambda e, o=it_[:, sl].ap, m=gt[:, sl].ap, a=W[:].ap: e.max_index(o, m, a), reads=[W, gt], writes=[it_])
            P.op("dve", lambda e, o=W[:].ap, m=gt[:, sl].ap, a=W[:].ap: e.match_replace(o, m, a, -1.0), reads=[gt], writes=[W])
        P.dma("sp", gsc[:], gt[:])
        P.dma("sp", isc[:], it_[:])
        P.dma("sp", gcol[:], gsc.v(gsc.h.rearrange("r (t p) -> p r t", p=128)), allow_slow_non_contiguous=True)
        P.dma("sp", icol[:], isc.v(isc.h.rearrange("r (t p) -> p r t", p=128)), allow_slow_non_contiguous=True)
        if with_ctx:
            P.dma("sp", W[:, 0:LCTX], affc[:])
            for i in range(CAPC // 8):
                sl = slice(i * 8, (i + 1) * 8)
                P.op("dve", lambda e, o=gt[:, sl].ap, a=W[:, 0:LCTX].ap: e.max(o, a), reads=[W], writes=[gt])
                P.op("dve", lambda e, o=it_[:, sl].ap, m=gt[:, sl].ap, a=W[:, 0:LCTX].ap: e.max_index(o, m, a), reads=[W, gt], writes=[it_])
                P.op("dve", lambda e, o=W[:, 0:LCTX].ap, m=gt[:, sl].ap, a=W[:, 0:LCTX].ap: e.match_replace(o, m, a, -1.0), reads=[gt], writes=[W])
            P.dma("sp", gscc[:], gt[:, 0:CAPC])
            P.dma("sp", iscc[:], it_[:, 0:CAPC])
            P.dma("sp", gcolc[:], gscc.v(gscc.h.rearrange("r p -> p r")), allow_slow_non_contiguous=True)
            P.dma("sp", icolc[:], iscc.v(iscc.h.rearrange("r p -> p r")), allow_slow_non_contiguous=True)
    xsT = P.sb("xsT", [128, 16, NTK], MMDT)
    hidT = P.sb("hidT", [128, 16, NTK], MMDT)
    wbuf = [P.sb("wbuf%d" % i, [128, 16, 512], MMDT) for i in range(4)]
    xs = [P.sb("xs%d" % i, [128, D]) for i in range(2)]
    ysb = [P.sb("ysb%d" % i, [128, D]) for i in range(2)]
    gsb = P.sb("gsb", [128, 512])
    pst = [P.ps("ps%d" % i, [128, 512]) for i in range(8)]
    acc_bufs = [Tile(accx.h.rearrange("b s d -> (b s) d"), "accx%d" % b) for b in range(B)]
    accc_bufs = [Tile(accc.h.rearrange("b s d -> (b s) d"), "accc%d" % b) for b in range(B)]
    h2xf = h2x.v(h2x.h.rearrange("b s d -> (b s) d"))
    h2cf = h2c.v(h2c.h.rearrange("b s d -> (b s) d"))
    blocks = [(0, 512), (512, 512)] + ([(1024, 64)] if with_ctx else [])
    cnt = {"x": 0, "w": 0, "y": 0}
    for el in range(2):
        for b in range(B):
            r = el * 2 + b
            for t in range(4):
                x_ = xs[cnt["x"] % 2]
                cnt["x"] += 1
                P.idma(x_[:, :], None, h2xf, (icol[:, r, t:t + 1], 0), element_offset=b * S * D)
                transpose_to_fm(P, x_, 128, xsT, b * 512 + t * 128, ident, pst[0:2], 0)
            if with_ctx:
                x_ = xs[cnt["x"] % 2]
                cnt["x"] += 1
                P.idma(x_[0:32, :], None, h2cf, (icolc[:, r:r + 1], 0), element_offset=b * LCTX * D)
                transpose_to_fm(P, x_, 32, xsT, 1024 + b * 32, ident, pst[0:2], 0)
        wgv = wg_d.h[el].rearrange("(k p) n -> p k n", p=128)
        wuv = wu_d.h[el].rearrange("(k p) n -> p k n", p=128)
        for fb in range(4):
            wg_t = wbuf[cnt["w"] % 4]
            wu_t = wbuf[(cnt["w"] + 1) % 4]
            cnt["w"] += 2
            for kk in range(0, 16, 4):
                P.dma("pool", wg_t[:, kk:kk + 4, :], wg_d.v(wgv[:, kk:kk + 4, fb * 512:(fb + 1) * 512]))
                P.dma("pool", wu_t[:, kk:kk + 4, :], wu_d.v(wuv[:, kk:kk + 4, fb * 512:(fb + 1) * 512]))
            for fi in range(4):
                ft = fb * 4 + fi
                for bi, (c0, cn) in enumerate(blocks):
                    pg = pst[2 + (bi % 2) * 2]
                    pu = pst[3 + (bi % 2) * 2]
                    for k in range(16):
                        P.matmul(pg[:, 0:cn], wg_t[:, k, fi * 128:(fi + 1) * 128], xsT[:, k, c0:c0 + cn], start=(k == 0), stop=(k == 15))
                    for k in range(16):
                        P.matmul(pu[:, 0:cn], wu_t[:, k, fi * 128:(fi + 1) * 128], xsT[:, k, c0:c0 + cn], start=(k == 0), stop=(k == 15))
                    P.act(gsb[:, 0:cn], pg[:, 0:cn], AF.Silu)
                    P.tt("dve", hidT[:, ft, c0:c0 + cn], gsb[:, 0:cn], pu[:, 0:cn], ALU.mult)
        wdv = wd_d.h[el].rearrange("(k p) n -> p k n", p=128)
        for db in range(4):
            for kk in range(0, 16, 4):
                P.dma("pool", wbuf[db][:, kk:kk + 4, :], wd_d.v(wdv[:, kk:kk + 4, db * 512:(db + 1) * 512]))
        cnt["w"] = 0
        ctiles = [(b, t, b * 512 + t * 128, 128) for b in range(B) for t in range(4)]
        if with_ctx:
            ctiles += [(b, None, 1024 + b * 32, 32) for b in range(B)]
        for (b, t, c0, rows) in ctiles:
            r = el * 2 + b
            y_ = ysb[cnt["y"] % 2]
            for db in range(4):
                pt = pst[6 + db % 2]
                for ft in range(16):
                    P.matmul(pt[0:rows, :], hidT[:, ft, c0:c0 + rows], wbuf[db][:, ft, :], start=(ft == 0), stop=(ft == 15))
                gv = gcol[:, r, t:t + 1] if t is not None else gcolc[:, r:r + 1]
                if db % 2 == 0:
                    P.ts("dve", y_[0:rows, db * 512:(db + 1) * 512], pt[0:rows, :], gv, ALU.mult)
                else:
                    P.act(y_[0:rows, db * 512:(db + 1) * 512], pt[0:rows, :], AF.Copy, scale=gv)
            cnt["y"] += 1
            if t is not None:
                P.idma(acc_bufs[b][:, :], (icol[:, r, t:t + 1], 0), y_[0:rows, :], None, compute_op=ALU.add, element_offset=b * S * D)
            else:
                P.idma(accc_bufs[b][:, :], (icolc[:, r:r + 1], 0), y_[0:rows, :], None, compute_op=ALU.add, element_offset=b * LCTX * D)
    return P


def run_L4(afx, afc, h2x, h2c, wg, wu, wd, with_ctx):
    P = build_L4(with_ctx)
    ident = np.eye(128, dtype=np.float32)
    maps = []
    for i in range(NCORES):
        ax = np.stack([afx[b, :, 2 * i + el] for el in range(2) for b in range(B)])
        ac = np.stack([afc[b, :, 2 * i + el] for el in range(2) for b in range(B)])
        maps.append({"affx": np.ascontiguousarray(ax), "affc": np.ascontiguousarray(ac), "h2x": h2x, "h2c": h2c,
                     "wg": wg[2 * i:2 * i + 2], "wu": wu[2 * i:2 * i + 2], "wd": wd[2 * i:2 * i + 2], "ident": ident})
    res = run_prog(P, maps)
    return np.stack([r["accx"] for r in res]), np.stack([r["accc"] for r in res])


def build_L5(final):
    P = Prog()
    parts = P.dram("parts", [NCORES, NTOK1, D], F32, "ExternalInput")
    h1 = P.dram("h1", [NTOK1, D], F32, "ExternalInput")
    gat = P.dram("gate", [2, D], F32, "ExternalInput")
    fw = P.dram("fw", [1, D], F32, "ExternalInput")
    out = P.dram("out", [NTOK1, D], F32, "ExternalOutput")
    G_l, G_c = P.sb("G_l", [128, D]), P.sb("G_c", [128, D])
    P.dma("sp", G_l[:], gat.v(gat.h[0:1, :].to_broadcast([128, D])))
    P.dma("sp", G_c[:], gat.v(gat.h[1:2, :].to_broadcast([128, D])))
    FW = P.sb("FW", [128, D])
    P.dma("sp", FW[:], fw.v(fw.h[0:1, :].to_broadcast([128, D])))
    pt = [P.sb("pt%d" % i, [128, D]) for i in range(3)]
    acc = [P.sb("acc%d" % i, [128, D]) for i in range(2)]
    ht = [P.sb("ht%d" % i, [128, D]) for i in range(2)]
    yt = P.sb("yt", [128, D])
    ss = P.sb("ss", [128, 1])
    rstd = P.sb("rstd", [128, 1])
    tiles = [(i * 128, 128) for i in range(8)] + [(1024, 64)]
    n = 0
    for ti, (t0, rows) in enumerate(tiles):
        a = acc[ti % 2]
        h_t = ht[ti % 2]
        P.dma("sp", h_t[0:rows, :], h1[t0:t0 + rows, :])
        P.dma("sp", a[0:rows, :], parts.v(parts.h[0, t0:t0 + rows, :]))
        for c in range(1, NCORES):
            p_ = pt[n % 3]
            n += 1
            P.dma("sp", p_[0:rows, :], parts.v(parts.h[c, t0:t0 + rows, :]))
            P.tt("dve" if c % 2 else "pool", a[0:rows, :], a[0:rows, :], p_[0:rows, :], ALU.add)
        G = G_l if ti < 8 else G_c
        P.tt("dve", a[0:rows, :], a[0:rows, :], G[0:rows, :], ALU.mult)
        P.tt("pool", a[0:rows, :], a[0:rows, :], h_t[0:rows, :], ALU.add)
        if final:
            rms_modulate(P, a, rows, FW, None, yt, ss, rstd)
            P.dma("sp", out[t0:t0 + rows, :], yt[0:rows, :])
        else:
            P.dma("sp", out[t0:t0 + rows, :], a[0:rows, :])
    return P


def run_L5(partx, partc, h1x, h1c, mod_l, final_w, final):
    P = build_L5(final)
    maps = []
    for i in range(NCORES):
        b = i // 4
        gate = np.stack([mod_l[b, 5 * D:6 * D], mod_l[2, 5 * D:6 * D]])
        parts = np.stack([tok_shard(partx[c], partc[c], i) for c in range(NCORES)])
        maps.append({"parts": np.ascontiguousarray(parts), "h1": np.ascontiguousarray(tok_shard(h1x, h1c, i)),
                     "gate": gate, "fw": np.ascontiguousarray(final_w[None, :])})
    res = run_prog(P, maps)
    return tok_unshard([r["out"] for r in res], D)


def kernel(x, c, ctx, c_ctx, w_ada, b_ada, norm_mix_w, norm_ffn_w, w_in, w_out, na_rpb,
           gla_gate_w, gla_gate_b, gla_norm_w, gdn_conv_w, gdn_a_log, gdn_dt_bias, gdn_norm_w,
           w_router, w_exp_gate, w_exp_up, w_exp_down, final_norm_w):
    f = lambda a: np.asarray(a, dtype=np.float32)
    x, c, ctx, c_ctx = f(x), f(c), f(ctx), f(c_ctx)
    mod = run_L0({"c": c, "c_ctx": c_ctx, "w_ada": f(w_ada), "b_ada": f(b_ada)})
    hx, hc = x, ctx
    for l in range(DEPTH):
        ctx_out = l < DEPTH - 1
        px, pc = run_L1(hx, hc, mod[l], f(norm_mix_w)[l], f(w_in)[l])
        ox_na, oc_na = run_NA(px, pc, f(na_rpb)[l], ctx_out)
        gx, gc = run_GLA(px, pc, f(gla_gate_w)[l], f(gla_gate_b)[l], f(gla_norm_w)[l], ctx_out)
        dx, dc = run_GDN(px, pc, f(gdn_conv_w)[l], f(gdn_a_log)[l], f(gdn_dt_bias)[l], f(gdn_norm_w)[l], ctx_out)
        catx = np.concatenate([ox_na, gx, dx], axis=-1)
        catc = np.concatenate([oc_na, gc, dc], axis=-1)
        h1x, h1c, h2x, h2c, afx, afc = run_L3(catx, catc, hx, hc, mod[l], f(norm_ffn_w)[l], f(w_out)[l], f(w_router)[l])
        partx, partc = run_L4(afx, afc, h2x, h2c, f(w_exp_gate)[l], f(w_exp_up)[l], f(w_exp_down)[l], ctx_out)
        hx, hc = run_L5(partx, partc, h1x, h1c, mod[l], f(final_norm_w), l == DEPTH - 1)
    return hx
```

```python
import numpy as np
from contextlib import ExitStack
import concourse.bass as bass
import concourse.mybir as mybir
from concourse.bass_utils import run_bass_kernel_spmd

F32 = mybir.dt.float32
BF16 = mybir.dt.bfloat16
I32 = mybir.dt.int32
U32 = mybir.dt.uint32
AF = mybir.ActivationFunctionType
ALU = mybir.AluOpType
AX = mybir.AxisListType

NCORES = 8
D = 2048
B = 2
S = 4096
LCTX = 256
DEPTH = 2
IN_W = 6704
EPS = 1e-6
NDMA = 24


class Buf:
    __slots__ = ("w", "r", "name", "excl", "multi")

    def __init__(self, name):
        self.w = None
        self.r = {}
        self.name = name
        self.excl = False
        self.multi = False


class View:
    __slots__ = ("t", "ap")

    def __init__(self, t, ap):
        self.t = t
        self.ap = ap


class Tile:
    def __init__(self, handle, name):
        self.h = handle
        self.buf = Buf(name)

    def __getitem__(self, idx):
        return View(self.buf, self.h[idx])

    def v(self, ap):
        return View(self.buf, ap)

    def sub(self, name, idx):
        t = Tile(self.h[idx], name)
        t.buf = self.buf
        return t


class Prog:
    def __init__(self):
        self.nc = bass.Bass("TRN2", target_bir_lowering=False)
        nc = self.nc
        self.es = ExitStack()
        self.engs = {"pe": nc.tensor, "act": nc.scalar, "dve": nc.vector, "pool": nc.gpsimd, "sp": nc.sync}
        self.esem = {e: self.es.enter_context(nc.semaphore("s_" + e)) for e in ("pe", "act", "dve", "pool")}
        self.ecnt = {e: 0 for e in self.esem}
        self.dsems = [self.es.enter_context(nc.semaphore("d%d" % i)) for i in range(NDMA)]
        self.dval = [0] * NDMA
        self.dnext = 0
        self.seen = {e: {} for e in self.engs}
        self.nins = 0
        self._psum = None

    def dram(self, name, shape, dt, kind, multi=False):
        t = self.nc.dram_tensor(name, list(shape), dt, kind=kind)
        tl = Tile(t.ap(), name)
        tl.buf.multi = multi
        return tl

    def sb(self, name, shape, dt=F32):
        h = self.es.enter_context(self.nc.sbuf_tensor(name, list(shape), dt))
        return Tile(h, name)

    def ps(self, name, shape, dt=F32):
        h = self.es.enter_context(self.nc.psum_tensor(name, list(shape), dt))
        t = Tile(h, name)
        t.buf.excl = True
        return t

    def _wait(self, eng, evs):
        seen = self.seen[eng]
        best = {}
        for ev in evs:
            if ev is None:
                continue
            k, sem, val = ev
            if eng == "pe" and k == "e:pe":
                continue
            if seen.get(k, 0) >= val:
                continue
            if k not in best or best[k][2] < val:
                best[k] = ev
        for k, (kk, sem, val) in best.items():
            self.engs[eng].wait_ge(sem, val)
            seen[k] = val
            self.nins += 1

    @staticmethod
    def _deps(reads, writes):
        evs = []
        for b in reads:
            if b.w is not None:
                evs.append(b.w)
            if b.excl:
                evs.extend(b.r.values())
        for b in writes:
            if b.w is not None and not b.multi:
                evs.append(b.w)
            evs.extend(b.r.values())
        return evs

    @staticmethod
    def _mark(ev, reads, writes):
        k = ev[0]
        for b in reads:
            b.r[k] = ev
        for b in writes:
            b.w = ev
            b.r = {}

    def op(self, eng, fn, reads=(), writes=()):
        reads = [v.t if isinstance(v, View) else (v.buf if isinstance(v, Tile) else v) for v in reads]
        writes = [v.t if isinstance(v, View) else (v.buf if isinstance(v, Tile) else v) for v in writes]
        self._wait(eng, self._deps(reads, writes))
        ins = fn(self.engs[eng])
        self.ecnt[eng] += 1
        ins.then_inc(self.esem[eng], 1)
        ev = ("e:" + eng, self.esem[eng], self.ecnt[eng])
        self._mark(ev, reads, writes)
        self.nins += 1
        return ev

    def dma(self, q, out, in_, **kw):
        i = self.dnext
        self.dnext = (i + 1) % NDMA
        reads, writes = [in_.t], [out.t]
        evs = self._deps(reads, writes)
        if self.dval[i] > 0:
            evs.append(("d:%d" % i, self.dsems[i], self.dval[i]))
        self._wait(q, evs)
        ins = self.engs[q].dma_start(out=out.ap, in_=in_.ap, **kw)
        self.dval[i] += 16
        ins.then_inc(self.dsems[i], 16)
        ev = ("d:%d" % i, self.dsems[i], self.dval[i])
        self._mark(ev, reads, writes)
        self.nins += 1
        return ev

    def idma(self, out, out_off, in_, in_off, extra_reads=(), **kw):
        i = self.dnext
        self.dnext = (i + 1) % NDMA
        reads, writes = [in_.t] + [v.t for v in extra_reads], [out.t]
        evs = self._deps(reads, writes)
        if self.dval[i] > 0:
            evs.append(("d:%d" % i, self.dsems[i], self.dval[i]))
        self._wait("pool", evs)
        oo = bass.IndirectOffsetOnAxis(ap=out_off[0].ap, axis=out_off[1]) if out_off is not None else None
        io = bass.IndirectOffsetOnAxis(ap=in_off[0].ap, axis=in_off[1]) if in_off is not None else None
        ins = self.nc.gpsimd.indirect_dma_start(out=out.ap, out_offset=oo, in_=in_.ap, in_offset=io, **kw)
        self.dval[i] += 16
        ins.then_inc(self.dsems[i], 16)
        ev = ("d:%d" % i, self.dsems[i], self.dval[i])
        self._mark(ev, reads, writes)
        self.nins += 1
        return ev

    def finish(self):
        evs = [("d:%d" % i, self.dsems[i], self.dval[i]) for i in range(NDMA) if self.dval[i] > 0]
        evs += [("e:" + e, self.esem[e], self.ecnt[e]) for e in self.esem if self.ecnt[e] > 0]
        self._wait("sp", evs)
        self.es.close()
        return self.nc

    def matmul(self, out, lhsT, rhs, start=True, stop=True):
        return self.op("pe", lambda e: e.matmul(out.ap, lhsT.ap, rhs.ap, start=start, stop=stop),
                       reads=[lhsT, rhs], writes=[out])

    def transpose(self, out, in_, ident):
        return self.op("pe", lambda e: e.transpose(out.ap, in_.ap, ident.ap), reads=[in_, ident], writes=[out])

    def act(self, out, in_, func, bias=None, scale=None, accum=None, eng="act"):
        kw = {}
        reads = [in_]
        writes = [out]
        if bias is not None:
            if isinstance(bias, View):
                kw["bias"] = bias.ap
                reads.append(bias)
            else:
                kw["bias"] = bias
        if scale is not None:
            if isinstance(scale, View):
                kw["scale"] = scale.ap
                reads.append(scale)
            else:
                kw["scale"] = scale
        if accum is not None:
            kw["accum_out"] = accum.ap
            writes.append(accum)
        return self.op("act", lambda e: e.activation(out.ap, in_.ap, func, **kw), reads=reads, writes=writes)

    def copy(self, eng, out, in_):
        if eng == "act":
            return self.op("act", lambda e: e.copy(out.ap, in_.ap), reads=[in_], writes=[out])
        return self.op(eng, lambda e: e.tensor_copy(out.ap, in_.ap), reads=[in_], writes=[out])

    def tt(self, eng, out, a, b, op):
        return self.op(eng, lambda e: e.tensor_tensor(out.ap, a.ap, b.ap, op), reads=[a, b], writes=[out])

    def ts(self, eng, out, a, s1, op0, s2=None, op1=None, accum=None):
        reads = [a]
        writes = [out]
        s1a = s1.ap if isinstance(s1, View) else s1
        s2a = s2.ap if isinstance(s2, View) else s2
        if isinstance(s1, View):
            reads.append(s1)
        if isinstance(s2, View):
            reads.append(s2)
        kw = {}
        if op1 is not None:
            kw["op1"] = op1
        if accum is not None:
            kw["accum_out"] = accum.ap
            writes.append(accum)
        return self.op(eng, lambda e: e.tensor_scalar(out.ap, a.ap, s1a, s2a, op0, **kw), reads=reads, writes=writes)

    def stt(self, eng, out, a, s, b, op0, op1):
        reads = [a, b]
        sa = s.ap if isinstance(s, View) else s
        if isinstance(s, View):
            reads.append(s)
        return self.op(eng, lambda e: e.scalar_tensor_tensor(out.ap, a.ap, sa, b.ap, op0, op1), reads=reads, writes=[out])

    def recip(self, out, in_):
        return self.op("dve", lambda e: e.reciprocal(out.ap, in_.ap), reads=[in_], writes=[out])

    def memset(self, eng, out, val):
        return self.op(eng, lambda e: e.memset(out.ap, val), reads=[], writes=[out])


def run_prog(prog, in_maps):
    nc = prog.finish()
    res = run_bass_kernel_spmd(nc, in_maps, core_ids=list(range(NCORES)))
    return res.results


MODC = 6 * D // NCORES


def build_L0():
    P = Prog()
    cT = P.dram("cT", [128, 16, 3], F32, "ExternalInput")
    w = P.dram("w", [DEPTH, D, MODC], F32, "ExternalInput")
    bb = P.dram("b", [DEPTH, MODC], F32, "ExternalInput")
    out = P.dram("out", [DEPTH, 3, MODC], F32, "ExternalOutput")
    c_sb = P.sb("c_sb", [128, 16, 3])
    sc = P.sb("sc", [128, 16, 3])
    P.dma("sp", c_sb[:], cT[:])
    P.act(sc[:], c_sb[:], AF.Silu)
    wt = [P.sb("wt%d" % i, [128, 16, 512]) for i in range(2)]
    bt = [P.sb("bt%d" % i, [3, 512]) for i in range(2)]
    ot = [P.sb("ot%d" % i, [3, 512]) for i in range(2)]
    pt = [P.ps("pt%d" % i, [128, 512]) for i in range(2)]
    it = 0
    for l in range(DEPTH):
        wv = w.h[l].rearrange("(k p) n -> p k n", p=128)
        for j in range(MODC // 512):
            s = it % 2
            P.dma("sp", wt[s][:], w.v(wv[:, :, j * 512:(j + 1) * 512]))
            for r in range(3):
                P.dma("sp", bt[s][r:r + 1, :], bb.v(bb.h[l:l + 1, j * 512:(j + 1) * 512]))
            for k in range(16):
                P.matmul(pt[s][0:3, :], sc[:, k, :], wt[s][:, k, :], start=(k == 0), stop=(k == 15))
            P.tt("dve", ot[s][:], pt[s][0:3, :], bt[s][:], ALU.add)
            P.dma("sp", out.v(out.h[l, :, j * 512:(j + 1) * 512]), ot[s][:])
            it += 1
    return P


def run_L0(inp):
    c_all = np.concatenate([inp["c"], inp["c_ctx"][None, :]], axis=0).astype(np.float32)
    cT = np.ascontiguousarray(c_all.reshape(3, 16, 128).transpose(2, 1, 0))
    P = build_L0()
    maps = []
    for i in range(NCORES):
        maps.append({
            "cT": cT,
            "w": np.ascontiguousarray(inp["w_ada"][:, :, i * MODC:(i + 1) * MODC]),
            "b": np.ascontiguousarray(inp["b_ada"][:, i * MODC:(i + 1) * MODC]),
        })
    res = run_prog(P, maps)
    return np.concatenate([r["out"] for r in res], axis=2)


def _barrier(P):
    evs = [("d:%d" % i, P.dsems[i], P.dval[i]) for i in range(NDMA) if P.dval[i] > 0]
    evs += [("e:" + e, P.esem[e], P.ecnt[e]) for e in P.esem if P.ecnt[e] > 0]
    for e in P.engs:
        P._wait(e, evs)


Prog.barrier = _barrier

MMDT = BF16
NTOK1 = 1024 + 64


def rms_modulate(P, x_t, rows, A, Bt, y_t, ss, rstd):
    P.memset("dve", ss[0:rows, :], 0.0)
    P.act(y_t[0:rows, :], x_t[0:rows, :], AF.Square, accum=ss[0:rows, :])
    P.ts("dve", rstd[0:rows, :], ss[0:rows, :], 1.0 / D, ALU.mult, EPS, ALU.add)
    P.act(rstd[0:rows, :], rstd[0:rows, :], AF.Sqrt)
    P.recip(rstd[0:rows, :], rstd[0:rows, :])
    P.stt("dve", y_t[0:rows, :], x_t[0:rows, :], rstd[0:rows, 0:1], A[0:rows, :], ALU.mult, ALU.mult)
    if Bt is not None:
        P.tt("dve", y_t[0:rows, :], y_t[0:rows, :], Bt[0:rows, :], ALU.add)


def load_mod_vectors(P, vec, A, Bt):
    tmp = P.sb("tmpv_" + A.buf.name, [128, D])
    P.dma("sp", A[:], vec.v(vec.h[0:1, :].to_broadcast([128, D])))
    P.dma("sp", tmp[:], vec.v(vec.h[1:2, :].to_broadcast([128, D])))
    P.dma("sp", Bt[:], vec.v(vec.h[2:3, :].to_broadcast([128, D])))
    P.stt("dve", A[:], tmp[:], 1.0, A[:], ALU.add, ALU.mult)


def transpose_to_fm(P, y_t, rows, dstT, tok0, ident, pst, ev_i):
    for g in range(4):
        pt = pst[(ev_i + g) % len(pst)]
        for j in range(4):
            k = g * 4 + j
            P.transpose(pt[:, j * 128:j * 128 + rows], y_t[0:rows, k * 128:(k + 1) * 128], ident[0:rows, 0:rows])
        src = pt.v(pt.h[:, 0:512].rearrange("p (j t) -> p j t", j=4)[:, :, 0:rows])
        dst = dstT[:, g * 4:(g + 1) * 4, tok0:tok0 + rows]
        P.copy("act" if g % 2 == 0 else "dve", dst, src)


def build_L1():
    P = Prog()
    h = P.dram("h", [NTOK1, D], F32, "ExternalInput")
    vlat = P.dram("vlat", [3, D], F32, "ExternalInput")
    vctx = P.dram("vctx", [3, D], F32, "ExternalInput")
    w = P.dram("w", [D, IN_W], F32, "ExternalInput")
    idn = P.dram("ident", [128, 128], F32, "ExternalInput")
    p = P.dram("p", [NTOK1, IN_W], F32, "ExternalOutput", multi=True)
    ident = P.sb("ident_sb", [128, 128])
    P.dma("sp", ident[:], idn[:])
    A_l, B_l, A_c, B_c = (P.sb(n, [128, D]) for n in ("A_l", "B_l", "A_c", "B_c"))
    load_mod_vectors(P, vlat, A_l, B_l)
    load_mod_vectors(P, vctx, A_c, B_c)
    nxT = P.sb("nxT", [128, 16, NTOK1], MMDT)
    xt = [P.sb("xt%d" % i, [128, D]) for i in range(2)]
    yt = P.sb("yt", [128, D])
    ss = P.sb("ss", [128, 1])
    rstd = P.sb("rstd", [128, 1])
    pst = [P.ps("ps%d" % i, [128, 512]) for i in range(8)]
    tiles = [(i * 128, 128) for i in range(8)] + [(1024, 64)]
    for ti, (t0, rows) in enumerate(tiles):
        x_t = xt[ti % 2]
        P.dma("sp", x_t[0:rows, :], h[t0:t0 + rows, :])
        lat = ti < 8
        rms_modulate(P, x_t, rows, A_l if lat else A_c, B_l if lat else B_c, yt, ss, rstd)
        transpose_to_fm(P, yt, rows, nxT, t0, ident, pst[0:4], ti * 4)
    wv = w.h.rearrange("(k p) n -> p k n", p=128)
    wb = [P.sb("wb%d" % i, [128, 16, 512], MMDT) for i in range(2)]
    ob = [P.sb("ob%d" % i, [128, 512]) for i in range(4)]
    nblk = (IN_W + 511) // 512
    oi = 0
    for nb in range(nblk):
        c0 = nb * 512
        cw = min(512, IN_W - c0)
        wt = wb[nb % 2]
        for kk in range(0, 16, 4):
            P.dma("pool", wt[:, kk:kk + 4, 0:cw], w.v(wv[:, kk:kk + 4, c0:c0 + cw]))
        for ti, (t0, rows) in enumerate(tiles):
            pt = pst[4 + oi % 4]
            for k in range(16):
                P.matmul(pt[0:rows, 0:cw], nxT[:, k, t0:t0 + rows], wt[:, k, 0:cw], start=(k == 0), stop=(k == 15))
            o = ob[oi % 4]
            P.copy("act" if oi % 2 == 0 else "dve", o[0:rows, 0:cw], pt[0:rows, 0:cw])
            P.dma("sp", p[t0:t0 + rows, c0:c0 + cw], o[0:rows, 0:cw])
            oi += 1
    return P


def tok_shard(lat, ctx, i):
    b, q = i // 4, i % 4
    return np.concatenate([lat[b, q * 1024:(q + 1) * 1024], ctx[b, q * 64:(q + 1) * 64]], axis=0)


def tok_unshard(parts, width):
    lat = np.zeros((B, S, width), np.float32)
    ctx = np.zeros((B, LCTX, width), np.float32)
    for i, pp in enumerate(parts):
        b, q = i // 4, i % 4
        lat[b, q * 1024:(q + 1) * 1024] = pp[:1024]
        ctx[b, q * 64:(q + 1) * 64] = pp[1024:]
    return lat, ctx


def run_L1(hx, hc, mod_l, norm_w, w_in):
    P = build_L1()
    ident = np.eye(128, dtype=np.float32)
    maps = []
    for i in range(NCORES):
        b = i // 4
        vlat = np.stack([norm_w, mod_l[b, D:2 * D], mod_l[b, 0:D]])
        vctx = np.stack([norm_w, mod_l[2, D:2 * D], mod_l[2, 0:D]])
        maps.append({"h": np.ascontiguousarray(tok_shard(hx, hc, i)), "vlat": vlat, "vctx": vctx,
                     "w": w_in, "ident": ident})
    res = run_prog(P, maps)
    return tok_unshard([r["p"] for r in res], IN_W)


NEG = -30000.0
NA_DT = BF16


def na_row_class(r):
    return r if r < 4 else (4 if r < 60 else r - 55)


def build_NA(with_ctx):
    P = Prog()
    NH = 4
    TT = S + LCTX
    qT_d = P.dram("qT", [NH, 64, TT], F32, "ExternalInput")
    kT_d = P.dram("kT", [NH, 64, TT], F32, "ExternalInput")
    v_d = P.dram("v", [NH, 64, 68, 64], F32, "ExternalInput")
    bias_d = P.dram("bias", [NH, 64, 9, 512], F32, "ExternalInput")
    idn = P.dram("ident", [128, 128], F32, "ExternalInput")
    o_d = P.dram("o", [NH, 64, 68, 64], F32, "ExternalOutput")
    ident = P.sb("ident_sb", [128, 128])
    P.dma("sp", ident[:], idn[:])
    qT = [P.sb("qT%d" % i, [64, TT], NA_DT) for i in range(2)]
    kT = [P.sb("kT%d" % i, [64, TT], NA_DT) for i in range(2)]
    V = [P.sb("V%d" % i, [64, 68, 64], NA_DT) for i in range(2)]
    bias = [P.sb("bias%d" % i, [64, 9, 512]) for i in range(2)]
    O = [P.sb("O%d" % i, [64, 68, 64]) for i in range(2)]
    Sb = [P.sb("Sb%d" % i, [64, 768]) for i in range(2)]
    Pm = [P.sb("Pm%d" % i, [64, 768], NA_DT) for i in range(2)]
    PT = [P.sb("PT%d" % i, [64, 12, 64], NA_DT) for i in range(2)]
    st = [P.sb("st%d" % i, [64, 4]) for i in range(2)]
    s_loc = [P.ps("s_loc%d" % i, [64, 512]) for i in range(2)]
    s_ctx = [P.ps("s_ctx%d" % i, [64, 256]) for i in range(2)]
    pt_all = [P.ps("pt%d" % i, [64, 768], NA_DT) for i in range(2)]
    o_ps = [P.ps("o_ps%d" % i, [64, 64]) for i in range(2)]
    identb = P.sb("identb", [64, 64], NA_DT)
    P.copy("dve", identb[:, :], ident[0:64, 0:64])
    it = 0
    for hh in range(NH):
        s = hh % 2
        dq = "pool" if NA_DT != F32 else "sp"
        P.dma(dq, qT[s][:], qT_d.v(qT_d.h[hh]))
        P.dma(dq, kT[s][:], kT_d.v(kT_d.h[hh]))
        P.dma(dq, V[s][:], v_d.v(v_d.h[hh]))
        P.dma("sp", bias[s][:], bias_d.v(bias_d.h[hh]))
        nrows = 68 if with_ctx else 64
        if not with_ctx:
            P.memset("pool", O[s][:, 64:68, :], 0.0)
        for r in range(nrows):
            u = it % 2
            it += 1
            local = r < 64
            q_ap = qT[s][:, r * 64:(r + 1) * 64]
            nk = 12 if local else 4
            wid = nk * 64
            if local:
                r0 = min(max(r - 4, 0), 56)
                P.matmul(s_loc[u][:, :], q_ap, kT[s][:, r0 * 64:r0 * 64 + 512])
                P.matmul(s_ctx[u][:, :], q_ap, kT[s][:, S:S + 256])
                P.stt("dve", Sb[u][:, 0:512], s_loc[u][:, :], 0.125, bias[s][:, na_row_class(r), :], ALU.mult, ALU.add)
                P.act(Sb[u][:, 512:768], s_ctx[u][:, :], AF.Identity, scale=0.125)
            else:
                P.matmul(s_ctx[u][:, :], q_ap, kT[s][:, S:S + 256])
                P.act(Sb[u][:, 0:256], s_ctx[u][:, :], AF.Identity, scale=0.125)
            mx, nmx, sm, rs = (st[u][:, j:j + 1] for j in range(4))
            P.op("dve", lambda e, a=mx.ap, b_=Sb[u][:, 0:wid].ap: e.reduce_max(a, b_, AX.X), reads=[Sb[u]], writes=[st[u]])
            P.ts("dve", nmx, mx, -1.0, ALU.mult)
            P.memset("dve", sm, 0.0)
            P.act(Pm[u][:, 0:wid], Sb[u][:, 0:wid], AF.Exp, bias=nmx, accum=sm)
            P.recip(rs, sm)
            for j in range(nk):
                P.transpose(pt_all[u][:, j * 64:(j + 1) * 64], Pm[u][:, j * 64:(j + 1) * 64], identb[:, :])
            ptv = pt_all[u].v(pt_all[u].h[:, 0:nk * 64].rearrange("p (j q) -> p j q", j=nk))
            P.copy("dve" if it % 2 else "act", PT[u][:, 0:nk, :], ptv)
            for j in range(nk):
                if local:
                    vr = (r0 + j) if j < 8 else (64 + j - 8)
                else:
                    vr = 64 + j
                P.matmul(o_ps[u][:, :], PT[u][:, j, :], V[s][:, vr, :], start=(j == 0), stop=(j == nk - 1))
            P.ts("dve", O[s][:, r, :], o_ps[u][:, :], rs, ALU.mult)
        P.dma("sp", o_d.v(o_d.h[hh]), O[s][:])
    return P


def na_bias_tables(rpb_l):
    cq = np.arange(64)
    cstart = np.clip(cq - 8, 0, 48)
    colmask = (cq[None, :] >= cstart[:, None]) & (cq[None, :] < cstart[:, None] + 16)
    dc = np.clip(cq[None, :] - cq[:, None] + 15, 0, 30)
    out = np.full((16, 64, 9, 8, 64), NEG, np.float32)
    for rc in range(9):
        r = rc if rc < 4 else (4 if rc == 4 else rc + 55)
        ridx = min(max(r - 4, 0), 56) + np.arange(8)
        dr = ridx - r + 7
        g = rpb_l[:, dr[:, None, None], dc[None, :, :]]
        g = np.where(colmask[None, None], g, NEG)
        out[:, :, rc] = g.transpose(0, 2, 1, 3)
    return out.reshape(16, 64, 9, 512)


def run_NA(px, pc, rpb_l, with_ctx):
    P = build_NA(with_ctx)
    ident = np.eye(128, dtype=np.float32)
    bias_all = na_bias_tables(rpb_l)
    maps = []
    for i in range(NCORES):
        b, h0 = i // 4, 4 * (i % 4)
        full = np.concatenate([px[b, :, 0:3072], pc[b, :, 0:3072]], axis=0)
        q = full[:, 0:1024].reshape(S + LCTX, 16, 64)[:, h0:h0 + 4]
        k = full[:, 1024:2048].reshape(S + LCTX, 16, 64)[:, h0:h0 + 4]
        v = full[:, 2048:3072].reshape(68, 64, 16, 64)[:, :, h0:h0 + 4]
        maps.append({
            "qT": np.ascontiguousarray(q.transpose(1, 2, 0)),
            "kT": np.ascontiguousarray(k.transpose(1, 2, 0)),
            "v": np.ascontiguousarray(v.transpose(2, 1, 0, 3)),
            "bias": np.ascontiguousarray(bias_all[h0:h0 + 4]),
            "ident": ident,
        })
    res = run_prog(P, maps)
    ox = np.zeros((B, S, 1024), np.float32)
    oc = np.zeros((B, LCTX, 1024), np.float32)
    for i, r in enumerate(res):
        b, h0 = i // 4, 4 * (i % 4)
        o = r["o"].transpose(2, 1, 0, 3).reshape(S + LCTX, 4, 64)
        ox[b, :, h0 * 64:(h0 + 4) * 64] = o[:S].reshape(S, 256)
        oc[b, :, h0 * 64:(h0 + 4) * 64] = o[S:].reshape(LCTX, 256)
    return ox, oc


TT = S + LCTX


def build_GLA(with_ctx):
    P = Prog()
    qT_d = P.dram("qT", [64, TT], F32, "ExternalInput")
    kT_d = P.dram("kT", [64, TT], F32, "ExternalInput")
    v_d = P.dram("v", [64, 68, 128], F32, "ExternalInput")
    g_d = P.dram("g", [64, 68, 128], F32, "ExternalInput")
    lr_d = P.dram("lr", [2, 16, TT], F32, "ExternalInput")
    gw_d = P.dram("gw", [2, 17, 64], F32, "ExternalInput")
    ct_d = P.dram("ct", [64, S], F32, "ExternalInput")
    st_d = P.dram("st", [64, S], F32, "ExternalInput")
    psw_d = P.dram("psw", [64, 64], F32, "ExternalInput")
    tri_d = P.dram("tri", [2, 64, 512], F32, "ExternalInput")
    nw_d = P.dram("nw", [64, 512], F32, "ExternalInput")
    idn = P.dram("ident", [128, 128], F32, "ExternalInput")
    o_d = P.dram("o", [64, 68, 128], F32, "ExternalOutput")

    ident = P.sb("ident_sb", [128, 128])
    P.dma("sp", ident[:], idn[:])
    psw = P.sb("psw_sb", [64, 64])
    P.dma("sp", psw[:], psw_d[:])
    tri = [P.sb("tri%d" % i, [64, 512]) for i in range(2)]
    gw = [P.sb("gw%d" % i, [17, 64]) for i in range(2)]
    for i in range(2):
        P.dma("sp", tri[i][:], tri_d.v(tri_d.h[i]))
        P.dma("sp", gw[i][:], gw_d.v(gw_d.h[i]))
    nw4 = P.sb("nw4", [64, 512])
    P.dma("sp", nw4[:], nw_d[:])
    ob_store = P.sb("ob_store", [64, 68, 128])

    NB = 2
    qg = [P.sb("qg%d" % i, [64, 512]) for i in range(NB)]
    kg = [P.sb("kg%d" % i, [64, 512]) for i in range(NB)]
    vg = [P.sb("vg%d" % i, [64, 8, 128]) for i in range(NB)]
    gg = [P.sb("gg%d" % i, [64, 8, 128]) for i in range(NB)]
    lra = [P.sb("lra%d" % i, [17, 512]) for i in range(NB)]
    ctg = [P.sb("ctg%d" % i, [64, 512]) for i in range(NB)]
    stg = [P.sb("stg%d" % i, [64, 512]) for i in range(NB)]
    tmp = P.sb("tmp", [64, 512])
    e_sb = P.sb("e_sb", [64, 512])
    l_sb = P.sb("l_sb", [64, 512])
    Eq = P.sb("Eq", [64, 512])
    Ek = P.sb("Ek", [64, 512])
    qe = [P.sb("qe%d" % i, [64, 512]) for i in range(NB)]
    keT = P.sb("keT", [64, 512])
    ke = [P.sb("ke%d" % i, [64, 512]) for i in range(NB)]
    attT = [P.sb("attT%d" % i, [64, 512]) for i in range(NB)]
    glast = [P.sb("glast%d" % i, [64, 8]) for i in range(NB)]
    mgt = [P.sb("mgt%d" % i, [64, 128]) for i in range(2)]
    Sb_ = [P.sb("S%d" % i, [64, 128]) for i in range(4)]
    osum = [P.sb("osum%d" % i, [64, 4, 128]) for i in range(2)]
    sq = P.sb("sq", [64, 4, 128])
    on = [P.sb("on%d" % i, [64, 4, 128]) for i in range(2)]
    sg = P.sb("sg", [64, 4, 128])
    ss4 = P.sb("ss4", [64, 4])
    zps = P.ps("zps", [64, 512])
    cps = P.ps("cps", [64, 512])
    aps = P.ps("aps", [64, 512])
    kps = P.ps("kps", [64, 512])
    mps = [P.ps("mps%d" % i, [64, 512]) for i in range(2)]
    ops = [P.ps("ops%d" % i, [64, 512]) for i in range(2)]

    cnt = {"g": 0, "s": 0, "m": 0, "o": 0}

    def group(dr, tok0, ntok, rope, final):
        nch = ntok // 64
        u = cnt["g"] % NB
        cnt["g"] += 1
        c0 = tok0 // 64
        q_, k_, v_, l_ = qg[u], kg[u], vg[u], lra[u]
        P.dma("sp", q_[:, 0:ntok], qT_d[:, tok0:tok0 + ntok])
        P.dma("sp", k_[:, 0:ntok], kT_d[:, tok0:tok0 + ntok])
        P.dma("sp", v_[:, 0:nch, :], v_d[:, c0:c0 + nch, :])
        P.memset("pool", l_[:, :], 1.0)
        P.dma("sp", l_[0:16, 0:ntok], lr_d.v(lr_d.h[dr, :, tok0:tok0 + ntok]))
        if final:
            P.dma("sp", gg[u][:, 0:nch, :], g_d[:, c0:c0 + nch, :])
        if rope:
            P.dma("sp", ctg[u][:, 0:ntok], ct_d[:, tok0:tok0 + ntok])
            P.dma("sp", stg[u][:, 0:ntok], st_d[:, tok0:tok0 + ntok])
            for x in (q_, k_):
                P.matmul(zps[:, 0:ntok], psw[:, :], x[:, 0:ntok])
                P.tt("dve", tmp[:, 0:ntok], zps[:, 0:ntok], stg[u][:, 0:ntok], ALU.mult)
                P.tt("pool", x[:, 0:ntok], x[:, 0:ntok], ctg[u][:, 0:ntok], ALU.mult)
                P.tt("dve", x[:, 0:ntok], x[:, 0:ntok], tmp[:, 0:ntok], ALU.add)
        for j in range(nch):
            P.matmul(zps[:, j * 64:(j + 1) * 64], l_[:, j * 64:(j + 1) * 64], gw[dr][:, :])
        P.act(e_sb[:, 0:ntok], zps[:, 0:ntok], AF.Exp, scale=-1.0)
        P.act(l_sb[:, 0:ntok], e_sb[:, 0:ntok], AF.Ln, bias=1.0)
        for j in range(nch):
            P.matmul(cps[:, j * 64:(j + 1) * 64], l_sb[:, j * 64:(j + 1) * 64], tri[dr][:, 0:64])
        P.act(Eq[:, 0:ntok], cps[:, 0:ntok], AF.Exp, scale=-1.0 / 16.0)
        P.act(Ek[:, 0:ntok], cps[:, 0:ntok], AF.Exp, scale=1.0 / 16.0)
        P.stt("dve", qe[u][:, 0:ntok], q_[:, 0:ntok], 0.125, Eq[:, 0:ntok], ALU.mult, ALU.mult)
        P.tt("dve", keT[:, 0:ntok], k_[:, 0:ntok], Ek[:, 0:ntok], ALU.mult)
        lastcol = 63 if dr == 0 else 0
        P.copy("dve", glast[u][:, 0:nch], Eq.v(Eq.h[:, 0:ntok].rearrange("p (c t) -> p c t", t=64)[:, :, lastcol]))
        for j in range(nch):
            P.matmul(aps[:, j * 64:(j + 1) * 64], keT[:, j * 64:(j + 1) * 64], qe[u][:, j * 64:(j + 1) * 64])
        P.tt("dve", attT[u][:, 0:ntok], aps[:, 0:ntok], tri[dr][:, 0:ntok], ALU.mult)
        for j in range(nch):
            P.transpose(kps[:, j * 64:(j + 1) * 64], keT[:, j * 64:(j + 1) * 64], ident[0:64, 0:64])
        P.copy("act", ke[u][:, 0:ntok], kps[:, 0:ntok])
        for j in range(nch):
            P.matmul(mps[j // 4][:, (j % 4) * 128:(j % 4 + 1) * 128], ke[u][:, j * 64:(j + 1) * 64], v_[:, j, :])
        need_out = with_ctx or tok0 < S
        js = list(range(nch)) if dr == 0 else list(range(nch - 1, -1, -1))
        for n, j in enumerate(js):
            col = (j % 4) * 128
            S_cur = Sb_[cnt["s"] % 4]
            S_nxt = Sb_[(cnt["s"] + 1) % 4]
            cnt["s"] += 1
            if need_out:
                ob = ops[j // 4]
                P.matmul(ob[:, col:col + 128], qe[u][:, j * 64:(j + 1) * 64], S_cur[:, :], start=True, stop=False)
                P.matmul(ob[:, col:col + 128], attT[u][:, j * 64:(j + 1) * 64], v_[:, j, :], start=False, stop=True)
            mg = mgt[cnt["m"] % 2]
            cnt["m"] += 1
            P.ts("dve", mg[:, :], mps[j // 4][:, col:col + 128], glast[u][:, j:j + 1], ALU.mult)
            P.stt("dve", S_nxt[:, :], S_cur[:, :], glast[u][:, j:j + 1], mg[:, :], ALU.mult, ALU.add)
            done_bank = (j % 4 == 3) if dr == 0 else (j % 4 == 0)
            if need_out and done_bank:
                q4 = j // 4
                cb = c0 + 4 * q4
                ob = ops[q4]
                ob3 = ob.v(ob.h[:, :].rearrange("p (c d) -> p c d", c=4))
                if not final:
                    P.copy("act", ob_store[:, cb:cb + 4, :], ob3)
                else:
                    w_ = cnt["o"] % 2
                    cnt["o"] += 1
                    P.tt("dve", osum[w_][:, :, :], ob3, ob_store[:, cb:cb + 4, :], ALU.add)
                    P.act(sq[:, :, :], osum[w_][:, :, :], AF.Square)
                    P.op("dve", lambda e, a=ss4[:, :].ap, b_=sq[:, :, :].ap: e.tensor_reduce(a, b_, AX.X, ALU.add),
                         reads=[sq], writes=[ss4])
                    P.ts("dve", ss4[:, :], ss4[:, :], 1.0 / 128.0, ALU.mult, EPS, ALU.add)
                    P.act(ss4[:, :], ss4[:, :], AF.Sqrt)
                    P.recip(ss4[:, :], ss4[:, :])
                    for jj in range(4):
                        P.ts("dve", on[w_][:, jj, :], osum[w_][:, jj, :], ss4[:, jj:jj + 1], ALU.mult)
                    P.tt("pool", on[w_][:, :, :], on[w_][:, :, :], nw4.v(nw4.h[:, :].rearrange("p (c d) -> p c d", c=4)), ALU.mult)
                    P.act(sg[:, :, :], gg[u][:, 4 * q4:4 * q4 + 4, :], AF.Silu)
                    P.tt("dve", on[w_][:, :, :], on[w_][:, :, :], sg[:, :, :], ALU.mult)
                    P.dma("sp", o_d[:, cb:cb + 4, :], on[w_][:, :, :])

    for dr, final in ((1, False), (0, True)):
        P.memset("dve", Sb_[cnt["s"] % 4][:, :], 0.0)
        group(dr, S, LCTX, False, final)
        gl = list(range(8)) if dr == 0 else list(range(7, -1, -1))
        for g_ in gl:
            group(dr, g_ * 512, 512, True, final)
    if not with_ctx:
        P.memset("dve", on[0][:, :, :], 0.0)
        P.dma("sp", o_d[:, 64:68, :], on[0][:, :, :])
    return P


def rope_tables():
    nf = 16
    freqs = (10000.0 ** (-np.arange(nf, dtype=np.float32) / nf)).astype(np.float32)
    pos = np.arange(S)
    rp, cp = (pos // 64).astype(np.float32), (pos % 64).astype(np.float32)
    ct = np.zeros((64, S), np.float32)
    st = np.zeros((64, S), np.float32)
    psw = np.zeros((64, 64), np.float32)
    for d in range(64):
        half, i = d // 32, d % 32
        f = i % 16
        ang = ((rp if half == 0 else cp) * freqs[f]).astype(np.float32)
        ct[d] = np.cos(ang)
        st[d] = -np.sin(ang) if i < 16 else np.sin(ang)
        partner = d + 16 if i < 16 else d - 16
        psw[partner, d] = 1.0
    return ct, st, psw


def tri_masks():
    a = np.arange(64)
    f = (a[:, None] <= a[None, :]).astype(np.float32)
    bk = (a[:, None] >= a[None, :]).astype(np.float32)
    return np.stack([np.tile(f, (1, 8)), np.tile(bk, (1, 8))])


def chunk_tm(a):
    return np.ascontiguousarray(a.reshape(68, 64, a.shape[-1]).transpose(1, 0, 2))


def unchunk_tm(a):
    return a.transpose(1, 0, 2).reshape(68 * 64, a.shape[-1])


def run_GLA(px, pc, gate_w, gate_b, norm_w, with_ctx):
    P = build_GLA(with_ctx)
    ident = np.eye(128, dtype=np.float32)
    ct, st, psw = rope_tables()
    tri = tri_masks()
    nw = np.ascontiguousarray(np.tile(norm_w[None, :], (64, 4)))
    maps = []
    o0 = 3072
    for i in range(NCORES):
        b, h = i // 4, i % 4
        full = np.concatenate([px[b], pc[b]], axis=0)
        q = full[:, o0 + h * 64:o0 + (h + 1) * 64]
        k = full[:, o0 + 256 + h * 64:o0 + 256 + (h + 1) * 64]
        v = full[:, o0 + 512 + h * 128:o0 + 512 + (h + 1) * 128]
        g = full[:, o0 + 1024 + h * 128:o0 + 1024 + (h + 1) * 128]
        lr = full[:, o0 + 1536:o0 + 1568].reshape(TT, 2, 16)
        gw = np.stack([np.concatenate([gate_w[d][:, h * 64:(h + 1) * 64], gate_b[d][None, h * 64:(h + 1) * 64]], 0) for d in range(2)])
        maps.append({"qT": np.ascontiguousarray(q.T), "kT": np.ascontiguousarray(k.T), "v": chunk_tm(v), "g": chunk_tm(g),
                     "lr": np.ascontiguousarray(lr.transpose(1, 2, 0)), "gw": gw, "ct": ct, "st": st, "psw": psw,
                     "tri": tri, "nw": nw, "ident": ident})
    res = run_prog(P, maps)
    ox = np.zeros((B, S, 512), np.float32)
    oc = np.zeros((B, LCTX, 512), np.float32)
    for i, r in enumerate(res):
        b, h = i // 4, i % 4
        o = unchunk_tm(r["o"])
        ox[b, :, h * 128:(h + 1) * 128] = o[:S]
        oc[b, :, h * 128:(h + 1) * 128] = o[S:]
    return ox, oc


def build_GDN(with_ctx):
    P = Prog()
    raw_d = P.dram("raw", [3, 128, TT], F32, "ExternalInput")
    cw_d = P.dram("cw", [3, 128, 5], F32, "ExternalInput")
    z_d = P.dram("z", [64, 68, 128], F32, "ExternalInput")
    a_d = P.dram("a", [2, 64, 68], F32, "ExternalInput")
    b_d = P.dram("bb", [2, 64, 68], F32, "ExternalInput")
    al_d = P.dram("alog", [64, 2], F32, "ExternalInput")
    dtb_d = P.dram("dtb", [64, 2], F32, "ExternalInput")
    ctri_d = P.dram("ctri", [2, 64, 64], F32, "ExternalInput")
    negm_d = P.dram("negm", [2, 64, 256], F32, "ExternalInput")
    smask_d = P.dram("smask", [2, 64, 256], F32, "ExternalInput")
    nw_d = P.dram("nw", [64, 512], F32, "ExternalInput")
    idn = P.dram("ident", [128, 128], F32, "ExternalInput")
    o_d = P.dram("o", [64, 68, 128], F32, "ExternalOutput")

    ident = P.sb("ident_sb", [128, 128])
    P.dma("sp", ident[:], idn[:])
    ident4 = P.sb("ident4", [64, 256])
    for j in range(4):
        P.dma("sp", ident4[:, j * 64:(j + 1) * 64], idn[0:64, 0:64])
    ones = P.sb("ones", [128, 128])
    P.memset("dve", ones[:], 1.0)
    negones = P.sb("negones", [64, 64])
    P.memset("dve", negones[:], -1.0)
    ctri = [P.sb("ctri%d" % i, [64, 64]) for i in range(2)]
    trione = [P.sb("trione%d" % i, [64, 192]) for i in range(2)]
    negm = [P.sb("negm%d" % i, [64, 256]) for i in range(2)]
    smask = [P.sb("smask%d" % i, [64, 256]) for i in range(2)]
    for i in range(2):
        P.dma("sp", ctri[i][:], ctri_d.v(ctri_d.h[i]))
        P.dma("sp", trione[i][:, 0:64], ctri_d.v(ctri_d.h[i]))
        P.memset("dve", trione[i][:, 64:192], 1.0)
        P.dma("sp", negm[i][:], negm_d.v(negm_d.h[i]))
        P.dma("sp", smask[i][:], smask_d.v(smask_d.h[i]))
    nw4 = P.sb("nw4", [64, 512])
    P.dma("sp", nw4[:], nw_d[:])
    cw = P.sb("cw_sb", [128, 3, 5])
    for i in range(3):
        P.dma("sp", cw[:, i, :], cw_d.v(cw_d.h[i]))

    xc = [P.sb("xc%d" % i, [128, TT]) for i in range(3)]
    upad = P.sb("upad", [128, S + 4])
    upc = P.sb("upc", [128, LCTX + 4])
    P.memset("pool", upad[:], 0.0)
    P.memset("pool", upc[:], 0.0)
    for i in range(3):
        P.dma("sp", upad[:, 2:2 + S], raw_d.v(raw_d.h[i, :, 0:S]))
        P.dma("sp", upc[:, 2:2 + LCTX], raw_d.v(raw_d.h[i, :, S:TT]))
        for (src, off, n) in ((upad, 0, S), (upc, S, LCTX)):
            y = xc[i][:, off:off + n]
            P.ts("dve", y, src[:, 0:n], cw[:, i, 0:1], ALU.mult)
            for j in range(1, 5):
                P.stt("dve", y, src[:, j:j + n], cw[:, i, j:j + 1], y, ALU.mult, ALU.add)
            P.act(y, y, AF.Silu)

    al = P.sb("al_sb", [64, 2])
    dtb = P.sb("dtb_sb", [64, 2])
    P.dma("sp", al[:], al_d[:])
    P.dma("sp", dtb[:], dtb_d[:])
    nea = P.sb("nea", [64, 2])
    P.act(nea[:], al[:], AF.Exp)
    P.ts("dve", nea[:], nea[:], -1.0, ALU.mult)
    g_all = [P.sb("g_all%d" % i, [64, 68]) for i in range(2)]
    beta = [P.sb("beta%d" % i, [64, 68]) for i in range(2)]
    nbeta = [P.sb("nbeta%d" % i, [64, 68]) for i in range(2)]
    tmpa = P.sb("tmpa", [64, 68])
    for d_ in range(2):
        P.dma("sp", tmpa[:], a_d.v(a_d.h[d_]))
        P.act(tmpa[:], tmpa[:], AF.Exp, bias=dtb[:, d_:d_ + 1])
        P.act(tmpa[:], tmpa[:], AF.Ln, bias=1.0)
        P.ts("dve", g_all[d_][:], tmpa[:], nea[:, d_:d_ + 1], ALU.mult)
        P.dma("sp", beta[d_][:], b_d.v(b_d.h[d_]))
        P.act(beta[d_][:], beta[d_][:], AF.Sigmoid)
        P.ts("dve", nbeta[d_][:], beta[d_][:], -1.0, ALU.mult)

    import os
    STOP = int(os.environ.get("GDN_STOP", "99"))
    if STOP == 0:
        return P
    ob_store = P.sb("ob_store", [64, 68, 128])
    def t64(n):
        return P.sb(n, [64, 256])
    qdT = [P.sb("qdT%d" % i, [128, 256]) for i in range(2)]
    egcb = P.sb("egcb", [128, 256])
    Gt = P.sb("Gt", [64, 4, 192])
    glb = [P.sb("glb_sb%d" % i, [128, 4]) for i in range(2)]
    gct = P.sb("gct_sb", [64, 4])
    egc = P.sb("egc", [64, 4])
    ekd = P.sb("ekd", [64, 4])
    bws = P.sb("bws", [64, 4])
    Dm, decay, decs, A_, aqk, AT, Pj, PTj, RT = (t64(n) for n in
                                                 ("Dm", "decay", "decs", "A_", "aqk", "AT", "Pj", "PTj", "RT"))
    aqkT = [t64("aqkT%d" % i) for i in range(2)]
    ktok = P.sb("ktok", [64, 4, 128])
    vtok = P.sb("vtok", [64, 4, 128])
    bu = P.sb("bu", [64, 4, 128])
    bwk = P.sb("bwk", [64, 4, 128])
    kdec = [P.sb("kdec%d" % i, [64, 4, 128]) for i in range(2)]
    u_sb = [P.sb("u_sb%d" % i, [64, 4, 128]) for i in range(2)]
    wT = [P.sb("wT%d" % i, [128, 256]) for i in range(2)]
    vnew = [P.sb("vnew%d" % i, [64, 128]) for i in range(2)]
    Sst = [P.sb("Sst%d" % i, [128, 128]) for i in range(4)]
    zg = [P.sb("zg%d" % i, [64, 4, 128]) for i in range(2)]
    osum = P.sb("osum", [64, 4, 128])
    sq4 = P.sb("sq4", [64, 4, 128])
    on = [P.sb("on%d" % i, [64, 4, 128]) for i in range(2)]
    sg = P.sb("sg", [64, 4, 128])
    ss4 = P.sb("ss4", [64, 4])
    bk = [P.ps("bank%d" % i, [128, 512]) for i in range(8)]
    half = lambda t, i, nm, rows=128: t.sub(nm, (slice(0, rows), slice(i * 256, (i + 1) * 256)))
    p_D, p_egc = half(bk[0], 0, "p_D", 64), half(bk[0], 1, "p_egc")
    p_QK, p_RP = half(bk[1], 0, "p_QK", 64), half(bk[1], 1, "p_RP", 64)
    p_A, p_B = half(bk[2], 0, "p_A", 64), half(bk[2], 1, "p_B", 64)
    p_wT = half(bk[7], 1, "p_wT")
    p_vn = bk[3].sub("p_vn", (slice(0, 64), slice(256, 384)))
    p_S = bk[3].sub("p_S", (slice(0, 128), slice(384, 512)))
    p_tok = bk[4].sub("p_tok", (slice(0, 64), slice(0, 512)))
    p_u = bk[5]
    p_o = bk[6].sub("p_o", (slice(0, 64), slice(0, 512)))
    p_glb = bk[7].sub("p_glb", (slice(0, 128), slice(0, 4)))
    p_gct = bk[7].sub("p_gct", (slice(0, 64), slice(4, 8)))

    cnt = {"s": 0, "v": 0, "o": 0}
    i64 = ident[0:64, 0:64]

    def c64(j):
        return slice(j * 64, (j + 1) * 64)

    class _Stop(Exception):
        pass

    def ck(n):
        if STOP == n:
            raise _Stop()

    def prep(dr, tok0, final, par):
        c0 = tok0 // 64
        ts_ = slice(tok0, tok0 + 256)
        qnT = xc[0].sub("qnTv", (slice(None), ts_))
        knT = xc[1].sub("knTv", (slice(None), ts_))
        yield
        gsl = g_all[dr][:, c0:c0 + 4]
        P.matmul(p_glb[:, :], ones[0:64, :], gsl)
        P.matmul(p_gct[:, :], ctri[dr][:, :], gsl)
        P.act(glb[par][:], p_glb[:, :], AF.Exp)
        P.act(egc[:], p_gct[:, :], AF.Exp)
        P.copy("dve", gct[:], p_gct[:, :])
        P.tt("dve", ekd[:], p_glb[0:64, :], gct[:], ALU.subtract)
        P.act(ekd[:], ekd[:], AF.Exp)
        P.tt("dve", bws[:], beta[dr][:, c0:c0 + 4], egc[:], ALU.mult)
        yield
        for (srcT, dst) in ((knT, ktok), (None, vtok)):
            for j in range(4):
                in_ = srcT[:, c64(j)] if srcT is not None else xc[2][:, tok0 + j * 64:tok0 + (j + 1) * 64]
                P.transpose(p_tok[:, j * 128:(j + 1) * 128], in_, ident[:, :])
            P.copy("act", dst[:, :, :], p_tok.v(p_tok.h[:, :].rearrange("p (c d) -> p c d", c=4)))
        for j in range(4):
            P.ts("dve", bu[:, j, :], vtok[:, j, :], beta[dr][:, c0 + j:c0 + j + 1], ALU.mult)
            P.ts("pool", bwk[:, j, :], ktok[:, j, :], bws[:, j:j + 1], ALU.mult)
            P.ts("pool", kdec[par][:, j, :], ktok[:, j, :], ekd[:, j:j + 1], ALU.mult)
        yield
        for j in range(4):
            P.ts("dve", Gt[:, j, :], trione[dr][:, :], g_all[dr][:, c0 + j:c0 + j + 1], ALU.mult)
        yield
        for j in range(4):
            P.matmul(p_D[:, c64(j)], ctri[dr][:, :], Gt[:, j, 64:128], start=True, stop=False)
            P.matmul(p_D[:, c64(j)], negones[:, :], Gt[:, j, 0:64], start=False, stop=True)
        yield
        for j in range(4):
            P.matmul(p_egc[:, c64(j)], Gt[:, j, 64:192], ctri[dr][:, :])
        yield
        P.act(egcb[:], p_egc[:, :], AF.Exp)
        P.tt("dve", qdT[par][:], qnT[:], egcb[:], ALU.mult)
        P.tt("dve", Dm[:], p_D[:, :], negm[dr][:], ALU.add)
        P.act(decay[:], Dm[:], AF.Exp)
        P.tt("pool", decs[:], decay[:], smask[dr][:], ALU.mult)
        yield
        for j in range(4):
            P.matmul(p_D[:, c64(j)], knT[:, c64(j)], knT[:, c64(j)])
            P.matmul(p_QK[:, c64(j)], qnT[:, c64(j)], knT[:, c64(j)])
        for j in range(4):
            P.stt("dve", A_[:, c64(j)], p_D[:, c64(j)], nbeta[dr][:, c0 + j:c0 + j + 1], decs[:, c64(j)], ALU.mult, ALU.mult)
        P.tt("dve", aqk[:], p_QK[:, :], decay[:], ALU.mult)
        for j in range(4):
            P.transpose(p_A[:, c64(j)], A_[:, c64(j)], i64)
            P.transpose(p_B[:, c64(j)], aqk[:, c64(j)], i64)
        P.copy("act", AT[:], p_A[:, :])
        P.copy("dve", aqkT[par][:], p_B[:, :])
        yield
        P.tt("dve", RT[:], ident4[:], AT[:], ALU.add)
        Pc, PTc = A_, AT
        for lvl in range(1, 6):
            for j in range(4):
                P.matmul(p_A[:, c64(j)], PTc[:, c64(j)], Pc[:, c64(j)])
            if lvl < 5:
                for j in range(4):
                    P.matmul(p_B[:, c64(j)], Pc[:, c64(j)], PTc[:, c64(j)])
            P.copy("act", Pj[:], p_A[:, :])
            if lvl < 5:
                P.copy("dve", PTj[:], p_B[:, :])
            for j in range(4):
                P.matmul(p_RP[:, c64(j)], Pj[:, c64(j)], RT[:, c64(j)])
            P.tt("dve", RT[:], RT[:], p_RP[:, :], ALU.add)
            Pc, PTc = Pj, PTj
            yield
        yield
        for j in range(4):
            P.matmul(p_u[0:64, j * 128:(j + 1) * 128], RT[:, c64(j)], bu[:, j, :])
            P.matmul(p_wT[:, c64(j)], bwk[:, j, :], RT[:, c64(j)])
        P.copy("act", u_sb[par][:, :, :], p_u.v(p_u.h[0:64, :].rearrange("p (c d) -> p c d", c=4)))
        P.copy("dve", wT[par][:], p_wT[:, :])
        yield

    def recur(dr, tok0, final, par, first):
        c0 = tok0 // 64
        if first:
            P.memset("dve", Sst[cnt["s"] % 4][:, :], 0.0)
        need_out = with_ctx or tok0 < S
        if need_out and final:
            P.dma("sp", zg[par][:, :, :], z_d[:, c0:c0 + 4, :])
        js = list(range(4)) if dr == 0 else [3, 2, 1, 0]
        for j in js:
            S_cur = Sst[cnt["s"] % 4]
            S_nxt = Sst[(cnt["s"] + 1) % 4]
            cnt["s"] += 1
            vn = vnew[cnt["v"] % 2]
            cnt["v"] += 1
            P.matmul(p_vn[:, :], wT[par][:, c64(j)], S_cur[:, :])
            P.tt("dve", vn[:, :], u_sb[par][:, j, :], p_vn[:, :], ALU.subtract)
            if need_out:
                P.matmul(p_o[:, j * 128:(j + 1) * 128], qdT[par][:, c64(j)], S_cur[:, :], start=True, stop=False)
                P.matmul(p_o[:, j * 128:(j + 1) * 128], aqkT[par][:, c64(j)], vn[:, :], start=False, stop=True)
            P.matmul(p_S[:, :], kdec[par][:, j, :], vn[:, :])
            P.stt("dve", S_nxt[:, :], S_cur[:, :], glb[par][:, j:j + 1], p_S[:, :], ALU.mult, ALU.add)
            yield
        if need_out:
            ob3 = p_o.v(p_o.h[:, :].rearrange("p (c d) -> p c d", c=4))
            if not final:
                P.copy("act", ob_store[:, c0:c0 + 4, :], ob3)
            else:
                w_ = cnt["o"] % 2
                cnt["o"] += 1
                P.tt("dve", osum[:, :, :], ob3, ob_store[:, c0:c0 + 4, :], ALU.add)
                P.act(sq4[:, :, :], osum[:, :, :], AF.Square)
                P.op("dve", lambda e, a=ss4[:, :].ap, b_=sq4[:, :, :].ap: e.tensor_reduce(a, b_, AX.X, ALU.add),
                     reads=[sq4], writes=[ss4])
                P.ts("dve", ss4[:, :], ss4[:, :], 1.0 / 128.0, ALU.mult, EPS, ALU.add)
                P.act(ss4[:, :], ss4[:, :], AF.Sqrt)
                P.recip(ss4[:, :], ss4[:, :])
                for jj in range(4):
                    P.ts("dve", on[w_][:, jj, :], osum[:, jj, :], ss4[:, jj:jj + 1], ALU.mult)
                P.tt("pool", on[w_][:, :, :], on[w_][:, :, :], nw4.v(nw4.h[:, :].rearrange("p (c d) -> p c d", c=4)), ALU.mult)
                P.act(sg[:, :, :], zg[par][:, :, :], AF.Silu)
                P.tt("dve", on[w_][:, :, :], on[w_][:, :, :], sg[:, :, :], ALU.mult)
                P.dma("sp", o_d[:, c0:c0 + 4, :], on[w_][:, :, :])

    sq2 = [P.sb("sq2_%d" % i, [128, 512]) for i in range(2)]
    rn2 = [P.sb("rn2_%d" % i, [128, 512]) for i in range(2)]
    nb_ = 0
    for blk in range(0, TT, 512):
        n_ = min(512, TT - blk)
        for (src, sc_) in ((xc[0], 128.0 ** -0.5), (xc[1], None)):
            q_, r_, pb = sq2[nb_ % 2], rn2[nb_ % 2], bk[5 + nb_ % 2]
            nb_ += 1
            P.act(q_[:, 0:n_], src[:, blk:blk + n_], AF.Square)
            P.matmul(pb[:, 0:n_], ones[:, :], q_[:, 0:n_])
            P.ts("dve", r_[:, 0:n_], pb[:, 0:n_], 1e-6, ALU.add)
            P.act(r_[:, 0:n_], r_[:, 0:n_], AF.Sqrt)
            P.recip(r_[:, 0:n_], r_[:, 0:n_])
            if sc_ is not None:
                P.stt("dve", src[:, blk:blk + n_], src[:, blk:blk + n_], sc_, r_[:, 0:n_], ALU.mult, ALU.mult)
            else:
                P.tt("dve", src[:, blk:blk + n_], src[:, blk:blk + n_], r_[:, 0:n_], ALU.mult)

    groups = []
    for dr, final in ((1, False), (0, True)):
        gl = list(range(16)) if dr == 0 else list(range(15, -1, -1))
        toks = [S] + [g_ * 256 for g_ in gl]
        for n_, t_ in enumerate(toks):
            groups.append((dr, t_, final, len(groups) % 2, n_ == 0))

    def interleave(ga, gb):
        la, lb = ga is not None, gb is not None
        while la or lb:
            for _ in range(3):
                if la:
                    try:
                        next(ga)
                    except StopIteration:
                        la = False
            if lb:
                try:
                    next(gb)
                except StopIteration:
                    lb = False

    prev = None
    for g in groups:
        interleave(prep(g[0], g[1], g[2], g[3]), recur(*prev) if prev is not None else None)
        prev = g
    interleave(None, recur(*prev))
    if not with_ctx:
        P.memset("dve", on[0][:, :, :], 0.0)
        P.dma("sp", o_d[:, 64:68, :], on[0][:, :, :])
    return P


def gdn_masks():
    a = np.arange(64)
    tf = (a[:, None] <= a[None, :]).astype(np.float32)
    tb = (a[:, None] >= a[None, :]).astype(np.float32)
    ctri = np.stack([tf, tb])
    incl = np.stack([tb, tf])
    strict = np.stack([(a[:, None] > a[None, :]).astype(np.float32), (a[:, None] < a[None, :]).astype(np.float32)])
    negm = np.tile((incl - 1.0) * 1e30, (1, 1, 4)).astype(np.float32)
    smask = np.tile(strict, (1, 1, 4)).astype(np.float32)
    return ctri, negm, smask


def run_GDN(px, pc, conv_w, a_log, dt_bias, norm_w, with_ctx):
    P = build_GDN(with_ctx)
    ident = np.eye(128, dtype=np.float32)
    ctri, negm, smask = gdn_masks()
    nw = np.ascontiguousarray(np.tile(norm_w[None, :], (64, 4)))
    maps = []
    o0 = 4640
    for i in range(NCORES):
        b, h = i // 4, i % 4
        full = np.concatenate([px[b], pc[b]], axis=0)
        raw = np.stack([full[:, o0 + j * 512 + h * 128:o0 + j * 512 + (h + 1) * 128].T for j in range(3)])
        cw = np.stack([conv_w[:, j * 512 + h * 128:j * 512 + (h + 1) * 128].T for j in range(3)])
        z = full[:, 6176 + h * 128:6176 + (h + 1) * 128]
        a = np.stack([full[:, 6688 + d_ * 4 + h].reshape(68, 64).T for d_ in range(2)])
        bb = np.stack([full[:, 6696 + d_ * 4 + h].reshape(68, 64).T for d_ in range(2)])
        maps.append({"raw": np.ascontiguousarray(raw), "cw": np.ascontiguousarray(cw), "z": chunk_tm(z),
                     "a": np.ascontiguousarray(a), "bb": np.ascontiguousarray(bb),
                     "alog": np.ascontiguousarray(np.tile(a_log[None, :, h], (64, 1))),
                     "dtb": np.ascontiguousarray(np.tile(dt_bias[None, :, h], (64, 1))),
                     "ctri": ctri, "negm": negm, "smask": smask, "nw": nw, "ident": ident})
    res = run_prog(P, maps)
    ox = np.zeros((B, S, 512), np.float32)
    oc = np.zeros((B, LCTX, 512), np.float32)
    for i, r in enumerate(res):
        b, h = i // 4, i % 4
        o = unchunk_tm(r["o"])
        ox[b, :, h * 128:(h + 1) * 128] = o[:S]
        oc[b, :, h * 128:(h + 1) * 128] = o[S:]
    return ox, oc


class _Scope:
    def __init__(self, P):
        self.P = P

    def __enter__(self):
        self.old = self.P.es
        self.P.es = ExitStack()
        return self

    def __exit__(self, *a):
        _barrier(self.P)
        self.P.es.close()
        self.P.es = self.old
        return False


Prog.scope = lambda self: _Scope(self)


def build_L3():
    P = Prog()
    cat = P.dram("cat", [NTOK1, D], F32, "ExternalInput")
    hin = P.dram("h", [NTOK1, D], F32, "ExternalInput")
    vlat = P.dram("vlat", [3, D], F32, "ExternalInput")
    vctx = P.dram("vctx", [3, D], F32, "ExternalInput")
    gat = P.dram("gate", [2, D], F32, "ExternalInput")
    w = P.dram("w", [D, D], F32, "ExternalInput")
    wr_d = P.dram("wr", [D, 16], F32, "ExternalInput")
    idn = P.dram("ident", [128, 128], F32, "ExternalInput")
    h1_o = P.dram("h1", [NTOK1, D], F32, "ExternalOutput", multi=True)
    h2_o = P.dram("h2", [NTOK1, D], F32, "ExternalOutput", multi=True)
    aff_o = P.dram("aff", [NTOK1, 16], F32, "ExternalOutput", multi=True)
    ident = P.sb("ident_sb", [128, 128])
    P.dma("sp", ident[:], idn[:])
    A_l, B_l, A_c, B_c = (P.sb(n, [128, D]) for n in ("A_l", "B_l", "A_c", "B_c"))
    load_mod_vectors(P, vlat, A_l, B_l)
    load_mod_vectors(P, vctx, A_c, B_c)
    G_l, G_c = P.sb("G_l", [128, D]), P.sb("G_c", [128, D])
    P.dma("sp", G_l[:], gat.v(gat.h[0:1, :].to_broadcast([128, D])))
    P.dma("sp", G_c[:], gat.v(gat.h[1:2, :].to_broadcast([128, D])))
    wsb = P.sb("wsb", [128, 16, D], MMDT)
    wv = w.h.rearrange("(k p) n -> p k n", p=128)
    for kk in range(0, 16, 2):
        P.dma("pool", wsb[:, kk:kk + 2, :], w.v(wv[:, kk:kk + 2, :]))
    wr = P.sb("wr_sb", [128, 16, 16])
    P.dma("sp", wr[:], wr_d.v(wr_d.h.rearrange("(k p) n -> p k n", p=128)))
    ct = [P.sb("ct%d" % i, [128, D]) for i in range(2)]
    ht = [P.sb("ht%d" % i, [128, D]) for i in range(2)]
    catT = P.sb("catT", [128, 16, 128], MMDT)
    h1 = P.sb("h1_sb", [128, D])
    h2 = P.sb("h2_sb", [128, D])
    h2T = P.sb("h2T", [128, 16, 128])
    tmp = P.sb("tmp", [128, 512])
    ss = P.sb("ss", [128, 1])
    rstd = P.sb("rstd", [128, 1])
    lg = P.sb("lg", [128, 16])
    st = P.sb("st", [128, 4])
    pst = [P.ps("ps%d" % i, [128, 512]) for i in range(8)]
    tiles = [(i * 128, 128) for i in range(8)] + [(1024, 64)]
    for ti, (t0, rows) in enumerate(tiles):
        lat = ti < 8
        c_t, h_t = ct[ti % 2], ht[ti % 2]
        P.dma("sp", c_t[0:rows, :], cat[t0:t0 + rows, :])
        P.dma("sp", h_t[0:rows, :], hin[t0:t0 + rows, :])
        transpose_to_fm(P, c_t, rows, catT, 0, ident, pst[0:2], 0)
        G = G_l if lat else G_c
        for nb in range(4):
            pt = pst[2 + nb % 2]
            for k in range(16):
                P.matmul(pt[0:rows, :], catT[:, k, 0:rows], wsb[:, k, nb * 512:(nb + 1) * 512], start=(k == 0), stop=(k == 15))
            P.tt("dve", tmp[0:rows, :], pt[0:rows, :], G[0:rows, nb * 512:(nb + 1) * 512], ALU.mult)
            P.tt("pool", h1[0:rows, nb * 512:(nb + 1) * 512], tmp[0:rows, :], h_t[0:rows, nb * 512:(nb + 1) * 512], ALU.add)
        P.dma("sp", h1_o[t0:t0 + rows, :], h1[0:rows, :])
        rms_modulate(P, h1, rows, A_l if lat else A_c, B_l if lat else B_c, h2, ss, rstd)
        P.dma("sp", h2_o[t0:t0 + rows, :], h2[0:rows, :])
        transpose_to_fm(P, h2, rows, h2T, 0, ident, pst[4:6], 0)
        pl = pst[6]
        for k in range(16):
            P.matmul(pl[0:rows, 0:16], h2T[:, k, 0:rows], wr[:, k, :], start=(k == 0), stop=(k == 15))
        mx, nmx, sm, rs = (st[0:rows, j:j + 1] for j in range(4))
        P.op("dve", lambda e, a=mx.ap, b_=pl[0:rows, 0:16].ap: e.reduce_max(a, b_, AX.X), reads=[pl], writes=[st])
        P.ts("dve", nmx, mx, -1.0, ALU.mult)
        P.memset("dve", sm, 0.0)
        P.act(lg[0:rows, :], pl[0:rows, 0:16], AF.Exp, bias=nmx, accum=sm)
        P.recip(rs, sm)
        P.ts("dve", lg[0:rows, :], lg[0:rows, :], rs, ALU.mult)
        P.dma("sp", aff_o[t0:t0 + rows, :], lg[0:rows, :])
    return P


def run_L3(catx, catc, hx, hc, mod_l, norm_w, w_out, w_router):
    P = build_L3()
    ident = np.eye(128, dtype=np.float32)
    maps = []
    for i in range(NCORES):
        b = i // 4
        vlat = np.stack([norm_w, mod_l[b, 4 * D:5 * D], mod_l[b, 3 * D:4 * D]])
        vctx = np.stack([norm_w, mod_l[2, 4 * D:5 * D], mod_l[2, 3 * D:4 * D]])
        gate = np.stack([mod_l[b, 2 * D:3 * D], mod_l[2, 2 * D:3 * D]])
        maps.append({"cat": np.ascontiguousarray(tok_shard(catx, catc, i)), "h": np.ascontiguousarray(tok_shard(hx, hc, i)),
                     "vlat": vlat, "vctx": vctx, "gate": gate, "w": w_out, "wr": w_router, "ident": ident})
    res = run_prog(P, maps)
    h1x, h1c = tok_unshard([r["h1"] for r in res], D)
    h2x, h2c = tok_unshard([r["h2"] for r in res], D)
    afx, afc = tok_unshard([r["aff"] for r in res], 16)
    return h1x, h1c, h2x, h2c, afx, afc


CAPX = 512
CAPC = 32


def build_L4(with_ctx):
    P = Prog()
    NTK = 2 * CAPX + (2 * CAPC if with_ctx else 0)
    affx = P.dram("affx", [4, S], F32, "ExternalInput")
    affc = P.dram("affc", [4, LCTX], F32, "ExternalInput")
    h2x = P.dram("h2x", [B, S, D], F32, "ExternalInput")
    h2c = P.dram("h2c", [B, LCTX, D], F32, "ExternalInput")
    wg_d = P.dram("wg", [2, D, D], F32, "ExternalInput")
    wu_d = P.dram("wu", [2, D, D], F32, "ExternalInput")
    wd_d = P.dram("wd", [2, D, D], F32, "ExternalInput")
    idn = P.dram("ident", [128, 128], F32, "ExternalInput")
    accx = P.dram("accx", [B, S, D], F32, "ExternalOutput")
    accc = P.dram("accc", [B, LCTX, D], F32, "ExternalOutput")
    gsc = P.dram("gsc", [4, CAPX], F32, "Internal")
    isc = P.dram("isc", [4, CAPX], U32, "Internal")
    gscc = P.dram("gscc", [4, CAPC], F32, "Internal")
    iscc = P.dram("iscc", [4, CAPC], U32, "Internal")

    ident = P.sb("ident_sb", [128, 128])
    P.dma("sp", ident[:], idn[:])
    gcol = P.sb("gcol", [128, 4, 4])
    icol = P.sb("icol", [128, 4, 4], U32)
    gcolc = P.sb("gcolc", [32, 4])
    icolc = P.sb("icolc", [32, 4], U32)
    zero = P.sb("zero", [128, 2, D])
    P.memset("pool", zero[:], 0.0)
    for b in range(B):
        av = accx.h[b].rearrange("(n p j) d -> n p j d", p=128, j=2)
        for n in range(S // 256):
            P.dma("sp", accx.v(av[n]), zero[:])
        P.dma("sp", accc.v(accc.h[b].rearrange("(p j) d -> p j d", j=2)), zero[:])
    with P.scope():
        W = P.sb("topk_w", [4, S])
        gt = P.sb("topk_g", [4, CAPX])
        it_ = P.sb("topk_i", [4, CAPX], U32)
        P.dma("sp", W[:], affx[:])
        for i in range(CAPX // 8):
            sl = slice(i * 8, (i + 1) * 8)
            P.op("dve", lambda e, o=gt[:, sl].ap, a=W[:].ap: e.max(o, a), reads=[W], writes=[gt])
            P.op("dve", lambda e, o=it_[:, sl].ap, m=gt[:, sl].ap, a=W[:].ap: e.max_index(o, m, a), reads=[W, gt], writes=[it_])
            P.op("dve", lambda e, o=W[:].ap, m=gt[:, sl].ap, a=W[:].ap: e.match_replace(o, m, a, -1.0), reads=[gt], writes=[W])
        P.dma("sp", gsc[:], gt[:])
        P.dma("sp", isc[:], it_[:])
        P.dma("sp", gcol[:], gsc.v(gsc.h.rearrange("r (t p) -> p r t", p=128)), allow_slow_non_contiguous=True)
        P.dma("sp", icol[:], isc.v(isc.h.rearrange("r (t p) -> p r t", p=128)), allow_slow_non_contiguous=True)
        if with_ctx:
            P.dma("sp", W[:, 0:LCTX], affc[:])
            for i in range(CAPC // 8):
                sl = slice(i * 8, (i + 1) * 8)
                P.op("dve", lambda e, o=gt[:, sl].ap, a=W[:, 0:LCTX].ap: e.max(o, a), reads=[W], writes=[gt])
                P.op("dve", lambda e, o=it_[:, sl].ap, m=gt[:, sl].ap, a=W[:, 0:LCTX].ap: e.max_index(o, m, a), reads=[W, gt], writes=[it_])
                P.op("dve", lambda e, o=W[:, 0:LCTX].ap, m=gt[:, sl].ap, a=W[:, 0:LCTX].ap: e.match_replace(o, m, a, -1.0), reads=[gt], writes=[W])
            P.dma("sp", gscc[:], gt[:, 0:CAPC])
            P.dma("sp", iscc[:], it_[:, 0:CAPC])
            P.dma("sp", gcolc[:], gscc.v(gscc.h.rearrange("r p -> p r")), allow_slow_non_contiguous=True)
            P.dma("sp", icolc[:], iscc.v(iscc.h.rearrange("r p -> p r")), allow_slow_non_contiguous=True)
    xsT = P.sb("xsT", [128, 16, NTK], MMDT)
    hidT = P.sb("hidT", [128, 16, NTK], MMDT)
    wbuf = [P.sb("wbuf%d" % i, [128, 16, 512], MMDT) for i in range(4)]
    xs = [P.sb("xs%d" % i, [128, D]) for i in range(2)]
    ysb = [P.sb("ysb%d" % i, [128, D]) for i in range(2)]
    gsb = P.sb("gsb", [128, 512])
    pst = [P.ps("ps%d" % i, [128, 512]) for i in range(8)]
    acc_bufs = [Tile(accx.h.rearrange("b s d -> (b s) d"), "accx%d" % b) for b in range(B)]
    accc_bufs = [Tile(accc.h.rearrange("b s d -> (b s) d"), "accc%d" % b) for b in range(B)]
    h2xf = h2x.v(h2x.h.rearrange("b s d -> (b s) d"))
    h2cf = h2c.v(h2c.h.rearrange("b s d -> (b s) d"))
    blocks = [(0, 512), (512, 512)] + ([(1024, 64)] if with_ctx else [])
    cnt = {"x": 0, "w": 0, "y": 0}
    for el in range(2):
        for b in range(B):
            r = el * 2 + b
            for t in range(4):
                x_ = xs[cnt["x"] % 2]
                cnt["x"] += 1
                P.idma(x_[:, :], None, h2xf, (icol[:, r, t:t + 1], 0), element_offset=b * S * D)
                transpose_to_fm(P, x_, 128, xsT, b * 512 + t * 128, ident, pst[0:2], 0)
            if with_ctx:
                x_ = xs[cnt["x"] % 2]
                cnt["x"] += 1
                P.idma(x_[0:32, :], None, h2cf, (icolc[:, r:r + 1], 0), element_offset=b * LCTX * D)
                transpose_to_fm(P, x_, 32, xsT, 1024 + b * 32, ident, pst[0:2], 0)
        wgv = wg_d.h[el].rearrange("(k p) n -> p k n", p=128)
        wuv = wu_d.h[el].rearrange("(k p) n -> p k n", p=128)
        for fb in range(4):
            wg_t = wbuf[cnt["w"] % 4]
            wu_t = wbuf[(cnt["w"] + 1) % 4]
            cnt["w"] += 2
            for kk in range(0, 16, 4):
                P.dma("pool", wg_t[:, kk:kk + 4, :], wg_d.v(wgv[:, kk:kk + 4, fb * 512:(fb + 1) * 512]))
                P.dma("pool", wu_t[:, kk:kk + 4, :], wu_d.v(wuv[:, kk:kk + 4, fb * 512:(fb + 1) * 512]))
            for fi in range(4):
                ft = fb * 4 + fi
                for bi, (c0, cn) in enumerate(blocks):
                    pg = pst[2 + (bi % 2) * 2]
                    pu = pst[3 + (bi % 2) * 2]
                    for k in range(16):
                        P.matmul(pg[:, 0:cn], wg_t[:, k, fi * 128:(fi + 1) * 128], xsT[:, k, c0:c0 + cn], start=(k == 0), stop=(k == 15))
                    for k in range(16):
                        P.matmul(pu[:, 0:cn], wu_t[:, k, fi * 128:(fi + 1) * 128], xsT[:, k, c0:c0 + cn], start=(k == 0), stop=(k == 15))
                    P.act(gsb[:, 0:cn], pg[:, 0:cn], AF.Silu)
                    P.tt("dve", hidT[:, ft, c0:c0 + cn], gsb[:, 0:cn], pu[:, 0:cn], ALU.mult)
        wdv = wd_d.h[el].rearrange("(k p) n -> p k n", p=128)
        for db in range(4):
            for kk in range(0, 16, 4):
                P.dma("pool", wbuf[db][:, kk:kk + 4, :], wd_d.v(wdv[:, kk:kk + 4, db * 512:(db + 1) * 512]))
        cnt["w"] = 0
        ctiles = [(b, t, b * 512 + t * 128, 128) for b in range(B) for t in range(4)]
        if with_ctx:
            ctiles += [(b, None, 1024 + b * 32, 32) for b in range(B)]
        for (b, t, c0, rows) in ctiles:
            r = el * 2 + b
            y_ = ysb[cnt["y"] % 2]
            for db in range(4):
                pt = pst[6 + db % 2]
                for ft in range(16):
                    P.matmul(pt[0:rows, :], hidT[:, ft, c0:c0 + rows], wbuf[db][:, ft, :], start=(ft == 0), stop=(ft == 15))
                gv = gcol[:, r, t:t + 1] if t is not None else gcolc[:, r:r + 1]
                if db % 2 == 0:
                    P.ts("dve", y_[0:rows, db * 512:(db + 1) * 512], pt[0:rows, :], gv, ALU.mult)
                else:
                    P.act(y_[0:rows, db * 512:(db + 1) * 512], pt[0:rows, :], AF.Copy, scale=gv)
            cnt["y"] += 1
            if t is not None:
                P.idma(acc_bufs[b][:, :], (icol[:, r, t:t + 1], 0), y_[0:rows, :], None, compute_op=ALU.add, element_offset=b * S * D)
            else:
                P.idma(accc_bufs[b][:, :], (icolc[:, r:r + 1], 0), y_[0:rows, :], None, compute_op=ALU.add, element_offset=b * LCTX * D)
    return P


def run_L4(afx, afc, h2x, h2c, wg, wu, wd, with_ctx):
    P = build_L4(with_ctx)
    ident = np.eye(128, dtype=np.float32)
    maps = []
    for i in range(NCORES):
        ax = np.stack([afx[b, :, 2 * i + el] for el in range(2) for b in range(B)])
        ac = np.stack([afc[b, :, 2 * i + el] for el in range(2) for b in range(B)])
        maps.append({"affx": np.ascontiguousarray(ax), "affc": np.ascontiguousarray(ac), "h2x": h2x, "h2c": h2c,
                     "wg": wg[2 * i:2 * i + 2], "wu": wu[2 * i:2 * i + 2], "wd": wd[2 * i:2 * i + 2], "ident": ident})
    res = run_prog(P, maps)
    return np.stack([r["accx"] for r in res]), np.stack([r["accc"] for r in res])


def build_L5(final):
    P = Prog()
    parts = P.dram("parts", [NCORES, NTOK1, D], F32, "ExternalInput")
    h1 = P.dram("h1", [NTOK1, D], F32, "ExternalInput")
    gat = P.dram("gate", [2, D], F32, "ExternalInput")
    fw = P.dram("fw", [1, D], F32, "ExternalInput")
    out = P.dram("out", [NTOK1, D], F32, "ExternalOutput", multi=True)
    G_l, G_c = P.sb("G_l", [128, D]), P.sb("G_c", [128, D])
    P.dma("sp", G_l[:], gat.v(gat.h[0:1, :].to_broadcast([128, D])))
    P.dma("sp", G_c[:], gat.v(gat.h[1:2, :].to_broadcast([128, D])))
    FW = P.sb("FW", [128, D])
    P.dma("sp", FW[:], fw.v(fw.h[0:1, :].to_broadcast([128, D])))
    pt = [P.sb("pt%d" % i, [128, D]) for i in range(3)]
    acc = [P.sb("acc%d" % i, [128, D]) for i in range(2)]
    ht = [P.sb("ht%d" % i, [128, D]) for i in range(2)]
    yt = P.sb("yt", [128, D])
    ss = P.sb("ss", [128, 1])
    rstd = P.sb("rstd", [128, 1])
    tiles = [(i * 128, 128) for i in range(8)] + [(1024, 64)]
    n = 0
    for ti, (t0, rows) in enumerate(tiles):
        a = acc[ti % 2]
        h_t = ht[ti % 2]
        P.dma("sp", h_t[0:rows, :], h1[t0:t0 + rows, :])
        P.dma("sp", a[0:rows, :], parts.v(parts.h[0, t0:t0 + rows, :]))
        for c in range(1, NCORES):
            p_ = pt[n % 3]
            n += 1
            P.dma("sp", p_[0:rows, :], parts.v(parts.h[c, t0:t0 + rows, :]))
            P.tt("dve" if c % 2 else "pool", a[0:rows, :], a[0:rows, :], p_[0:rows, :], ALU.add)
        G = G_l if ti < 8 else G_c
        P.tt("dve", a[0:rows, :], a[0:rows, :], G[0:rows, :], ALU.mult)
        P.tt("pool", a[0:rows, :], a[0:rows, :], h_t[0:rows, :], ALU.add)
        if final:
            rms_modulate(P, a, rows, FW, None, yt, ss, rstd)
            P.dma("sp", out[t0:t0 + rows, :], yt[0:rows, :])
        else:
            P.dma("sp", out[t0:t0 + rows, :], a[0:rows, :])
    return P


def run_L5(partx, partc, h1x, h1c, mod_l, final_w, final):
    P = build_L5(final)
    maps = []
    for i in range(NCORES):
        b = i // 4
        gate = np.stack([mod_l[b, 5 * D:6 * D], mod_l[2, 5 * D:6 * D]])
        parts = np.stack([tok_shard(partx[c], partc[c], i) for c in range(NCORES)])
        maps.append({"parts": np.ascontiguousarray(parts), "h1": np.ascontiguousarray(tok_shard(h1x, h1c, i)),
                     "gate": gate, "fw": np.ascontiguousarray(final_w[None, :])})
    res = run_prog(P, maps)
    return tok_unshard([r["out"] for r in res], D)


def kernel(x, c, ctx, c_ctx, w_ada, b_ada, norm_mix_w, norm_ffn_w, w_in, w_out, na_rpb,
           gla_gate_w, gla_gate_b, gla_norm_w, gdn_conv_w, gdn_a_log, gdn_dt_bias, gdn_norm_w,
           w_router, w_exp_gate, w_exp_up, w_exp_down, final_norm_w):
    f = lambda a: np.asarray(a, dtype=np.float32)
    x, c, ctx, c_ctx = f(x), f(c), f(ctx), f(c_ctx)
    mod = run_L0({"c": c, "c_ctx": c_ctx, "w_ada": f(w_ada), "b_ada": f(b_ada)})
    hx, hc = x, ctx
    for l in range(DEPTH):
        ctx_out = l < DEPTH - 1
        px, pc = run_L1(hx, hc, mod[l], f(norm_mix_w)[l], f(w_in)[l])
        ox_na, oc_na = run_NA(px, pc, f(na_rpb)[l], ctx_out)
        gx, gc = run_GLA(px, pc, f(gla_gate_w)[l], f(gla_gate_b)[l], f(gla_norm_w)[l], ctx_out)
        dx, dc = run_GDN(px, pc, f(gdn_conv_w)[l], f(gdn_a_log)[l], f(gdn_dt_bias)[l], f(gdn_norm_w)[l], ctx_out)
        catx = np.concatenate([ox_na, gx, dx], axis=-1)
        catc = np.concatenate([oc_na, gc, dc], axis=-1)
        h1x, h1c, h2x, h2c, afx, afc = run_L3(catx, catc, hx, hc, mod[l], f(norm_ffn_w)[l], f(w_out)[l], f(w_router)[l])
        partx, partc = run_L4(afx, afc, h2x, h2c, f(w_exp_gate)[l], f(w_exp_up)[l], f(w_exp_down)[l], ctx_out)
        hx, hc = run_L5(partx, partc, h1x, h1c, mod[l], f(final_norm_w), l == DEPTH - 1)
    return hx
```
